# Optimizing a Trainium2 kernel written in Bass

```python
import functools
import jax
import jax.numpy as jnp
from jax import lax
import numpy as np

D_MODEL = 2048
BATCH = 2
SEQ = 16384
DEPTH = 2

CTX_LEN = 256
GRID_W = 64

GLA_HEADS = 4
GLA_DK = 128
GLA_DV = 256
GLA_GATE_RANK = 16
GLA_TAU = 16.0
GLA_CHUNK = 64
LRU_WIDTH = 1024
LRU_BLOCKS = 8
LRU_C = 8.0
CONV_W = 4
MLSTM_HEADS = 8
MLSTM_DK = 128
MLSTM_DV = 256
MLSTM_CHUNK = 64
M_INIT = -1e30
N_GROUPS = 4
EXPERTS_PER_GROUP = 8
N_EXPERTS = N_GROUPS * EXPERTS_PER_GROUP
TOP_K = 2
D_EXPERT = 512
MOE_BLOCK = 128
DN_ALPHA = (2 * DEPTH) ** 0.25
DN_BETA = (8 * DEPTH) ** -0.25
LN_EPS = 1e-5
N_EVEN = (DEPTH + 1) // 2
N_ODD = DEPTH // 2
GLA_QK = GLA_HEADS * GLA_DK
GLA_V = GLA_HEADS * GLA_DV
EVEN_SIZES = (GLA_QK, GLA_QK, GLA_V, GLA_V, GLA_GATE_RANK, GLA_GATE_RANK, LRU_WIDTH, LRU_WIDTH)
EVEN_IN = sum(EVEN_SIZES)
EVEN_MIX = GLA_V + LRU_WIDTH
ML_QK = MLSTM_HEADS * MLSTM_DK
ML_V = MLSTM_HEADS * MLSTM_DV
ODD_SIZES = (ML_QK, ML_QK, ML_V, ML_V, MLSTM_HEADS, MLSTM_HEADS, MLSTM_HEADS, MLSTM_HEADS)
ODD_IN = sum(ODD_SIZES)

kernel_name = 'hybrid_gla_rglru_mlstm_hmoe_dit'


def layer_norm(x, g, b):
    xf = x.astype(jnp.float32)
    mu = jnp.mean(xf, -1, keepdims=True)
    var = jnp.mean(jnp.square(xf - mu), -1, keepdims=True)
    return ((xf - mu) * lax.rsqrt(var + LN_EPS)).astype(x.dtype) * g + b


def head_rms_norm(h, g):
    return h * lax.rsqrt(jnp.mean(jnp.square(h), -1, keepdims=True) + LN_EPS) * g


def cond_modulation(cond, w, b):
    return (jax.nn.silu(cond) @ w + b).reshape(cond.shape[0], 6, -1)


def modulate(h, m, j):
    return h * (1 + m[:, j + 1, None]) + m[:, j, None]


def split_cols(p, sizes):
    out, off = [], 0
    for s in sizes:
        out.append(p[..., off:off + s])
        off += s
    return out


def split_heads(a, n_heads):
    B, T, _ = a.shape
    return a.reshape(B, T, n_heads, -1).transpose(0, 2, 1, 3)


def merge_heads(a):
    B, H, T, d = a.shape
    return a.transpose(0, 2, 1, 3).reshape(B, T, H * d)


def to_chunks(a, chunk):
    B, H, T = a.shape[:3]
    return jnp.moveaxis(a.reshape((B, H, T // chunk, chunk) + a.shape[3:]), 2, 0)


def from_chunks(a):
    a = jnp.moveaxis(a, 0, 2)
    return a.reshape(a.shape[:2] + (-1,) + a.shape[4:])


def centred_dwconv(x, w, b):
    left = CONV_W // 2
    right = CONV_W - 1 - left
    T = x.shape[1]
    xp = jnp.pad(x, ((0, 0), (left, right), (0, 0)))
    return b + sum(xp[:, j:j + T] * w[j] for j in range(CONV_W))


def bidir_with_prefix(scan_f, scan_b, ctx_f, lat_f, ctx_b, lat_b, init, t_axis):
    def flip(a):
        return jnp.flip(a, t_axis)
    yc_f, st_f = scan_f(*ctx_f, init)
    yl_f, _ = scan_f(*lat_f, st_f)
    yc_b, st_b = scan_b(*[flip(a) for a in ctx_b], init)
    yl_b, _ = scan_b(*[flip(a) for a in lat_b], st_b)
    return yc_f + flip(yc_b), yl_f + flip(yl_b)


def gla_chunk_scan(q, k, v, log_a, s0):
    mask = jnp.tril(jnp.ones((GLA_CHUNK, GLA_CHUNK), bool))

    def step(s, blk):
        qc, kc, vc, gc = blk
        b = jnp.cumsum(gc, axis=-2)
        b_last = b[..., -1:, :]
        q_dec = qc * jnp.exp(b)
        k_inv = kc * jnp.exp(-b)
        k_end = kc * jnp.exp(b_last - b)
        att = jnp.where(mask, jnp.einsum('bhtd,bhsd->bhts', q_dec, k_inv), 0.0)
        o = jnp.einsum('bhts,bhsv->bhtv', att, vc) + jnp.einsum('bhtd,bhdv->bhtv', q_dec, s)
        s_new = jnp.swapaxes(jnp.exp(b_last), -1, -2) * s + jnp.einsum('bhsd,bhsv->bhdv', k_end, vc)
        return s_new, o

    xs = tuple(to_chunks(a, GLA_CHUNK) for a in (q, k, v, log_a))
    s_fin, o = lax.scan(step, s0, xs)
    return from_chunks(o), s_fin


def rglru_scan(xc, h0, *, w_r, b_r, w_i, b_i, lam):
    B, T, W = xc.shape
    xb = xc.reshape(B, T, LRU_BLOCKS, W // LRU_BLOCKS)
    r = jax.nn.sigmoid(jnp.einsum('btnc,ncd->btnd', xb, w_r).reshape(B, T, W) + b_r)
    i = jax.nn.sigmoid(jnp.einsum('btnc,ncd->btnd', xb, w_i).reshape(B, T, W) + b_i)
    log_a = LRU_C * r * jax.nn.log_sigmoid(lam)
    a = jnp.exp(log_a)
    u = jnp.sqrt(-jnp.expm1(2.0 * log_a)) * (i * xc)
    u = u.at[:, 0].add(a[:, 0] * h0)

    def combine(left, right):
        a_l, u_l = left
        a_r, u_r = right
        return a_l * a_r, a_r * u_l + u_r

    _, h = lax.associative_scan(combine, (a, u), axis=1)
    return h, h[:, -1]


def mlstm_chunk_scan(q, k, v, i_pre, f_log, state):
    mask = jnp.tril(jnp.ones((MLSTM_CHUNK, MLSTM_CHUNK), bool))

    def step(carry, blk):
        cs, ns, m = carry
        qc, kc, vc, ic, fc = blk
        b = jnp.cumsum(fc, axis=-1)
        dmat = jnp.where(mask, b[..., :, None] - b[..., None, :] + ic[..., None, :], -jnp.inf)
        inter = b + m[..., None]
        m_t = jnp.maximum(inter, jnp.max(dmat, -1))
        w_intra = jnp.exp(dmat - m_t[..., None])
        w_inter = jnp.exp(inter - m_t)
        s = jnp.einsum('bhtd,bhsd->bhts', qc, kc) * w_intra
        num = jnp.einsum('bhts,bhsv->bhtv', s, vc) + w_inter[..., None] * jnp.einsum('bhtd,bhdv->bhtv', qc, cs)
        den = jnp.sum(s, -1) + w_inter * jnp.einsum('bhtd,bhd->bht', qc, ns)
        h = num / jnp.maximum(jnp.abs(den), jnp.exp(-m_t))[..., None]
        b_last = b[..., -1]
        g = b_last[..., None] - b + ic
        m_new = jnp.maximum(b_last + m, jnp.max(g, -1))
        wk = jnp.exp(g - m_new[..., None])
        decay = jnp.exp(b_last + m - m_new)
        cs_new = decay[..., None, None] * cs + jnp.einsum('bhs,bhsd,bhsv->bhdv', wk, kc, vc)
        ns_new = decay[..., None] * ns + jnp.einsum('bhs,bhsd->bhd', wk, kc)
        return (cs_new, ns_new, m_new), h

    xs = tuple(to_chunks(a, MLSTM_CHUNK) for a in (q, k, v, i_pre, f_log))
    st, h = lax.scan(step, state, xs)
    return from_chunks(h), st


def even_mixer(u_lat, u_ctx, w_in, w_a2, b_a, gla_g, conv_w, conv_b, w_r, b_r, w_i, b_i, lam, w_out, ctx_out):
    def prep(u):
        q, k, v, r, af, ab, xr, xg = split_cols((u @ w_in).astype(jnp.float32), EVEN_SIZES)
        log_af = split_heads(jax.nn.log_sigmoid(af @ w_a2[0] + b_a[0]) / GLA_TAU, GLA_HEADS)
        log_ab = split_heads(jax.nn.log_sigmoid(ab @ w_a2[1] + b_a[1]) / GLA_TAU, GLA_HEADS)
        q = split_heads(q, GLA_HEADS) * GLA_DK ** -0.5
        k = split_heads(k, GLA_HEADS)
        v = split_heads(v, GLA_HEADS)
        xc = centred_dwconv(xr, conv_w, conv_b)
        return (q, k, v, log_af), (q, k, v, log_ab), r, (xc,), xg

    cf, cb, rc, xcc, gc = prep(u_ctx)
    lf, lb, rl, xcl, gl = prep(u_lat)
    B = u_lat.shape[0]
    s0 = jnp.zeros((B, GLA_HEADS, GLA_DK, GLA_DV), jnp.float32)
    o_ctx, o_lat = bidir_with_prefix(gla_chunk_scan, gla_chunk_scan, cf, lf, cb, lb, s0, 2)
    lru_f = functools.partial(rglru_scan, w_r=w_r[0], b_r=b_r[0], w_i=w_i[0], b_i=b_i[0], lam=lam[0])
    lru_b = functools.partial(rglru_scan, w_r=w_r[1], b_r=b_r[1], w_i=w_i[1], b_i=b_i[1], lam=lam[1])
    h0 = jnp.zeros((B, LRU_WIDTH), jnp.float32)
    r_ctx, r_lat = bidir_with_prefix(lru_f, lru_b, xcc, xcl, xcc, xcl, h0, 1)

    def finish(o, r, h, g, dtype):
        o = merge_heads(head_rms_norm(o, gla_g)) * jax.nn.silu(r)
        y = h * jax.nn.gelu(g)
        return jnp.concatenate([o, y], -1).astype(dtype) @ w_out

    y_lat = finish(o_lat, rl, r_lat, gl, u_lat.dtype)
    y_ctx = finish(o_ctx, rc, r_ctx, gc, u_ctx.dtype) if ctx_out else None
    return y_lat, y_ctx


def odd_mixer(u_lat, u_ctx, w_in, b_gate, ml_g, w_out, ctx_out):
    B, S, D = u_lat.shape
    rows = S // GRID_W
    u_col = u_lat.reshape(B, rows, GRID_W, D).transpose(0, 2, 1, 3).reshape(B, S, D)

    def gate(a, j):
        return jnp.swapaxes(a, 1, 2) + b_gate[j][None, :, None]

    def prep(u):
        q, k, v, o, i_f, f_f, i_b, f_b = split_cols((u @ w_in).astype(jnp.float32), ODD_SIZES)
        q = split_heads(q, MLSTM_HEADS)
        k = split_heads(k, MLSTM_HEADS) * MLSTM_DK ** -0.5
        v = split_heads(v, MLSTM_HEADS)
        fwd = (q, k, v, gate(i_f, 0), jax.nn.log_sigmoid(gate(f_f, 1)))
        bwd = (q, k, v, gate(i_b, 2), jax.nn.log_sigmoid(gate(f_b, 3)))
        return fwd, bwd, o

    cf, cb, oc = prep(u_ctx)
    lf, lb, ol = prep(u_col)
    init = (jnp.zeros((B, MLSTM_HEADS, MLSTM_DK, MLSTM_DV), jnp.float32),
            jnp.zeros((B, MLSTM_HEADS, MLSTM_DK), jnp.float32),
            jnp.full((B, MLSTM_HEADS), M_INIT, jnp.float32))
    h_ctx, h_lat = bidir_with_prefix(mlstm_chunk_scan, mlstm_chunk_scan, cf, lf, cb, lb, init, 2)

    def finish(h, o, dtype):
        return (merge_heads(head_rms_norm(h, ml_g)) * jax.nn.sigmoid(o)).astype(dtype) @ w_out

    y_col = finish(h_lat, ol, u_lat.dtype)
    y_lat = y_col.reshape(B, GRID_W, rows, D).transpose(0, 2, 1, 3).reshape(B, S, D)
    y_ctx = finish(h_ctx, oc, u_ctx.dtype) if ctx_out else None
    return y_lat, y_ctx


def hier_moe(u, w_group, b_group, w_expert, b_expert, w_gate, w_up, w_down):
    T, D = u.shape
    uf = u.astype(jnp.float32)
    g_logits = uf @ w_group + b_group
    g_idx = jnp.argmax(g_logits, -1)
    p_g = jnp.take_along_axis(jax.nn.softmax(g_logits, -1), g_idx[:, None], 1)
    e_logits = (uf @ w_expert + b_expert).reshape(T, N_GROUPS, EXPERTS_PER_GROUP)
    e_logits = jnp.take_along_axis(e_logits, g_idx[:, None, None], 1)[:, 0]
    top_p, top_i = lax.top_k(jax.nn.softmax(e_logits, -1), TOP_K)
    gate = (p_g * top_p / jnp.sum(top_p, -1, keepdims=True)).reshape(-1)
    expert_id = (g_idx[:, None] * EXPERTS_PER_GROUP + top_i).reshape(-1).astype(jnp.int32)
    token_id = jnp.repeat(jnp.arange(T, dtype=jnp.int32), TOP_K)
    n_assign = T * TOP_K
    counts = jnp.zeros((N_EXPERTS,), jnp.int32).at[expert_id].add(1)
    padded = (counts + MOE_BLOCK - 1) // MOE_BLOCK * MOE_BLOCK
    pad_end = jnp.cumsum(padded)
    pad_start = pad_end - padded
    cnt_start = jnp.cumsum(counts) - counts
    order = jnp.argsort(expert_id)
    e_sorted = expert_id[order]
    dest = pad_start[e_sorted] + jnp.arange(n_assign, dtype=jnp.int32) - cnt_start[e_sorted]
    n_blocks = -(-(n_assign + N_EXPERTS * (MOE_BLOCK - 1)) // MOE_BLOCK)
    n_slots = n_blocks * MOE_BLOCK
    slot_tok = jnp.full((n_slots,), T, jnp.int32).at[dest].set(token_id[order])
    slot_gate = jnp.zeros((n_slots,), jnp.float32).at[dest].set(gate[order])
    block_start = jnp.arange(n_blocks, dtype=jnp.int32) * MOE_BLOCK
    block_e = jnp.minimum(jnp.searchsorted(pad_end, block_start, side='right'), N_EXPERTS - 1)
    u_pad = jnp.concatenate([u, jnp.zeros((1, D), u.dtype)], 0)

    def expert_block(args):
        tok, g, e = args
        xb = u_pad[tok]
        h = jax.nn.silu(xb @ w_gate[e]) * (xb @ w_up[e])
        return (h @ w_down[e]) * g[:, None].astype(u.dtype)

    y = lax.map(expert_block, (slot_tok.reshape(n_blocks, MOE_BLOCK), slot_gate.reshape(n_blocks, MOE_BLOCK), block_e))
    out = jnp.zeros((T + 1, D), y.dtype).at[slot_tok].add(y.reshape(n_slots, D))
    return out[:T]


def setup_inputs(seed: int = 0) -> dict:
    key = jax.random.key(seed)
    keys = list(jax.random.split(key, 40))

    def nrm(shape, std):
        return std * jax.random.normal(keys.pop(), shape, jnp.float32)

    D = D_MODEL
    bw = LRU_WIDTH // LRU_BLOCKS
    x = nrm((BATCH, SEQ, D), 1.0)
    c = nrm((BATCH, D), 1.0)
    ctx = nrm((BATCH, CTX_LEN, D), 1.0)
    c_ctx = nrm((D,), 1.0)
    w_mod = nrm((DEPTH, D, 6 * D), 0.5 * D ** -0.5)
    b_mod = nrm((DEPTH, 6 * D), 0.02)
    ln_g = 1.0 + nrm((DEPTH, 2, D), 0.02)
    ln_b = nrm((DEPTH, 2, D), 0.02)
    ev_w_in = nrm((N_EVEN, D, EVEN_IN), D ** -0.5)
    gla_w_a2 = nrm((N_EVEN, 2, GLA_GATE_RANK, GLA_QK), GLA_GATE_RANK ** -0.5)
    gla_b_a = nrm((N_EVEN, 2, GLA_QK), 0.1)
    gla_norm = 1.0 + nrm((N_EVEN, GLA_DV), 0.02)
    lru_conv_w = nrm((N_EVEN, CONV_W, LRU_WIDTH), CONV_W ** -0.5)
    lru_conv_b = nrm((N_EVEN, LRU_WIDTH), 0.02)
    lru_w_r = nrm((N_EVEN, 2, LRU_BLOCKS, bw, bw), bw ** -0.5)
    lru_b_r = nrm((N_EVEN, 2, LRU_WIDTH), 0.1)
    lru_w_i = nrm((N_EVEN, 2, LRU_BLOCKS, bw, bw), bw ** -0.5)
    lru_b_i = nrm((N_EVEN, 2, LRU_WIDTH), 0.1)
    a_pow = jax.random.uniform(keys.pop(), (N_EVEN, 2, LRU_WIDTH), jnp.float32, 0.9, 0.999) ** (1.0 / LRU_C)
    lru_lam = jnp.log(a_pow) - jnp.log1p(-a_pow)
    ev_w_out = nrm((N_EVEN, EVEN_MIX, D), DN_BETA * EVEN_MIX ** -0.5)
    od_w_in = nrm((N_ODD, D, ODD_IN), D ** -0.5)
    f_bias = jnp.linspace(3.0, 6.0, MLSTM_HEADS, dtype=jnp.float32)
    zeros_h = jnp.zeros((MLSTM_HEADS,), jnp.float32)
    mlstm_b_gate = jnp.stack([zeros_h, f_bias, zeros_h, f_bias])[None] + nrm((N_ODD, 4, MLSTM_HEADS), 0.1)
    mlstm_norm = 1.0 + nrm((N_ODD, MLSTM_DV), 0.02)
    od_w_out = nrm((N_ODD, ML_V, D), DN_BETA * ML_V ** -0.5)
    moe_w_group = nrm((DEPTH, D, N_GROUPS), D ** -0.5)
    moe_b_group = nrm((DEPTH, N_GROUPS), 0.01)
    moe_w_expert = nrm((DEPTH, D, N_EXPERTS), D ** -0.5)
    moe_b_expert = nrm((DEPTH, N_EXPERTS), 0.01)
    moe_w_gate = nrm((DEPTH, N_EXPERTS, D, D_EXPERT), D ** -0.5)
    moe_w_up = nrm((DEPTH, N_EXPERTS, D, D_EXPERT), D ** -0.5)
    moe_w_down = nrm((DEPTH, N_EXPERTS, D_EXPERT, D), DN_BETA * D_EXPERT ** -0.5)
    return {'x': x, 'c': c, 'ctx': ctx, 'c_ctx': c_ctx, 'w_mod': w_mod, 'b_mod': b_mod,
            'ln_g': ln_g, 'ln_b': ln_b, 'ev_w_in': ev_w_in, 'gla_w_a2': gla_w_a2, 'gla_b_a': gla_b_a,
            'gla_norm': gla_norm, 'lru_conv_w': lru_conv_w, 'lru_conv_b': lru_conv_b,
            'lru_w_r': lru_w_r, 'lru_b_r': lru_b_r, 'lru_w_i': lru_w_i, 'lru_b_i': lru_b_i,
            'lru_lam': lru_lam, 'ev_w_out': ev_w_out, 'od_w_in': od_w_in, 'mlstm_b_gate': mlstm_b_gate,
            'mlstm_norm': mlstm_norm, 'od_w_out': od_w_out, 'moe_w_group': moe_w_group,
            'moe_b_group': moe_b_group, 'moe_w_expert': moe_w_expert, 'moe_b_expert': moe_b_expert,
            'moe_w_gate': moe_w_gate, 'moe_w_up': moe_w_up, 'moe_w_down': moe_w_down}


def reference(x, c, ctx, c_ctx, w_mod, b_mod, ln_g, ln_b, ev_w_in, gla_w_a2, gla_b_a, gla_norm,
              lru_conv_w, lru_conv_b, lru_w_r, lru_b_r, lru_w_i, lru_b_i, lru_lam, ev_w_out,
              od_w_in, mlstm_b_gate, mlstm_norm, od_w_out, moe_w_group, moe_b_group,
              moe_w_expert, moe_b_expert, moe_w_gate, moe_w_up, moe_w_down):
    B, S, D = x.shape
    h_ctx = ctx
    for l in range(DEPTH):
        last = l == DEPTH - 1
        j = l // 2
        m_lat = cond_modulation(c, w_mod[l], b_mod[l])
        m_ctx = cond_modulation(c_ctx[None], w_mod[l], b_mod[l])
        u_lat = modulate(x, m_lat, 0)
        u_ctx = modulate(h_ctx, m_ctx, 0)
        if l % 2 == 0:
            y_lat, y_ctx = even_mixer(u_lat, u_ctx, ev_w_in[j], gla_w_a2[j], gla_b_a[j], gla_norm[j],
                                      lru_conv_w[j], lru_conv_b[j], lru_w_r[j], lru_b_r[j],
                                      lru_w_i[j], lru_b_i[j], lru_lam[j], ev_w_out[j], not last)
        else:
            y_lat, y_ctx = odd_mixer(u_lat, u_ctx, od_w_in[j], mlstm_b_gate[j], mlstm_norm[j],
                                     od_w_out[j], not last)
        x = layer_norm(DN_ALPHA * x + m_lat[:, 2, None] * y_lat, ln_g[l, 0], ln_b[l, 0])
        moe_args = (moe_w_group[l], moe_b_group[l], moe_w_expert[l], moe_b_expert[l],
                    moe_w_gate[l], moe_w_up[l], moe_w_down[l])
        if last:
            y_lat = hier_moe(modulate(x, m_lat, 3).reshape(B * S, D), *moe_args).reshape(B, S, D)
        else:
            h_ctx = layer_norm(DN_ALPHA * h_ctx + m_ctx[:, 2, None] * y_ctx, ln_g[l, 0], ln_b[l, 0])
            tokens = jnp.concatenate([modulate(x, m_lat, 3).reshape(B * S, D),
                                      modulate(h_ctx, m_ctx, 3).reshape(-1, D)], 0)
            y_all = hier_moe(tokens, *moe_args)
            y_lat = y_all[:B * S].reshape(B, S, D)
            y_ctx = y_all[B * S:].reshape(h_ctx.shape)
            h_ctx = layer_norm(DN_ALPHA * h_ctx + m_ctx[:, 5, None] * y_ctx, ln_g[l, 1], ln_b[l, 1])
        x = layer_norm(DN_ALPHA * x + m_lat[:, 5, None] * y_lat, ln_g[l, 1], ln_b[l, 1])
    return x
```

```python
import numpy as np
from contextlib import ExitStack
import ml_dtypes
import concourse.bass as bass
import concourse.mybir as mybir
from concourse.bass_utils import run_bass_kernel_spmd

F32 = mybir.dt.float32
BF16 = mybir.dt.bfloat16
AF = mybir.ActivationFunctionType
ALU = mybir.AluOpType
AX = mybir.AxisListType

D = 2048
ALPHA = 4.0 ** 0.25
EPS = 1e-5
NEG = -1e30


class Buf:
    __slots__ = ("w", "r")

    def __init__(self):
        self.w = None
        self.r = {}


class PBuf(Buf):
    __slots__ = ()
    excl = True


class Prog:
    CE = ("pe", "act", "dve", "pool")
    SEM_ROT = 16000

    def __init__(self, n_dma_sems=12):
        self.nc = bass.Bass("TRN2", target_bir_lowering=False)
        self.es = ExitStack()
        self.pes = None
        self.ops = {e: [] for e in ("pe", "act", "dve", "pool", "sp")}
        self.cnt = {e: 0 for e in self.CE}
        self.sems = []
        self.cur_sem = {}
        self.seen = {e: {} for e in self.ops}
        for e in self.CE:
            self.cur_sem[e] = self._new_sem("c_" + e)
        self.dq = {}
        for q in ("sp", "pool", "act"):
            self.dq[q] = dict(sems=[self._new_sem("d_%s" % q) for i in range(n_dma_sems)], i=0)
        self.n_t = 0

    def _new_sem(self, name):
        h = self.es.enter_context(self.nc.semaphore(name + "_%d" % len(self.sems)))
        self.sems.append(h)
        return len(self.sems) - 1

    def dram(self, name, shape, dt, kind="Internal"):
        return self.nc.dram_tensor(name, list(shape), dt, kind=kind).ap()

    def begin(self):
        self.pes = ExitStack()

    def sb(self, shape, dt=F32):
        self.n_t += 1
        st = self.pes if self.pes is not None else self.es
        return st.enter_context(self.nc.sbuf_tensor("t%d" % self.n_t, list(shape), dt))

    def ps(self, shape, dt=F32):
        self.n_t += 1
        st = self.pes if self.pes is not None else self.es
        return st.enter_context(self.nc.psum_tensor("p%d" % self.n_t, list(shape), dt))

    def _waits(self, eng, reads, writes, extra=()):
        waits = {}

        def need(ev):
            if ev is None:
                return
            s, v, src = ev
            if src == "pe" and eng == "pe":
                return
            if self.seen[eng].get(s, 0) >= v:
                return
            if waits.get(s, 0) < v:
                waits[s] = v
        for b in reads:
            need(b.w)
            if getattr(b, "excl", False):
                for ev in b.r.values():
                    if ev[2] != eng:
                        need(ev)
        for b in writes:
            need(b.w)
            for ev in b.r.values():
                need(ev)
        for ev in extra:
            need(ev)
        for s, v in waits.items():
            self.seen[eng][s] = v
            self.ops[eng].append(lambda e, s=s, v=v: e.wait_ge(self.sems[s], v))

    def _post(self, ev, reads, writes):
        key = ev[2] if not ev[2].startswith("dma") else ("d", ev[0])
        for b in reads:
            b.r[key] = ev
        for b in writes:
            b.w = ev
            b.r = {}

    def op(self, eng, f, reads=(), writes=()):
        self._waits(eng, reads, writes)
        if self.cnt[eng] >= self.SEM_ROT:
            self.cur_sem[eng] = self._new_sem("c_" + eng)
            self.cnt[eng] = 0
        self.cnt[eng] += 1
        s = self.cur_sem[eng]
        ev = (s, self.cnt[eng], eng)
        self.ops[eng].append(lambda e, s=s: f(e).then_inc(self.sems[s], 1))
        self._post(ev, reads, writes)
        return ev

    def dma(self, q, out, in_, reads=(), writes=(), **kw):
        d = self.dq[q]
        K = len(d["sems"])
        k = d["i"] % K
        j = d["i"] // K
        d["i"] += 1
        s = d["sems"][k]
        extra = [(s, 16 * j, "dma" + q)] if j > 0 else []
        self._waits(q, reads, writes, extra)
        ev = (s, 16 * (j + 1), "dma" + q)
        self.ops[q].append(lambda e, s=s: e.dma_start(out=out, in_=in_, **kw).then_inc(self.sems[s], 16))
        self._post(ev, reads, writes)
        return ev

    def dma_f(self, q, f, reads=(), writes=()):
        d = self.dq[q]
        K = len(d["sems"])
        k = d["i"] % K
        j = d["i"] // K
        d["i"] += 1
        s = d["sems"][k]
        extra = [(s, 16 * j, "dma" + q)] if j > 0 else []
        self._waits(q, reads, writes, extra)
        ev = (s, 16 * (j + 1), "dma" + q)
        self.ops[q].append(lambda e, s=s: f(e).then_inc(self.sems[s], 16))
        self._post(ev, reads, writes)
        return ev

    def coll(self, src, dst, reads=(), writes=(), groups=((0, 1, 2, 3), (4, 5, 6, 7))):
        if not hasattr(self, "cc_sem"):
            self.cc_sem = self._new_sem("cc")
            self.cc_n = 0
        s = self.cc_sem
        self.cc_n += 1
        self._waits("pool", reads, writes)
        rg = [list(g) for g in groups]
        self.ops["pool"].append(lambda e: e.collective_compute("AllGather", ALU.bypass, replica_groups=rg,
                                                               ins=[src.opt()], outs=[dst.opt()]).then_inc(self.sems[s]))
        ev = (s, self.cc_n, "dmacc")
        self._post(ev, reads, writes)
        self.cc_events = getattr(self, "cc_events", []) + [ev]
        return ev

    def flush(self, final=False):
        if final:
            finals = {}
            for q, d in self.dq.items():
                K = len(d["sems"])
                for k, s in enumerate(d["sems"]):
                    n = (d["i"] - k + K - 1) // K if d["i"] > k else 0
                    if n > 0:
                        finals[s] = 16 * n
            for e in self.CE:
                if self.cnt[e] > 0:
                    finals[self.cur_sem[e]] = self.cnt[e]
            for ev in getattr(self, "cc_events", []):
                finals[ev[0]] = ev[1]
            for s, v in finals.items():
                if self.seen["sp"].get(s, 0) < v:
                    self.seen["sp"][s] = v
                    self.ops["sp"].append(lambda e, s=s, v=v: e.wait_ge(self.sems[s], v))
        ops = self.ops
        with self.nc.Block() as block:
            @block.tensor
            def _(e):
                for f in ops["pe"]:
                    f(e)

            @block.scalar
            def _(e):
                for f in ops["act"]:
                    f(e)

            @block.vector
            def _(e):
                for f in ops["dve"]:
                    f(e)

            @block.gpsimd
            def _(e):
                for f in ops["pool"]:
                    f(e)

            @block.sync
            def _(e):
                for f in ops["sp"]:
                    f(e)
        self.ops = {e: [] for e in ops}
        if self.pes is not None:
            self.pes.close()
            self.pes = None
        if final:
            self.es.close()

    def mm(self, out, lhsT, rhs, start, stop, reads, writes):
        return self.op("pe", lambda e: e.matmul(out, lhsT, rhs, start=start, stop=stop), reads, writes)

    def tr(self, out, in_, ident, reads, writes):
        return self.op("pe", lambda e: e.transpose(out, in_, ident), reads, writes)

    def act(self, out, in_, func, reads, writes, bias=None, scale=None):
        kw = {}
        if bias is not None:
            kw["bias"] = bias
        if scale is not None:
            kw["scale"] = scale
        return self.op("act", lambda e: e.activation(out, in_, func, **kw), reads, writes)

    def tt(self, out, a, b, op, reads, writes, eng="dve"):
        return self.op(eng, lambda e: e.tensor_tensor(out, a, b, op), reads, writes)

    def ts(self, out, a, s1, s2, op0, op1, reads, writes):
        if s2 is None:
            return self.op("dve", lambda e: e.tensor_scalar(out, a, s1, None, op0), reads, writes)
        return self.op("dve", lambda e: e.tensor_scalar(out, a, s1, s2, op0, op1), reads, writes)

    def stt(self, out, a, s, b, op0, op1, reads, writes):
        return self.op("dve", lambda e: e.scalar_tensor_tensor(out, a, s, b, op0, op1), reads, writes)

    def cp(self, eng, out, in_, reads, writes):
        if eng == "act":
            return self.op("act", lambda e: e.copy(out, in_), reads, writes)
        return self.op(eng, lambda e: e.tensor_copy(out, in_), reads, writes)


def emit_mod_rows(P, ccols, wmod, bmod, ncols, mbc, add_one_groups, ident=None, mcol=None, bmcol=None, col_groups=None):
    P.begin()
    nco = len(ccols)
    col_groups = col_groups or {}
    if col_groups:
        idt = P.sb([128, 128]); bid = Buf()
        P.dma("sp", idt[:], ident, [], [bid])
        ptc = P.ps([128, 512]); bptc = PBuf()
    ones = P.sb([128, 128]); b_ones = Buf()
    P.op("dve", lambda e: e.memset(ones[:], 1.0), [], [b_ones])
    cbl = []
    for ci, cc in enumerate(ccols):
        cs = P.sb([128, 16]); bcs = Buf()
        P.dma("sp", cs[:], cc, [], [bcs])
        sc = P.sb([128, 16]); bsc = Buf()
        P.act(sc[:], cs[:], AF.Silu, [bcs], [bsc])
        cb = P.sb([128, 16, 128]); bcb = Buf()
        for k in range(16):
            P.ts(cb[:, k, :], ones[:], sc[:, k:k + 1], None, ALU.mult, None, [b_ones, bsc], [bcb])
        cbl.append((cb, bcb))
    bm = P.sb([1, ncols]); bbm = Buf()
    P.dma("sp", bm[:], bmod, [], [bbm])
    wts = [(P.sb([128, 8, 512]), Buf()) for _ in range(2)]
    pss = [(P.ps([128, 512]), PBuf()) for _ in range(2)]
    outs = [(P.sb([128, 512]), Buf()) for _ in range(2)]
    wv = wmod.rearrange("(k p) c -> p k c", p=128)
    n = 0
    for g in range(ncols // 512):
        cs_ = slice(g * 512, (g + 1) * 512)
        for half in range(2):
            wt, bwt = wts[half]
            P.dma("sp", wt[:], wv[:, half * 8:(half + 1) * 8, cs_], [], [bwt])
        for ci in range(nco):
            cb, bcb = cbl[ci]
            pp, bpp = pss[n % 2]
            ot, bot = outs[n % 2]
            n += 1
            P.mm(pp[:], ones[0:1, :], bm[0:1, cs_], True, False, [b_ones, bbm], [bpp])
            for k in range(16):
                wt, bwt = wts[k // 8]
                P.mm(pp[:], cb[:, k, :], wt[:, k % 8, :], False, k == 15, [bcb, bwt], [bpp])
            if g in add_one_groups:
                P.ts(ot[:], pp[:], 1.0, None, ALU.add, None, [bpp], [bot])
            else:
                P.cp("act", ot[:], pp[:], [bpp], [bot])
            if mbc is not None:
                P.dma("pool", mbc[ci, :, cs_], ot[:], [bot], [])
            if g in col_groups:
                for j in range(4):
                    P.tr(ptc[:, j * 128:(j + 1) * 128], ot[:, j * 128:(j + 1) * 128], idt[:], [bot, bid], [bptc])
                sl0 = col_groups[g] * 4
                P.cp("dve", mcol[:, ci, sl0:sl0 + 4], ptc[:].rearrange("p (a b) -> p a b", a=4)[:, :, 0], [bptc], [bmcol])
    P.flush()


def emit_ln(P, z, bz, out, bout, g_bc, b_bc, bconst, tmp):
    st, bst, mv, bmv = tmp
    for c4 in range(4):
        P.op("dve", lambda e, c4=c4: e.bn_stats(st[:, c4, :], z[:, c4 * 512:(c4 + 1) * 512]), [bz], [bst])
    P.op("dve", lambda e: e.bn_aggr(mv[:, 0:2], st[:]), [bst], [bmv])
    P.ts(mv[:, 2:3], mv[:, 1:2], EPS, None, ALU.add, None, [bmv], [bmv])
    P.op("act", lambda e: e.sqrt(mv[:, 2:3], mv[:, 2:3]), [bmv], [bmv])
    P.op("dve", lambda e: e.reciprocal(mv[:, 2:3], mv[:, 2:3]), [bmv], [bmv])
    P.ts(out[:], z[:], mv[:, 0:1], mv[:, 2:3], ALU.subtract, ALU.mult, [bz, bmv], [bout])
    P.tt(out[:], out[:], g_bc[:], ALU.mult, [bout, bconst], [bout])
    P.tt(out[:], out[:], b_bc[:], ALU.add, [bout, bconst], [bout])


def build_tokloc(NT, ctx_tile, stop=None, sub=99):
    P = Prog()
    emit_tokloc(P, "", NT, ctx_tile)
    P.begin()
    P.flush(final=True)
    return P.nc


def emit_tokloc(P, pre, NT, ctx_tile, mix=None, xres=None, xout=None, stop=None, sub=99, NE=32, wsrc=None, out_cm=None, xres_cm=None):
    NP = NT * 128
    I = lambda n, s, d=F32: P.dram(pre + n, s, d, kind="ExternalInput")
    mixT = I("mixT", [D, NP], BF16) if mix is None else None
    if xres is None:
        xres = I("xres", [NP, D])
    ccol = I("ccol", [128, 16])
    cccol = I("cccol", [128, 16]) if ctx_tile else None
    wmod = I("wmod", [D, 8192]); bmod = I("bmod", [1, 8192])
    lnp = I("lnp", [4, 128, D])
    wout = I("wout", [D, D])
    wr = I("wr", [D, 36]); brb = I("brb", [128, 36])
    if wsrc is None:
        wg = I("wg", [NE, D, 512]); wu = I("wu", [NE, D, 512]); wd = I("wd", [NE, 512, D])
        b_w = Buf()
    else:
        wg, wu, wd, b_w = wsrc
    ident = I("ident", [128, 128])
    if xout is None:
        xout = P.dram(pre + "xout", [NP, D], F32, kind="ExternalOutput")
    mbc = P.dram(pre + "mbc", [2, 128, 8192], F32)
    x1s = P.dram(pre + "x1s", [NP, D], F32)
    u2Ts = P.dram(pre + "u2Ts", [D, NP], BF16)
    Gs = P.dram(pre + "Gs", [NP, 32], F32)
    ys = P.dram(pre + "ys", [NP, D], F32)
    b_mbc = Buf(); b_x1s = Buf(); b_u2Ts = Buf(); b_Gs = Buf(); b_ys = Buf(); b_wbf = Buf()

    mcol = P.sb([128, 2, 32]); bmcol = Buf()
    cg_ = {4: 0, 5: 1, 6: 2, 7: 3, 8: 4, 9: 5, 10: 6, 11: 7}
    emit_mod_rows(P, [ccol[:, :], cccol[:, :]] if ctx_tile else [ccol[:, :]], wmod, bmod[:, :], 8192, mbc, (8, 9, 10, 11),
                  ident=ident[:, :], mcol=mcol, bmcol=bmcol, col_groups=cg_)
    d = P.dq["pool"]
    K = len(d["sems"])

    def drain_queue_events(q):
        d = P.dq[q]
        K = len(d["sems"])
        evs = []
        for k, s in enumerate(d["sems"]):
            n = (d["i"] - k + K - 1) // K if d["i"] > k else 0
            if n > 0:
                evs.append((s, 16 * n, "dma" + q))
        return evs

    def barrier_dram(bufs):
        evs = drain_queue_events("pool") + drain_queue_events("sp") + drain_queue_events("act")
        for q in ("sp", "pool", "act", "dve", "pe"):
            P._waits(q, [], [], evs)

    barrier_dram([b_mbc])

    P.begin()
    idt = P.sb([128, 128]); bid = Buf()
    P.dma("sp", idt[:], ident[:, :], [], [bid])
    woutb = P.sb([128, 16, D], BF16); bwo = Buf()
    stg = [(P.sb([128, D]), Buf()) for _ in range(2)]
    wov = wout.rearrange("(k p) c -> p k c", p=128)
    for j in range(16):
        s_, bs_ = stg[j % 2]
        P.dma("sp", s_[:], wov[:, j, :], [], [bs_])
        P.cp("act" if j % 2 else "dve", woutb[:, j, :], s_[:], [bs_], [bwo])
    wrs = P.sb([128, 16, 36]); bwr = Buf()
    P.dma("sp", wrs[:], wr.rearrange("(k p) c -> p k c", p=128), [], [bwr])
    brs = P.sb([128, 36]); bbr = Buf()
    P.dma("sp", brs[:], brb[:, :], [], [bbr])
    cA = P.sb([128, 3, D]); bcA = Buf()

    def load_consts_A(ci):
        P.dma("sp", cA[:, 0, :], mbc[ci, :, 0:2048], [], [bcA])
    load_consts_A(0)
    P.dma("sp", cA[:, 1, :], lnp[0], [], [bcA])
    P.dma("sp", cA[:, 2, :], lnp[1], [], [bcA])
    mts = [(P.sb([128, 16, 128], BF16), Buf()) for _ in range(2)]
    xts = [(P.sb([128, D]), Buf()) for _ in range(2)]
    zs = [(P.sb([128, D]), Buf()) for _ in range(2)]
    u2f = [(P.sb([128, 16, 128]), Buf()) for _ in range(1)]
    u2b = [(P.sb([128, 16, 128], BF16), Buf()) for _ in range(2)]
    py = [(P.ps([128, 512]), PBuf()) for _ in range(4)]
    ptr = [(P.ps([128, 512]), PBuf()) for _ in range(2 if mix is not None else 3)]
    prr = (P.ps([128, 64]), PBuf())
    if mix is not None:
        idb = P.sb([128, 128], BF16)
        P.cp("dve", idb[:], idt[:], [bid], [bid])
        mtms = [(P.sb([128, 4, 512], BF16), Buf()) for _ in range(2)]
        for m_, bm_ in mtms:
            P.op("dve", lambda e, m_=m_: e.memset(m_[:], 0.0), [], [bm_])
        pmt = (P.ps([128, 1024], BF16), PBuf())
        CR_ = mix["CR"]; S_ = mix["S"]; SQ_ = mix["SQ"]
        qcache = {}
        nq_ = SQ_ // CR_
        mixmine = P.dram(pre + "mixmine", [NP, 4, 512], BF16)
        bmm = [Buf() for _ in range(nq_ + 1)]
        for cb_ in range(nq_):
            def dynb(e, cb_=cb_):
                if "qoff" not in qcache:
                    qcache["q"] = e.partition_id() % 4
                    qcache["qoff"] = qcache["q"] * nq_
                return e.dma_start(out=mixmine[cb_ * CR_:(cb_ + 1) * CR_, :, :],
                                   in_=Gfull[cb_:cb_ + 3 * nq_ + 1][bass.ds(qcache["qoff"], 1), :, :, :].rearrange("c w r f -> (c w) r f"))
            P.dma_f("sp", dynb, [mix["bG"]], [bmm[cb_]])
        if ctx_tile:
            def dync(e):
                if "qoff" not in qcache:
                    qcache["q"] = e.partition_id() % 4
                    qcache["qoff"] = qcache["q"] * nq_
                return e.dma_start(out=mixmine[SQ_:SQ_ + mix["CQ"], :, :], in_=Glast[bass.ds(qcache["q"] * mix["CQ"], mix["CQ"]), :, :])
            P.dma_f("sp", dync, [mix["bG"]], [bmm[-1]])
        Gfull = mix["G"][0:4 * S_, :].rearrange("(c r w) f -> c w r f", r=4, w=CR_)
        if ctx_tile:
            Glast = mix["G"][4 * S_:4 * S_ + 4 * mix["NCTX"], :].rearrange("(r w) f -> w r f", r=4)
    lnt = (P.sb([128, 4, 6]), Buf(), P.sb([128, 4]), Buf())
    rt = P.sb([128, 128]); brt = Buf()
    lg = P.sb([128, 36]); blg = Buf()
    Gt = [(P.sb([128, 32]), Buf()) for _ in range(2)]
    mixv = mixT.rearrange("(k p) t -> p k t", p=128) if mix is None else None
    u2Tv = u2Ts.rearrange("(k p) t -> p k t", p=128)
    for t in range(NT):
        if ctx_tile and t == NT - 1:
            load_consts_A(1)
        tsl = slice(t * 128, (t + 1) * 128)
        mt, bmt = mts[t % 2]; xt, bxt = xts[t % 2]; z, bz = zs[t % 2]
        uf, buf_ = u2f[0]; ub, bub = u2b[t % 2]; G, bG = Gt[t % 2]
        ci = 1 if (ctx_tile and t == NT - 1) else 0
        if mix is None:
            P.dma("sp", mt[:], mixv[:, :, tsl], [], [bmt])
        else:
            mtm, bmtm = mtms[t % 2]
            if ctx_tile and t == NT - 1:
                P.dma("sp", mtm[0:mix["CQ"]], mixmine[SQ_:SQ_ + mix["CQ"], :, :], [bmm[-1]], [bmtm])
            else:
                P.dma("sp", mtm[:], mixmine[t * 128:(t + 1) * 128, :, :], [bmm[(t * 128) // CR_]], [bmtm])
            for rd in range(2):
                for k8 in range(8):
                    k = rd * 8 + k8
                    r_, kk = divmod(k, 4)
                    P.tr(pmt[0][:, k8 * 128:(k8 + 1) * 128], mtm[:, r_, kk * 128:(kk + 1) * 128], idb[:], [bmtm, bid], [pmt[1]])
                P.cp("act" if rd else "dve", mt[:, rd * 8:(rd + 1) * 8, :], pmt[0][:].rearrange("p (a b) -> p a b", b=128), [pmt[1]], [bmt])
        if xres_cm is None:
            P.dma("sp", xt[:], xres[tsl, :], [], [bxt])
        else:
            xv_ = xres[0:xres_cm * 64, :].rearrange("(c kk) d -> kk c d", kk=xres_cm)
            for hf_ in range(2):
                P.dma("sp", xt[hf_ * 64:(hf_ + 1) * 64, :], xv_[2 * t + hf_], [], [bxt])
        for cg in range(4):
            pp, bpp = py[cg]
            for k in range(16):
                P.mm(pp[:], mt[:, k, :], woutb[:, k, cg * 512:(cg + 1) * 512], k == 0, k == 15, [bmt, bwo], [bpp])
            P.tt(z[:, cg * 512:(cg + 1) * 512], pp[:], cA[:, 0, cg * 512:(cg + 1) * 512], ALU.mult, [bpp, bcA], [bz])
        P.stt(xt[:], xt[:], ALPHA, z[:], ALU.mult, ALU.add, [bxt, bz], [bxt])
        if sub <= 1:
            continue
        x1, bx1 = z, bz
        emit_ln(P, xt, bxt, x1, bx1, cA[:, 1, :], cA[:, 2, :], bcA, lnt)
        P.dma("pool", x1s[tsl, :], x1[:], [bx1], [b_x1s])
        if sub <= 2:
            continue
        for k4 in range(4):
            pp, bpp = ptr[k4 % len(ptr)]
            for kk in range(4):
                k = k4 * 4 + kk
                P.tr(pp[:, kk * 128:(kk + 1) * 128], x1[:, k * 128:(k + 1) * 128], idt[:], [bx1, bid], [bpp])
            for kk in range(4):
                k = k4 * 4 + kk
                P.act(uf[:, k, :], pp[:, kk * 128:(kk + 1) * 128], AF.Identity, [bpp, bmcol], [buf_],
                      bias=mcol[:, ci, k:k + 1], scale=mcol[:, ci, 16 + k:17 + k])
            P.cp("dve", ub[:, k4 * 4:(k4 + 1) * 4, :], uf[:, k4 * 4:(k4 + 1) * 4, :], [buf_], [bub])
        if sub <= 3:
            continue
        P.dma("act", u2Tv[:, :, tsl], ub[:], [bub], [b_u2Ts])
        if sub <= 4:
            continue
        pr, bpr = prr
        for k in range(16):
            P.mm(pr[:, 0:36], uf[:, k, :], wrs[:, k, :], k == 0, k == 15, [buf_, bwr], [bpr])
        P.tt(lg[:], pr[:, 0:36], brs[:], ALU.add, [bpr, bbr], [blg])
        if sub <= 5:
            continue
        R = [brt, blg]
        c = lambda i: rt[:, i:i + 1]
        gl = lg[:, 0:4]
        P.op("dve", lambda e: e.reduce_max(c(0), gl, AX.X), [blg], [brt])
        P.ts(rt[:, 8:12], gl, c(0), None, ALU.is_ge, None, R, [brt])
        P.ts(c(1), c(0), -1.0, None, ALU.mult, None, R, [brt])
        P.act(rt[:, 12:16], gl, AF.Exp, R, [brt], bias=c(1))
        P.op("dve", lambda e: e.reduce_sum(c(2), rt[:, 12:16], AX.X), R, [brt])
        P.op("dve", lambda e: e.reciprocal(c(2), c(2)), R, [brt])
        els = rt[:, 16:24]
        P.ts(els, lg[:, 4:12], c(8), None, ALU.mult, None, R, [brt])
        for g in range(1, 4):
            P.stt(els, lg[:, 4 + 8 * g:12 + 8 * g], c(8 + g), els, ALU.mult, ALU.add, R, [brt])
        P.op("dve", lambda e: e.reduce_max(c(3), els, AX.X), R, [brt])
        mk1 = rt[:, 24:32]
        P.ts(mk1, els, c(3), None, ALU.is_ge, None, R, [brt])
        els2 = rt[:, 32:40]
        P.stt(els2, mk1, NEG, els, ALU.mult, ALU.add, R, [brt])
        P.op("dve", lambda e: e.reduce_max(c(4), els2, AX.X), R, [brt])
        mk2 = rt[:, 40:48]
        P.ts(mk2, els2, c(4), None, ALU.is_ge, None, R, [brt])
        P.tt(c(5), c(4), c(3), ALU.subtract, R, [brt])
        P.act(c(5), c(5), AF.Exp, R, [brt])
        P.ts(c(6), c(5), 1.0, None, ALU.add, None, R, [brt])
        P.op("dve", lambda e: e.reciprocal(c(6), c(6)), R, [brt])
        P.tt(c(7), c(5), c(6), ALU.mult, R, [brt])
        P.tt(c(6), c(6), c(2), ALU.mult, R, [brt])
        P.tt(c(7), c(7), c(2), ALU.mult, R, [brt])
        gsel = rt[:, 48:56]
        P.ts(gsel, mk1, c(6), None, ALU.mult, None, R, [brt])
        P.stt(gsel, mk2, c(7), gsel, ALU.mult, ALU.add, R, [brt])
        for g in range(4):
            P.ts(G[:, 8 * g:8 * g + 8], gsel, c(8 + g), None, ALU.mult, None, R, [bG])
        P.dma("pool", Gs[tsl, :], G[:], [bG], [b_Gs])
    P.flush()
    barrier_dram([])

    P.begin()
    STT = 6
    n_st = (NT + STT - 1) // STT
    u2T = P.sb([128, 16, STT * 128], BF16); bu2T = Buf()
    Gst = P.sb([128, STT, 32]); bGst = Buf()
    acc = [(P.sb([128, D]), Buf()) for _ in range(STT)]
    slots = [(P.sb([128, 8192], BF16), Buf()) for _ in range(4)]
    stg = [(P.sb([128, 2048]), Buf()) for _ in range(2)]
    hT = [(P.sb([128, 4, 512], BF16), Buf()) for _ in range(2)]
    sil = [(P.sb([128, 512]), Buf()) for _ in range(2)]
    pg = [(P.ps([128, 512]), PBuf()) for _ in range(2)]
    pu = [(P.ps([128, 512]), PBuf()) for _ in range(2)]
    pd = [(P.ps([128, 512]), PBuf()) for _ in range(4)]
    wsrc = [wg, wu, wd]
    nslot = 0
    nstg = 0
    nh = 0
    for st in range(n_st):
        t0 = st * STT
        nt = min(STT, NT - t0)
        ntok = nt * 128
        P.dma("sp", u2T[:, :, 0:ntok], u2Tv[:, :, t0 * 128:t0 * 128 + ntok], [b_u2Ts], [bu2T])
        P.dma("sp", Gst[:, 0:nt, :], Gs[t0 * 128:t0 * 128 + ntok, :].rearrange("(t p) e -> p t e", p=128), [b_Gs], [bGst])
        for e_ in range(NE):
            mats = []
            for mi in range(3):
                sl, bsl = slots[nslot % 4]
                nslot += 1
                if mi < 2:
                    sv = sl[:].rearrange("p (k f) -> p k f", k=16)
                    srcv = wsrc[mi][e_].rearrange("(k p) f -> p k f", p=128)
                    pieces = [(sv[:, 4 * j:4 * j + 4, :], srcv[:, 4 * j:4 * j + 4, :], 4) for j in range(4)]
                else:
                    sv = sl[:].rearrange("p (k c) -> p k c", k=4)
                    srcv = wsrc[mi][e_].rearrange("(k p) c -> p k c", p=128)
                    pieces = [(sv[:, j:j + 1, :], srcv[:, j:j + 1, :], 1) for j in range(4)]
                for j, (dv, sv_, a) in enumerate(pieces):
                    sg_, bsg_ = stg[nstg % 2]
                    nstg += 1
                    P.dma("sp", sg_[:].rearrange("p (a b) -> p a b", a=a), sv_, [b_w], [bsg_])
                    P.cp("act" if (nstg % 2) else "dve", dv, sg_[:].rearrange("p (a b) -> p a b", a=a), [bsg_], [bsl])
                mats.append((sv, bsl))
            (Wg, bWg), (Wu, bWu), (Wd, bWd) = mats
            for tg in range(0, ntok, 512):
                n = min(512, ntok - tg)
                h, bh = hT[nh % 2]
                nh += 1
                for f in range(4):
                    g_, bg_ = pg[f % 2]; u_, bu_ = pu[f % 2]; s_, bs_ = sil[f % 2]
                    for k in range(16):
                        P.mm(g_[:, 0:n], Wg[:, k, f * 128:(f + 1) * 128], u2T[:, k, tg:tg + n], k == 0, k == 15, [bWg, bu2T], [bg_])
                    for k in range(16):
                        P.mm(u_[:, 0:n], Wu[:, k, f * 128:(f + 1) * 128], u2T[:, k, tg:tg + n], k == 0, k == 15, [bWu, bu2T], [bu_])
                    P.act(s_[:, 0:n], g_[:, 0:n], AF.Silu, [bg_], [bs_])
                    P.tt(h[:, f, 0:n], s_[:, 0:n], u_[:, 0:n], ALU.mult, [bs_, bu_], [bh])
                for ti in range(n // 128):
                    tl = (tg // 128) + ti
                    a_, ba_ = acc[tl]
                    for cg in range(4):
                        pp, bpp = pd[cg]
                        for f in range(4):
                            P.mm(pp[:], h[:, f, ti * 128:(ti + 1) * 128], Wd[:, f, cg * 512:(cg + 1) * 512], f == 0, f == 3, [bh, bWd], [bpp])
                        cs_ = slice(cg * 512, (cg + 1) * 512)
                        if e_ == 0:
                            P.ts(a_[:, cs_], pp[:], Gst[:, tl, 0:1], None, ALU.mult, None, [bpp, bGst], [ba_])
                        else:
                            P.stt(a_[:, cs_], pp[:], Gst[:, tl, e_:e_ + 1], a_[:, cs_], ALU.mult, ALU.add, [bpp, bGst, ba_], [ba_])
        for tl in range(nt):
            a_, ba_ = acc[tl]
            P.dma("pool", ys[(t0 + tl) * 128:(t0 + tl + 1) * 128, :], a_[:], [ba_], [b_ys])
    P.flush()
    barrier_dram([])

    P.begin()
    cC = P.sb([128, 3, D]); bcC = Buf()

    def load_consts_C(ci):
        P.dma("sp", cC[:, 0, :], mbc[ci, :, 6144:8192], [], [bcC])
        P.dma("sp", cC[:, 1, :], lnp[2], [], [bcC])
        P.dma("sp", cC[:, 2, :], lnp[3], [], [bcC])
    load_consts_C(0)
    xa = [(P.sb([128, D]), Buf()) for _ in range(2)]
    ya = [(P.sb([128, D]), Buf()) for _ in range(2)]
    oa = [(P.sb([128, D]), Buf()) for _ in range(2)]
    lnt = (P.sb([128, 4, 6]), Buf(), P.sb([128, 4]), Buf())
    for t in range(NT):
        if ctx_tile and t == NT - 1:
            load_consts_C(1)
        tsl = slice(t * 128, (t + 1) * 128)
        x1, bx1 = xa[t % 2]; y, by = ya[t % 2]; o, bo = oa[t % 2]
        P.dma("sp", x1[:], x1s[tsl, :], [b_x1s], [bx1])
        P.dma("sp", y[:], ys[tsl, :], [b_ys], [by])
        P.tt(y[:], y[:], cC[:, 0, :], ALU.mult, [by, bcC], [by])
        P.stt(y[:], x1[:], ALPHA, y[:], ALU.mult, ALU.add, [bx1, by], [by])
        emit_ln(P, y, by, o, bo, cC[:, 1, :], cC[:, 2, :], bcC, lnt)
        if out_cm is None or (ctx_tile and t == NT - 1):
            P.dma("pool", xout[tsl, :], o[:], [bo], [])
        else:
            ov_ = xout[0:out_cm * 64, :].rearrange("(c kk) d -> kk c d", kk=out_cm)
            for hf_ in range(2):
                P.dma("pool", ov_[2 * t + hf_], o[hf_ * 64:(hf_ + 1) * 64, :], [bo], [])
    P.flush()


def emit_projection(P, xin, T, n_ctx_tiles, win, ncols, mcol, bmcol, ident, tm_cols, tokmaj, fm_specs, load_x=None):
    P.begin()
    idt = P.sb([128, 128]); bid = Buf()
    P.dma("sp", idt[:], ident, [], [bid])
    Wb = P.sb([128, 16, ncols], BF16); bW = Buf()
    stg = [(P.sb([128, ncols]), Buf()) for _ in range(2)]
    wv = win.rearrange("(k p) c -> p k c", p=128)
    for k in range(16):
        s_, bs_ = stg[k % 2]
        P.dma("sp", s_[:], wv[:, k, :], [], [bs_])
        P.cp("act" if k % 2 else "dve", Wb[:, k, :], s_[:], [bs_], [bW])
    xts = [(P.sb([128, D]), Buf()) for _ in range(2)]
    uTs = [(P.sb([128, 16, 128], BF16), Buf()) for _ in range(2)]
    ptr = [(P.ps([128, 512]), PBuf()) for _ in range(2)]
    ptm = [(P.ps([128, 512]), PBuf()) for _ in range(3)]
    pfm = [(P.ps([128, 512]), PBuf()) for _ in range(2)]
    tms = [(P.sb([128, tm_cols]), Buf()) for _ in range(2)]
    fms = [(P.sb([128, 4, 128]), Buf()) for _ in range(2)]
    nfm = 0
    for t in range(T // 128):
        ci = 1 if t < n_ctx_tiles else 0
        tsl = slice(t * 128, (t + 1) * 128)
        xt, bxt = xts[t % 2]; uT, buT = uTs[t % 2]; tm, btm = tms[t % 2]
        if load_x is None:
            P.dma("sp", xt[:], xin[tsl, :], [], [bxt])
        else:
            load_x(t, xt, bxt)
        for k4 in range(4):
            pp, bpp = ptr[k4 % 2]
            for kk in range(4):
                k = k4 * 4 + kk
                P.tr(pp[:, kk * 128:(kk + 1) * 128], xt[:, k * 128:(k + 1) * 128], idt[:], [bxt, bid], [bpp])
            for kk in range(4):
                k = k4 * 4 + kk
                if k4 % 2 == 0:
                    P.act(uT[:, k, :], pp[:, kk * 128:(kk + 1) * 128], AF.Identity, [bpp, bmcol], [buT],
                          bias=mcol[:, ci, k:k + 1], scale=mcol[:, ci, 16 + k:17 + k])
                else:
                    P.ts(uT[:, k, :], pp[:, kk * 128:(kk + 1) * 128], mcol[:, ci, 16 + k:17 + k], mcol[:, ci, k:k + 1],
                         ALU.mult, ALU.add, [bpp, bmcol], [buT])
        nb = (tm_cols + 511) // 512
        for b_ in range(nb):
            c0 = b_ * 512
            w = min(512, tm_cols - c0)
            pp, bpp = ptm[b_ % 3]
            for k in range(16):
                P.mm(pp[:, 0:w], uT[:, k, :], Wb[:, k, c0:c0 + w], k == 0, k == 15, [buT, bW], [bpp])
            P.cp("act" if b_ % 2 else "dve", tm[:, c0:c0 + w], pp[:, 0:w], [bpp], [btm])
        P.dma("pool", tokmaj[tsl, :], tm[:], [btm], [])
        for j0 in range(0, len(fm_specs), 4):
            grp = fm_specs[j0:j0 + 4]
            pp, bpp = pfm[nfm % 2]; fm, bfm = fms[nfm % 2]
            nfm += 1
            for j, (c0, w, dst) in enumerate(grp):
                for k in range(16):
                    P.mm(pp[0:w, j * 128:(j + 1) * 128], Wb[:, k, c0:c0 + w], uT[:, k, :], k == 0, k == 15, [bW, buT], [bpp])
            P.cp("act" if nfm % 2 else "dve", fm[:, 0:len(grp), :], pp[:, 0:len(grp) * 128].rearrange("p (a b) -> p a b", b=128), [bpp], [bfm])
            for j, (c0, w, dst) in enumerate(grp):
                P.dma("act", dst[:, tsl], fm[0:w, j, :], [bfm], [])
    P.flush()


def all_dma_barrier(P):
    evs = []
    for q in ("sp", "pool", "act"):
        d = P.dq[q]
        K = len(d["sems"])
        for k, s in enumerate(d["sems"]):
            n = (d["i"] - k + K - 1) // K if d["i"] > k else 0
            if n > 0:
                evs.append((s, 16 * n, "dma" + q))
    for q in ("sp", "pool", "act", "dve", "pe"):
        P._waits(q, [], [], evs)


def emit_finish(P, T, nh, tokmaj, gate_c0, oF, oB, gn_bc, gate_func, ident, mixT, out_tm=None, t_start=0):
    P.begin()
    idt = P.sb([128, 128]); bid = Buf()
    P.dma("sp", idt[:], ident, [], [bid])
    gn = P.sb([128, 256]); bgn = Buf()
    P.dma("sp", gn[:], gn_bc, [], [bgn])
    A = [(P.sb([128, 256]), Buf()) for _ in range(2)]
    Bt = [(P.sb([128, 256]), Buf()) for _ in range(2)]
    Gt = [(P.sb([128, 256]), Buf()) for _ in range(2)]
    sq = P.sb([128, 256]); bsq = Buf()
    sc = P.sb([128, 4]); bsc = Buf()
    pt = [(P.ps([128, 512]), PBuf()) for _ in range(2)]
    ot = [(P.sb([128, 2, 128], BF16), Buf()) for _ in range(2)]
    n = 0
    otm = [(P.sb([128, 256], BF16), Buf()) for _ in range(2)]
    for t in range(t_start, T // 128):
        tsl = slice(t * 128, (t + 1) * 128)
        for h in range(nh):
            a, ba = A[n % 2]; b, bb = Bt[n % 2]; g, bg = Gt[n % 2]; pp, bpp = pt[n % 2]; o, bo = ot[n % 2]
            o2, bo2 = otm[n % 2]
            n += 1
            P.dma("sp", a[:], oF[h][tsl, :], [], [ba])
            P.dma("sp", b[:], oB[h][tsl, :], [], [bb])
            P.dma("sp", g[:], tokmaj[tsl, gate_c0 + h * 256:gate_c0 + (h + 1) * 256], [], [bg])
            P.tt(a[:], a[:], b[:], ALU.add, [ba, bb], [ba])
            P.tt(sq[:], a[:], a[:], ALU.mult, [ba], [bsq])
            P.op("dve", lambda e: e.reduce_sum(sc[:, 0:1], sq[:], AX.X), [bsq], [bsc])
            P.ts(sc[:, 0:1], sc[:, 0:1], 1.0 / 256, EPS, ALU.mult, ALU.add, [bsc], [bsc])
            P.op("act", lambda e: e.sqrt(sc[:, 0:1], sc[:, 0:1]), [bsc], [bsc])
            P.op("dve", lambda e: e.reciprocal(sc[:, 0:1], sc[:, 0:1]), [bsc], [bsc])
            P.stt(a[:], a[:], sc[:, 0:1], gn[:], ALU.mult, ALU.mult, [ba, bsc, bgn], [ba])
            P.act(g[:], g[:], gate_func, [bg], [bg])
            if out_tm is not None:
                P.tt(o2[:], a[:], g[:], ALU.mult, [ba, bg], [bo2])
                out_tm(t, h, o2, bo2)
                continue
            P.tt(a[:], a[:], g[:], ALU.mult, [ba, bg], [ba])
            for j in range(2):
                P.tr(pp[:, j * 128:(j + 1) * 128], a[:, j * 128:(j + 1) * 128], idt[:], [ba, bid], [bpp])
            P.cp("act", o[:], pp[:, 0:256].rearrange("p (a b) -> p a b", b=128), [bpp], [bo])
            for j in range(2):
                P.dma("act", mixT[h * 256 + j * 128:h * 256 + (j + 1) * 128, tsl], o[:, j, :], [bo], [])
    P.flush()


def build_evenmix(TL, NCTX=256):
    P = Prog()
    emit_evenmix(P, "", TL, NCTX)
    P.begin()
    P.flush(final=True)
    return P.nc


def emit_evenmix(P, pre, TL, NCTX=256, mixloc=None):
    T = NCTX + TL
    I = lambda n, s, d=F32: P.dram(pre + n, s, d, kind="ExternalInput")
    xin = I("xin", [T, D]); ccol = I("ccol", [128, 16]); cccol = I("cccol", [128, 16])
    wmod = I("wmod", [D, 4096]); bmod = I("bmod", [1, 4096])
    win = I("win", [D, 1312])
    wa2 = I("wa2", [2, 16, 128]); barow = I("barow", [2, 1, 128])
    gn_bc = I("gn_bc", [128, 256])
    convw = I("convw", [128, 2, 4]); convb = I("convb", [128, 2])
    wr_ = I("wr_", [2, 2, 128, 128]); wi_ = I("wi_", [2, 2, 128, 128])
    brc = I("brc", [128, 2, 2]); bic = I("bic", [128, 2, 2]); lamc = I("lamc", [128, 2, 2])
    ident = I("ident", [128, 128])
    cm = I("cm", [6, 64, 64])
    mixT = P.dram(pre + "mixT", [512, T], BF16, kind="ExternalOutput") if mixloc is None else None
    tokmaj = P.dram(pre + "tokmaj", [T, 640], F32)
    qT = P.dram(pre + "qT", [128, T], F32); kT = P.dram(pre + "kT", [128, T], F32)
    xrT = [P.dram(pre + "xrT%d" % i, [128, T], F32) for i in range(2)]
    xgT = [P.dram(pre + "xgT%d" % i, [128, T], F32) for i in range(2)]
    afT = P.dram(pre + "afT", [16, T], F32); abT = P.dram(pre + "abT", [16, T], F32)
    hF = [P.dram(pre + "hF%d" % i, [128, T], F32) for i in range(2)]
    oF = P.dram(pre + "oF", [T, 256], F32); oB = P.dram(pre + "oB", [T, 256], F32)

    mcol = P.sb([128, 2, 32]); bmcol = Buf()
    emit_mod_rows(P, [ccol[:, :], cccol[:, :]], wmod, bmod[:, :], 4096, None, (4, 5, 6, 7), ident=ident[:, :], mcol=mcol, bmcol=bmcol,
                  col_groups={g: g for g in range(8)})
    fm = [(640, 128, qT), (0, 128, kT), (768, 128, xrT[0]), (896, 128, xrT[1]), (1024, 128, xgT[0]), (1152, 128, xgT[1]),
          (1280, 16, afT), (1296, 16, abT)]
    emit_projection(P, xin, T, NCTX // 128, win, 1312, mcol, bmcol, ident[:, :], 640, tokmaj, fm)
    all_dma_barrier(P)

    P.begin()
    cw = P.sb([128, 2, 4]); cb = P.sb([128, 2]); bcw = Buf()
    P.dma("sp", cw[:], convw[:, :, :], [], [bcw]); P.dma("sp", cb[:], convb[:, :], [], [bcw])
    Wr = P.sb([128, 4, 128]); Wi = P.sb([128, 4, 128]); bWg = Buf()
    for d_ in range(2):
        for bl in range(2):
            P.dma("sp", Wr[:, d_ * 2 + bl, :], wr_[d_, bl], [], [bWg])
            P.dma("sp", Wi[:, d_ * 2 + bl, :], wi_[d_, bl], [], [bWg])
    gb = P.sb([128, 3, 4]); bgb = Buf()
    P.dma("sp", gb[:, 0, :], brc.rearrange("p a b -> p (a b)"), [], [bgb])
    P.dma("sp", gb[:, 1, :], bic.rearrange("p a b -> p (a b)"), [], [bgb])
    P.dma("sp", gb[:, 2, :], lamc.rearrange("p a b -> p (a b)"), [], [bgb])
    P.act(gb[:, 2, :], gb[:, 2, :], AF.Sigmoid, [bgb], [bgb])
    P.act(gb[:, 2, :], gb[:, 2, :], AF.Ln, [bgb], [bgb])
    P.ts(gb[:, 2, :], gb[:, 2, :], 8.0, None, ALU.mult, None, [bgb], [bgb])
    xh = [(P.sb([128, 516]), Buf()) for _ in range(2)]
    xc = P.sb([128, 512]); bxc = Buf()
    pr = (P.ps([128, 512]), PBuf()); pi = (P.ps([128, 512]), PBuf())
    r = P.sb([128, 512]); br = Buf(); i_ = P.sb([128, 512]); bi = Buf()
    a = P.sb([128, 512]); ba = Buf(); s = P.sb([128, 512]); bs = Buf()
    hh = [(P.sb([128, 512]), Buf()) for _ in range(2)]
    hst = P.sb([128, 1]); bhst = Buf()
    hf = [(P.sb([128, 512]), Buf()) for _ in range(2)]
    xg = [(P.sb([128, 512]), Buf()) for _ in range(2)]
    yo = [(P.sb([128, 512], BF16), Buf()) for _ in range(2)]
    if mixloc is not None:
        idl = P.sb([128, 128]); bidl = Buf()
        P.dma("sp", idl[:], ident[:, :], [], [bidl])
        yf = P.sb([128, 512]); byf = Buf()
        pyt = (P.ps([128, 512]), PBuf())
        yT = [(P.sb([128, 4, 128], BF16), Buf()) for _ in range(2)]
    seqs = [(0, NCTX)] + [(NCTX + g * 512, min(512, TL - g * 512)) for g in range((TL + 511) // 512)]
    n = 0
    for bl in range(2):
        for d_ in range(2):
            gi = d_ * 2 + bl
            P.op("dve", lambda e: e.memset(hst[:], 0.0), [], [bhst])
            order = seqs if d_ == 0 else [seqs[0]] + seqs[:0:-1]
            for (g0, gn_) in order:
                s0, s1 = (0, NCTX) if g0 < NCTX else (NCTX, T)
                x_, bx_ = xh[n % 2]; h_, bh_ = hh[n % 2]; hf_, bhf_ = hf[n % 2]; xg_, bxg_ = xg[n % 2]; y_, by_ = yo[n % 2]
                n += 1
                lo = max(g0 - 2, s0); hi = min(g0 + gn_ + 1, s1)
                P.op("dve", lambda e, x_=x_: e.memset(x_[:], 0.0), [], [bx_])
                P.dma("sp", x_[:, lo - (g0 - 2):hi - (g0 - 2)], xrT[bl][:, lo:hi], [], [bx_])
                P.ts(xc[:, 0:gn_], x_[:, 0:gn_], cw[:, bl, 0:1], cb[:, bl:bl + 1], ALU.mult, ALU.add, [bx_, bcw], [bxc])
                for j in range(1, 4):
                    P.stt(xc[:, 0:gn_], x_[:, j:j + gn_], cw[:, bl, j:j + 1], xc[:, 0:gn_], ALU.mult, ALU.add, [bx_, bcw, bxc], [bxc])
                P.mm(pr[0][:, 0:gn_], Wr[:, gi, :], xc[:, 0:gn_], True, True, [bWg, bxc], [pr[1]])
                P.mm(pi[0][:, 0:gn_], Wi[:, gi, :], xc[:, 0:gn_], True, True, [bWg, bxc], [pi[1]])
                P.act(r[:, 0:gn_], pr[0][:, 0:gn_], AF.Sigmoid, [pr[1], bgb], [br], bias=gb[:, 0, gi:gi + 1])
                P.act(i_[:, 0:gn_], pi[0][:, 0:gn_], AF.Sigmoid, [pi[1], bgb], [bi], bias=gb[:, 1, gi:gi + 1])
                P.act(a[:, 0:gn_], r[:, 0:gn_], AF.Exp, [br, bgb], [ba], scale=gb[:, 2, gi:gi + 1])
                P.tt(s[:, 0:gn_], a[:, 0:gn_], a[:, 0:gn_], ALU.mult, [ba], [bs])
                P.act(s[:, 0:gn_], s[:, 0:gn_], AF.Sqrt, [bs], [bs], bias=1.0, scale=-1.0)
                P.tt(i_[:, 0:gn_], i_[:, 0:gn_], xc[:, 0:gn_], ALU.mult, [bi, bxc], [bi])
                P.tt(i_[:, 0:gn_], i_[:, 0:gn_], s[:, 0:gn_], ALU.mult, [bi, bs], [bi])
                if d_ == 0:
                    P.op("dve", lambda e, h_=h_, gn_=gn_: e.tensor_tensor_scan(h_[:, 0:gn_], a[:, 0:gn_], i_[:, 0:gn_], hst[:, 0:1], ALU.mult, ALU.add),
                         [ba, bi, bhst], [bh_])
                    P.cp("dve", hst[:], h_[:, gn_ - 1:gn_], [bh_], [bhst])
                    P.dma("pool", hF[bl][:, g0:g0 + gn_], h_[:, 0:gn_], [bh_], [])
                else:
                    P.op("dve", lambda e, h_=h_, gn_=gn_: e.tensor_tensor_scan(h_[:, 0:gn_][:, ::-1], a[:, 0:gn_][:, ::-1], i_[:, 0:gn_][:, ::-1], hst[:, 0:1], ALU.mult, ALU.add),
                         [ba, bi, bhst], [bh_])
                    P.cp("dve", hst[:], h_[:, 0:1], [bh_], [bhst])
                    P.dma("sp", hf_[:, 0:gn_], hF[bl][:, g0:g0 + gn_], [], [bhf_])
                    P.dma("sp", xg_[:, 0:gn_], xgT[bl][:, g0:g0 + gn_], [], [bxg_])
                    P.tt(h_[:, 0:gn_], h_[:, 0:gn_], hf_[:, 0:gn_], ALU.add, [bh_, bhf_], [bh_])
                    P.act(xg_[:, 0:gn_], xg_[:, 0:gn_], AF.Gelu_apprx_tanh, [bxg_], [bxg_])
                    if mixloc is None:
                        P.tt(y_[:, 0:gn_], h_[:, 0:gn_], xg_[:, 0:gn_], ALU.mult, [bh_, bxg_], [by_])
                        P.dma("pool", mixT[256 + bl * 128:256 + (bl + 1) * 128, g0:g0 + gn_], y_[:, 0:gn_], [by_], [])
                    else:
                        yT_, byT_ = yT[n % 2]
                        nb_ = gn_ // 128
                        P.tt(yf[:, 0:gn_], h_[:, 0:gn_], xg_[:, 0:gn_], ALU.mult, [bh_, bxg_], [byf])
                        for j in range(nb_):
                            P.tr(pyt[0][:, j * 128:(j + 1) * 128], yf[:, j * 128:(j + 1) * 128], idl[:], [byf, bidl], [pyt[1]])
                        P.cp("act", yT_[:, 0:nb_, :], pyt[0][:, 0:gn_].rearrange("p (a b) -> p a b", b=128), [pyt[1]], [byT_])
                        mr0 = (TL + g0) if g0 < NCTX else (g0 - NCTX)
                        P.dma("pool", mixloc[mr0:mr0 + gn_, 256 + bl * 128:256 + (bl + 1) * 128].rearrange("(j p) c -> p j c", p=128),
                              yT_[:, 0:nb_, :], [byT_], [])
            if d_ == 0:
                all_dma_barrier(P)
    P.flush()
    all_dma_barrier(P)

    P.begin()
    cms = P.sb([64, 6, 64]); bcm = Buf()
    P.dma("sp", cms[:], cm.rearrange("a s t -> s a t"), [], [bcm])
    ones = P.sb([1, 64]); P.op("dve", lambda e: e.memset(ones[:], 1.0), [], [bcm])
    wa = P.sb([16, 2, 128]); bwa = Buf()
    P.dma("sp", wa[:], wa2.rearrange("d r c -> r d c"), [], [bwa])
    bar = P.sb([1, 2, 128])
    P.dma("sp", bar[:], barow.rearrange("d o c -> o d c"), [], [bwa])
    qs = [(P.sb([128, 512]), Buf()) for _ in range(2)]
    ks = [(P.sb([128, 512]), Buf()) for _ in range(2)]
    as_ = [(P.sb([16, 512]), Buf()) for _ in range(2)]
    tk = [(P.sb([64, 8, 640]), Buf()) for _ in range(2)]
    pla = (P.ps([64, 128]), PBuf()); pb = (P.ps([128, 64]), PBuf()); pdd = (P.ps([64, 128]), PBuf())
    pat = (P.ps([64, 64]), PBuf()); po = (P.ps([64, 256]), PBuf()); pS = (P.ps([128, 256]), PBuf())
    la = P.sb([64, 128]); bla = Buf()
    e1 = P.sb([128, 64]); be1 = Buf(); e2 = P.sb([128, 64]); be2 = Buf(); e3 = P.sb([64, 128]); be3 = Buf()
    qd = P.sb([128, 64], BF16); bqd = Buf(); ki = P.sb([128, 64], BF16); bki = Buf(); ke = P.sb([64, 128], BF16); bke = Buf()
    vb = P.sb([64, 256], BF16); bvb = Buf(); am = P.sb([64, 64], BF16); bam = Buf()
    S = P.sb([128, 256]); bS = Buf(); Sb = P.sb([128, 256], BF16); bSb = Buf()
    ost = [(P.sb([64, 256]), Buf()) for _ in range(2)]
    sgroups = [(0, NCTX)] + [(NCTX + g * 512, min(512, TL - g * 512)) for g in range((TL + 511) // 512)]
    n = 0
    nc_ = 0
    for d_ in range(2):
        aT = afT if d_ == 0 else abT
        oD = oF if d_ == 0 else oB
        P.op("dve", lambda e: e.memset(S[:], 0.0), [], [bS])
        P.op("dve", lambda e: e.memset(Sb[:], 0.0), [], [bSb])
        order = sgroups if d_ == 0 else [sgroups[0]] + sgroups[:0:-1]
        for (g0, gn_) in order:
            q_, bq_ = qs[n % 2]; k_, bk_ = ks[n % 2]; a_, ba_ = as_[n % 2]; t_, bt_ = tk[n % 2]
            n += 1
            nch = gn_ // 64
            P.dma("sp", q_[:, 0:gn_], qT[:, g0:g0 + gn_], [], [bq_])
            P.dma("sp", k_[:, 0:gn_], kT[:, g0:g0 + gn_], [], [bk_])
            P.dma("sp", a_[:, 0:gn_], aT[:, g0:g0 + gn_], [], [ba_])
            P.dma("sp", t_[:, 0:nch, :], tokmaj[g0:g0 + gn_, :].rearrange("(c p) f -> p c f", p=64), [], [bt_])
            chunks = range(nch) if d_ == 0 else range(nch - 1, -1, -1)
            for c in chunks:
                cs = slice(c * 64, (c + 1) * 64)
                P.mm(pla[0][:], a_[:, cs], wa[:, d_, :], True, False, [ba_, bwa], [pla[1]])
                P.mm(pla[0][:], ones[0:1, :], bar[0:1, d_, :], False, True, [bcm, bwa], [pla[1]])
                P.act(la[:], pla[0][:], AF.Sigmoid, [pla[1]], [bla])
                P.act(la[:], la[:], AF.Ln, [bla], [bla])
                P.mm(pb[0][:], la[:], cms[:, 3 * d_ + 0, :], True, True, [bla, bcm], [pb[1]])
                P.mm(pdd[0][:], cms[:, 3 * d_ + 1, :], la[:], True, True, [bla, bcm], [pdd[1]])
                P.act(e1[:], pb[0][:], AF.Exp, [pb[1]], [be1])
                P.act(e2[:], pb[0][:], AF.Exp, [pb[1]], [be2], scale=-1.0)
                P.act(e3[:], pdd[0][:], AF.Exp, [pdd[1]], [be3])
                P.stt(qd[:], q_[:, cs], 128.0 ** -0.5, e1[:], ALU.mult, ALU.mult, [bq_, be1], [bqd])
                P.tt(ki[:], k_[:, cs], e2[:], ALU.mult, [bk_, be2], [bki])
                P.tt(ke[:], t_[:, c, 0:128], e3[:], ALU.mult, [bt_, be3], [bke])
                P.cp("act", vb[:], t_[:, c, 128:384], [bt_], [bvb])
                P.mm(pat[0][:], ki[:], qd[:], True, True, [bki, bqd], [pat[1]])
                P.tt(am[:], pat[0][:], cms[:, 3 * d_ + 2, :], ALU.mult, [pat[1], bcm], [bam])
                P.mm(po[0][:], am[:], vb[:], True, False, [bam, bvb], [po[1]])
                P.mm(po[0][:], qd[:], Sb[:], False, True, [bqd, bSb], [po[1]])
                P.mm(pS[0][:], ke[:], vb[:], True, True, [bke, bvb], [pS[1]])
                el = e1[:, 63:64] if d_ == 0 else e1[:, 0:1]
                P.stt(S[:], S[:], el, pS[0][:], ALU.mult, ALU.add, [bS, be1, pS[1]], [bS])
                P.cp("act", Sb[:], S[:], [bS], [bSb])
                o_, bo_ = ost[nc_ % 2]
                nc_ += 1
                P.cp("act", o_[:], po[0][:], [po[1]], [bo_])
                P.dma("pool", oD[g0 + c * 64:g0 + (c + 1) * 64, :], o_[:], [bo_], [])
    P.flush()
    all_dma_barrier(P)
    out_tm = None
    if mixloc is not None:
        def out_tm(t, h, o2, bo2):
            mr0 = (TL + t * 128) if t < NCTX // 128 else (t * 128 - NCTX)
            P.dma("pool", mixloc[mr0:mr0 + 128, 0:256], o2[:], [bo2], [])
    emit_finish(P, T, 1, tokmaj, 384, [oF], [oB], gn_bc[:, :], AF.Silu, ident[:, :], mixT, out_tm=out_tm)


def build_oddmix(TL, NCTX=256):
    P = Prog()
    emit_oddmix(P, "", TL, NCTX)
    P.begin()
    P.flush(final=True)
    return P.nc


def emit_oddmix(P, pre, TL, NCTX=256, xsrc=None, mixloc=None):
    T = NCTX + TL
    GW = 64
    rows = TL // GW
    I = lambda n, s, d=F32: P.dram(pre + n, s, d, kind="ExternalInput")
    xin = I("xin", [T, D]) if xsrc is None else None
    ccol = I("ccol", [128, 16]); cccol = I("cccol", [128, 16])
    wmod = I("wmod", [D, 4096]); bmod = I("bmod", [1, 4096])
    win = I("win", [D, 1544])
    bgb = I("bgb", [64, 2, 4]); gn_bc = I("gn_bc", [128, 256])
    ident = I("ident", [128, 128])
    cm = I("cm", [10, 64, 64])
    mixT = P.dram(pre + "mixT", [512, T], BF16, kind="ExternalOutput") if mixloc is None else None
    tokmaj = P.dram(pre + "tokmaj", [T, 1288], F32)
    qT = [P.dram(pre + "qT%d" % i, [128, T], F32) for i in range(2)]
    kT = [P.dram(pre + "kT%d" % i, [128, T], F32) for i in range(2)]
    hF = [P.dram(pre + "hF%d" % i, [T, 256], F32) for i in range(2)]
    hB = [P.dram(pre + "hB%d" % i, [T, 256], F32) for i in range(2)]
    load_x = None
    if xsrc is not None:
        G = xsrc["G"]; SQ = TL // 4; CQ = NCTX // 4; RQ = rows // 4
        G1v = G.rearrange("(tl r w) d -> tl r w d", r=4, w=128)

        def load_x(t, xt, bxt):
            if t < NCTX // 128:
                for hf_ in range(128 // CQ):
                    r = t * (128 // CQ) + hf_
                    P.dma("sp", xt[hf_ * CQ:(hf_ + 1) * CQ, :], G1v[SQ // 128, r, 0:CQ, :], [xsrc["bG"]], [bxt])
            else:
                j0 = (t - NCTX // 128) * 128
                pos = j0
                while pos < j0 + 128:
                    c_, rw = divmod(pos, rows)
                    r, lr = divmod(rw, RQ)
                    n_ = min(RQ - lr, j0 + 128 - pos)
                    ip = c_ * RQ + lr
                    P.dma("sp", xt[pos - j0:pos - j0 + n_, :], G1v[ip // 128, r, ip % 128:ip % 128 + n_, :], [xsrc["bG"]], [bxt])
                    pos += n_
    mcol = P.sb([128, 2, 32]); bmcol = Buf()
    emit_mod_rows(P, [ccol[:, :], cccol[:, :]], wmod, bmod[:, :], 4096, None, (4, 5, 6, 7), ident=ident[:, :], mcol=mcol, bmcol=bmcol,
                  col_groups={g: g for g in range(8)})
    fm = [(1288, 128, qT[0]), (1416, 128, qT[1]), (0, 128, kT[0]), (128, 128, kT[1])]
    emit_projection(P, xin, T, NCTX // 128, win, 1544, mcol, bmcol, ident[:, :], 1288, tokmaj, fm, load_x=load_x)
    all_dma_barrier(P)

    P.begin()
    SC = 128.0 ** -0.5
    cms = P.sb([64, 10, 64]); bcm = Buf()
    P.dma("sp", cms[:], cm.rearrange("a s t -> s a t"), [], [bcm])
    ones = P.sb([64, 128]); P.op("dve", lambda e: e.memset(ones[:], 1.0), [], [bcm])
    bg = P.sb([64, 2, 4]); P.dma("sp", bg[:], bgb[:, :, :], [], [bcm])
    qs = [(P.sb([128, 512]), Buf()) for _ in range(2)]
    ks = [(P.sb([128, 512]), Buf()) for _ in range(2)]
    qb = [(P.sb([128, 512], BF16), Buf()) for _ in range(2)]
    kb = [(P.sb([128, 512], BF16), Buf()) for _ in range(2)]
    tk = [(P.sb([64, 8, 1288]), Buf()) for _ in range(2)]
    pD = (P.ps([64, 64]), PBuf()); pG = (P.ps([128, 64]), PBuf()); pc = (P.ps([128, 8]), PBuf())
    pQK = (P.ps([64, 64]), PBuf()); pT = (P.ps([64, 64]), PBuf()); pN = (P.ps([64, 257]), PBuf())
    pQC = (P.ps([64, 257]), PBuf()); pC = (P.ps([128, 257]), PBuf())
    gc = P.sb([64, 8]); bgc = Buf()
    A1 = P.sb([64, 64]); bA1 = Buf(); Fb = P.sb([64, 128]); bFb = Buf(); Ib = P.sb([64, 128]); bIb = Buf()
    Dk = P.sb([64, 64]); bDk = Buf(); wi = P.sb([64, 64]); bwi = Buf(); sm = P.sb([64, 64]); bsm = Buf()
    smT = P.sb([64, 64], BF16); bsmT = Buf()
    v = P.sb([128, 16]); bv = Buf()
    va = [(P.sb([64, 257], BF16), Buf()) for _ in range(2)]
    for va_, bva_ in va:
        P.op("dve", lambda e, va_=va_: e.memset(va_[:], 1.0), [], [bva_])
    nA = P.sb([64, 257]); bnA = Buf(); tot = P.sb([64, 257]); btot = Buf()
    kw = P.sb([64, 128], BF16); bkw = Buf()
    C = P.sb([128, 257]); bC = Buf(); Cb = P.sb([128, 257], BF16); bCb = Buf()
    m = P.sb([128, 1]); bm = Buf()
    ho = [(P.sb([64, 256]), Buf()) for _ in range(2)]
    sgroups = [(0, NCTX)] + [(NCTX + g * 512, min(512, TL - g * 512)) for g in range((TL + 511) // 512)]
    n = 0; nc_ = 0
    col = lambda i, p=64: v[0:p, i:i + 1]
    V = [bv]
    for h in range(2):
        for d_ in range(2):
            hD = hF[h] if d_ == 0 else hB[h]
            cb_ = 5 * d_
            P.op("dve", lambda e: e.memset(C[:], 0.0), [], [bC])
            P.op("dve", lambda e: e.memset(Cb[:], 0.0), [], [bCb])
            P.op("dve", lambda e: e.memset(m[:], NEG), [], [bm])
            order = sgroups if d_ == 0 else [sgroups[0]] + sgroups[:0:-1]
            for (g0, gn_) in order:
                q_, bq_ = qs[n % 2]; k_, bk_ = ks[n % 2]; t_, bt_ = tk[n % 2]; qb_, bqb_ = qb[n % 2]; kb_, bkb_ = kb[n % 2]
                n += 1
                nch = gn_ // 64
                P.dma("sp", q_[:, 0:gn_], qT[h][:, g0:g0 + gn_], [], [bq_])
                P.dma("sp", k_[:, 0:gn_], kT[h][:, g0:g0 + gn_], [], [bk_])
                P.dma("sp", t_[:, 0:nch, :], tokmaj[g0:g0 + gn_, :].rearrange("(c p) f -> p c f", p=64), [], [bt_])
                P.cp("dve", qb_[:, 0:gn_], q_[:, 0:gn_], [bq_], [bqb_])
                P.cp("act", kb_[:, 0:gn_], k_[:, 0:gn_], [bk_], [bkb_])
                chunks = range(nch) if d_ == 0 else range(nch - 1, -1, -1)
                for c in chunks:
                    cs = slice(c * 64, (c + 1) * 64)
                    va_, bva_ = va[nc_ % 2]; ho_, bho_ = ho[nc_ % 2]
                    nc_ += 1
                    P.tt(gc[:, 0:4], t_[:, c, 1280 + 4 * h:1284 + 4 * h], bg[:, h, :], ALU.add, [bt_, bcm], [bgc])
                    ic = gc[:, 2 * d_:2 * d_ + 1]; fc = gc[:, 4:5]
                    P.act(fc, gc[:, 2 * d_ + 1:2 * d_ + 2], AF.Sigmoid, [bgc], [bgc])
                    P.act(fc, fc, AF.Ln, [bgc], [bgc])
                    P.ts(A1[:], cms[:, cb_ + 0, :], fc, None, ALU.mult, None, [bcm, bgc], [bA1])
                    P.ts(Fb[:], ones[:], fc, None, ALU.mult, None, [bcm, bgc], [bFb])
                    P.ts(Ib[:], ones[:], ic, None, ALU.mult, None, [bcm, bgc], [bIb])
                    P.mm(pD[0][:], A1[:], ones[:, 0:64], True, False, [bA1, bcm], [pD[1]])
                    P.mm(pD[0][:], Fb[:, 0:64], cms[:, cb_ + 1, :], False, False, [bFb, bcm], [pD[1]])
                    P.mm(pD[0][:], Ib[:, 0:64], cms[:, cb_ + 4, :], False, True, [bIb, bcm], [pD[1]])
                    P.mm(pG[0][:], Fb[:], cms[:, cb_ + 2, :], True, False, [bFb, bcm], [pG[1]])
                    P.mm(pG[0][:], Ib[:], cms[:, cb_ + 4, :], False, True, [bIb, bcm], [pG[1]])
                    P.mm(pc[0][0:64, 0:1], cms[:, cb_ + 0, :], fc, True, True, [bcm, bgc], [pc[1]])
                    P.mm(pc[0][:, 1:2], ones[:], fc, True, True, [bcm, bgc], [pc[1]])
                    P.mm(pc[0][0:64, 2:3], cms[:, cb_ + 2, :], fc, True, True, [bcm, bgc], [pc[1]])
                    P.mm(pQK[0][:], qb_[:, cs], kb_[:, cs], True, True, [bqb_, bkb_], [pQK[1]])
                    P.tt(Dk[:], pD[0][:], cms[:, cb_ + 3, :], ALU.add, [pD[1], bcm], [bDk])
                    P.op("dve", lambda e: e.reduce_max(col(0), Dk[:], AX.X), [bDk], V)
                    P.tt(col(1), pc[0][0:64, 0:1], m[0:64, :], ALU.add, [pc[1], bm], V)
                    P.tt(col(2), col(1), col(0), ALU.max, V, V)
                    P.ts(col(3), col(2), -1.0, None, ALU.mult, None, V, V)
                    P.act(wi[:], Dk[:], AF.Exp, [bDk, bv], [bwi], bias=col(3))
                    P.act(col(4), col(1), AF.Exp, V, V, bias=col(3))
                    P.act(col(5), col(3), AF.Exp, V, V)
                    P.stt(sm[:], pQK[0][:], SC, wi[:], ALU.mult, ALU.mult, [pQK[1], bwi], [bsm])
                    P.tr(pT[0][:], sm[:], cms[:, cb_ + 4, :], [bsm, bcm], [pT[1]])
                    P.cp("act", smT[:], pT[0][:], [pT[1]], [bsmT])
                    P.cp("dve", va_[:, 0:256], t_[:, c, 256 + h * 256:512 + h * 256], [bt_], [bva_])
                    P.mm(pN[0][:], smT[:], va_[:], True, True, [bsmT, bva_], [pN[1]])
                    P.mm(pQC[0][:], qb_[:, cs], Cb[:], True, True, [bqb_, bCb], [pQC[1]])
                    P.cp("act", nA[:], pN[0][:], [pN[1]], [bnA])
                    P.stt(tot[:], pQC[0][:], col(4), nA[:], ALU.mult, ALU.add, [pQC[1], bv, bnA], [btot])
                    P.ts(col(13), tot[:, 256:257], -1.0, None, ALU.mult, None, [btot], V)
                    P.tt(col(6), tot[:, 256:257], col(13), ALU.max, [btot, bv], V)
                    P.tt(col(6), col(6), col(5), ALU.max, V, V)
                    P.op("dve", lambda e: e.reciprocal(col(6), col(6)), V, V)
                    P.ts(ho_[:], tot[:, 0:256], col(6), None, ALU.mult, None, [btot, bv], [bho_])
                    P.dma("pool", hD[g0 + c * 64:g0 + (c + 1) * 64, :], ho_[:], [bho_], [])
                    P.op("dve", lambda e: e.reduce_max(col(7, 128), pG[0][:], AX.X), [pG[1]], V)
                    P.tt(col(8, 128), pc[0][:, 1:2], m[:], ALU.add, [pc[1], bm], V)
                    P.tt(col(9, 128), col(8, 128), col(7, 128), ALU.max, V, V)
                    P.ts(col(10, 128), col(9, 128), -1.0, None, ALU.mult, None, V, V)
                    P.tt(col(11), pc[0][0:64, 2:3], ic, ALU.add, [pc[1], bgc], V)
                    P.act(col(11), col(11), AF.Exp, V, V, bias=col(10))
                    P.act(col(12, 128), col(8, 128), AF.Exp, V, V, bias=col(10, 128))
                    P.ts(kw[:], t_[:, c, h * 128:(h + 1) * 128], col(11), SC, ALU.mult, ALU.mult, [bt_, bv], [bkw])
                    P.mm(pC[0][:], kw[:], va_[:], True, True, [bkw, bva_], [pC[1]])
                    P.stt(C[:], C[:], col(12, 128), pC[0][:], ALU.mult, ALU.add, [bC, bv, pC[1]], [bC])
                    P.cp("act", Cb[:], C[:], [bC], [bCb])
                    P.cp("dve", m[:], col(9, 128), V, [bm])
    P.flush()
    all_dma_barrier(P)
    out_tm = None
    if mixloc is not None:
        mv_ = mixloc.rearrange("(r c) f -> c r f", c=GW)

        def out_tm(t, h, o2, bo2):
            j0 = (t - NCTX // 128) * 128
            pos = j0
            while pos < j0 + 128:
                c_, rw = divmod(pos, rows)
                n_ = min(rows - rw, j0 + 128 - pos)
                P.dma("pool", mv_[c_, rw:rw + n_, h * 256:(h + 1) * 256], o2[pos - j0:pos - j0 + n_, :], [bo2], [])
                pos += n_
    emit_finish(P, T, 2, tokmaj, 768, hF, hB, gn_bc[:, :], AF.Sigmoid, ident[:, :], mixT, out_tm=out_tm,
                t_start=(NCTX // 128 if mixloc is not None else 0))


def col16(v):
    return np.ascontiguousarray(v.reshape(16, 128).T)


def bc(v, n=128):
    return np.ascontiguousarray(np.broadcast_to(v, (n, v.shape[-1])))


def tri_consts():
    s = np.arange(64)[:, None]; t = np.arange(64)[None, :]
    f = np.float32
    sixteenth = np.float32(0.0625)
    return np.stack([np.where(s <= t, sixteenth, 0).astype(f), np.where(s > t, sixteenth, 0).astype(f), (s <= t).astype(f),
                     np.where(s >= t, sixteenth, 0).astype(f), np.where(s < t, sixteenth, 0).astype(f), (s >= t).astype(f)])


def odd_consts():
    u = np.arange(64)[:, None]; t = np.arange(64)[None, :]
    f = np.float32
    out = []
    for d in range(2):
        tri = (u <= t) if d == 0 else (u >= t)
        st = (u > t) if d == 0 else (u < t)
        ok = (t <= u) if d == 0 else (t >= u)
        out += [tri.astype(f), np.where(tri, -1.0, 0.0).astype(f), st.astype(f), np.where(ok, 0, NEG).astype(f), np.eye(64, dtype=f)]
    return np.stack(out)


def even_inputs(inp, j, x_b, ctx_b, c_b, c_ctx, hg):
    l = 2 * j
    w = inp["ev_w_in"][j]
    q0, k0, v0, r0, af0, ab0, xr0, xg0 = 0, 512, 1024, 2048, 3072, 3088, 3104, 4128
    h = hg
    cols = np.concatenate([np.arange(k0 + h * 128, k0 + (h + 1) * 128), np.arange(v0 + h * 256, v0 + (h + 1) * 256),
                           np.arange(r0 + h * 256, r0 + (h + 1) * 256), np.arange(q0 + h * 128, q0 + (h + 1) * 128),
                           np.arange(xr0 + h * 256, xr0 + (h + 1) * 256), np.arange(xg0 + h * 256, xg0 + (h + 1) * 256),
                           np.arange(af0, af0 + 16), np.arange(ab0, ab0 + 16)])
    ch = slice(h * 256, (h + 1) * 256)

    def colblk(v):
        return v[..., ch].reshape(v.shape[:-1] + (2, 128))
    return dict(
        xin=np.ascontiguousarray(np.concatenate([ctx_b, x_b], 0)), ccol=col16(c_b), cccol=col16(c_ctx),
        wmod=np.ascontiguousarray(inp["w_mod"][l][:, 0:4096]), bmod=np.ascontiguousarray(inp["b_mod"][l][None, 0:4096]),
        win=np.ascontiguousarray(w[:, cols]),
        wa2=np.ascontiguousarray(inp["gla_w_a2"][j][:, :, h * 128:(h + 1) * 128]),
        barow=np.ascontiguousarray(inp["gla_b_a"][j][:, None, h * 128:(h + 1) * 128]),
        gn_bc=bc(inp["gla_norm"][j]),
        convw=np.ascontiguousarray(colblk(inp["lru_conv_w"][j]).transpose(2, 1, 0)),
        convb=np.ascontiguousarray(colblk(inp["lru_conv_b"][j]).T),
        wr_=np.ascontiguousarray(inp["lru_w_r"][j][:, 2 * h:2 * h + 2]), wi_=np.ascontiguousarray(inp["lru_w_i"][j][:, 2 * h:2 * h + 2]),
        brc=np.ascontiguousarray(colblk(inp["lru_b_r"][j]).transpose(2, 0, 1)), bic=np.ascontiguousarray(colblk(inp["lru_b_i"][j]).transpose(2, 0, 1)),
        lamc=np.ascontiguousarray(colblk(inp["lru_lam"][j]).transpose(2, 0, 1)),
        ident=np.eye(128, dtype=np.float32), cm=tri_consts())


def odd_inputs(inp, j, xscan_b, c_b, c_ctx, hg):
    l = 2 * j + 1
    w = inp["od_w_in"][j]
    hs = [2 * hg, 2 * hg + 1]
    q0, k0, v0, o0, g0 = 0, 1024, 2048, 4096, 6144
    cols = np.concatenate([np.arange(k0 + h * 128, k0 + (h + 1) * 128) for h in hs] + [np.arange(v0 + h * 256, v0 + (h + 1) * 256) for h in hs]
                          + [np.arange(o0 + h * 256, o0 + (h + 1) * 256) for h in hs] + [np.array([g0 + h, g0 + 8 + h, g0 + 16 + h, g0 + 24 + h]) for h in hs]
                          + [np.arange(q0 + h * 128, q0 + (h + 1) * 128) for h in hs])
    bgate = inp["mlstm_b_gate"][j]
    bgb = np.ascontiguousarray(np.broadcast_to(np.stack([bgate[:, h] for h in hs])[None], (64, 2, 4))).astype(np.float32)
    return dict(xin=(None if xscan_b is None else np.ascontiguousarray(xscan_b)), ccol=col16(c_b), cccol=col16(c_ctx),
                wmod=np.ascontiguousarray(inp["w_mod"][l][:, 0:4096]), bmod=np.ascontiguousarray(inp["b_mod"][l][None, 0:4096]),
                win=np.ascontiguousarray(w[:, cols]), bgb=bgb, gn_bc=bc(inp["mlstm_norm"][j]),
                ident=np.eye(128, dtype=np.float32), cm=odd_consts())


def tok_inputs(inp, l, mixT, xres, c_b, c_ctx, wout):
    return dict(mixT=mixT, xres=xres, ccol=col16(c_b), cccol=col16(c_ctx),
                wmod=np.ascontiguousarray(inp["w_mod"][l][:, 4096:12288]), bmod=np.ascontiguousarray(inp["b_mod"][l][None, 4096:12288]),
                lnp=np.stack([bc(inp["ln_g"][l, 0]), bc(inp["ln_b"][l, 0]), bc(inp["ln_g"][l, 1]), bc(inp["ln_b"][l, 1])]),
                wout=wout, wr=np.ascontiguousarray(np.concatenate([inp["moe_w_group"][l], inp["moe_w_expert"][l]], 1)),
                brb=bc(np.concatenate([inp["moe_b_group"][l], inp["moe_b_expert"][l]])),
                wg=inp["moe_w_gate"][l], wu=inp["moe_w_up"][l], wd=inp["moe_w_down"][l], ident=np.eye(128, dtype=np.float32))


def gather_chunks(P, src, nrows, CR, dst):
    all_dma_barrier(P)
    bG = Buf()
    evs = []
    r0 = 0
    off = 0
    while r0 < nrows:
        n = min(CR, nrows - r0)
        evs.append(P.coll(src[r0:r0 + n, :], dst[off:off + 4 * n, :], [], [bG]))
        r0 += n
        off += 4 * n
    for q in ("sp", "act", "pool"):
        P._waits(q, [], [], evs)
    return bG


def build_fused(S, NCTX, NE=32):
    P = Prog()
    T = NCTX + S
    SQ = S // 4
    CQ = NCTX // 4
    NT0 = SQ // 128 + 1
    NP0 = NT0 * 128
    NT1 = SQ // 128
    CR = min(1024, SQ)
    mix0 = P.dram("mix0", [T, 512], BF16); G0 = P.dram("G0", [4 * T, 512], BF16)
    emit_evenmix(P, "e0_", S, NCTX, mixloc=mix0)
    bG0 = gather_chunks(P, mix0, T, CR, G0)
    xout0 = P.dram("xout0", [NP0, D], F32); G1 = P.dram("G1", [4 * NP0, D], F32)
    md = dict(G=G0, CR=CR, S=S, SQ=SQ, NCTX=NCTX, CQ=CQ, bG=bG0)
    RQ = S // 64 // 4
    emit_tokloc(P, "t0_", NT0, True, mix=md, xout=xout0, NE=NE, out_cm=RQ)
    bG1 = gather_chunks(P, xout0, NP0, 128, G1)
    mix1 = P.dram("mix1", [S, 512], BF16); Gm1 = P.dram("Gm1", [4 * S, 512], BF16)
    emit_oddmix(P, "o1_", S, NCTX, xsrc=dict(G=G1, bG=bG1), mixloc=mix1)
    bGm1 = gather_chunks(P, mix1, S, CR, Gm1)
    md1 = dict(G=Gm1, CR=CR, S=S, SQ=SQ, NCTX=NCTX, CQ=CQ, bG=bGm1)
    emit_tokloc(P, "t1_", NT1, False, mix=md1, xres=xout0, NE=NE, xres_cm=RQ)
    P.begin()
    P.flush(final=True)
    return P.nc


def fused_inputs(inp, b, q, S, NCTX, NE=32):
    x = inp["x"]; ctx = inp["ctx"]; c = inp["c"]; c_ctx = inp["c_ctx"]
    SQ = S // 4; CQ = NCTX // 4
    NP0 = (SQ // 128 + 1) * 128
    d = {}
    for k, v in even_inputs(inp, 0, x[b], ctx[b], c[b], c_ctx, q).items():
        d["e0_" + k] = v
    xr = np.zeros((NP0, D), np.float32)
    xr[0:SQ] = x[b, q * SQ:(q + 1) * SQ]
    xr[NP0 - 128:NP0 - 128 + CQ] = ctx[b, q * CQ:(q + 1) * CQ]
    perm0 = np.concatenate([np.concatenate([np.arange(r * 256, (r + 1) * 256), np.arange(1024 + r * 256, 1024 + (r + 1) * 256)]) for r in range(4)])
    for k, v in tok_inputs(inp, 0, None, xr, c[b], c_ctx, np.ascontiguousarray(inp["ev_w_out"][0][perm0])).items():
        if k != "mixT":
            d["t0_" + k] = v[:NE] if k in ("wg", "wu", "wd") else v
    for k, v in odd_inputs(inp, 0, None, c[b], c_ctx, q).items():
        if k != "xin":
            d["o1_" + k] = v
    for k, v in tok_inputs(inp, 1, None, None, c[b], c_ctx, inp["od_w_out"][0]).items():
        if k not in ("mixT", "xres", "cccol"):
            d["t1_" + k] = v[:NE] if k in ("wg", "wu", "wd") else v
    return d


def kernel(_NE=32, **inp):
    inp = {k: np.asarray(v) for k, v in inp.items()}
    x = inp["x"]
    B, S, _ = x.shape
    NCTX = inp["ctx"].shape[1]
    SQ = S // 4
    cores = [(b, q) for b in range(B) for q in range(4)]
    nc = build_fused(S, NCTX, _NE)
    res = run_bass_kernel_spmd(nc, [fused_inputs(inp, b, q, S, NCTX, _NE) for (b, q) in cores], core_ids=list(range(len(cores))))
    out = np.zeros_like(x)
    for i, (b, q) in enumerate(cores):
        out[b, q * SQ:(q + 1) * SQ] = res.results[i]["t1_xout"][0:SQ]
    return out
```

```python
import numpy as np
from contextlib import ExitStack
import ml_dtypes
import concourse.bass as bass
import concourse.mybir as mybir
from concourse.bass_utils import run_bass_kernel_spmd

F32 = mybir.dt.float32
BF16 = mybir.dt.bfloat16
AF = mybir.ActivationFunctionType
ALU = mybir.AluOpType
AX = mybir.AxisListType

D = 2048
ALPHA = 4.0 ** 0.25
EPS = 1e-5
NEG = -1e30


class Buf:
    __slots__ = ("w", "r")

    def __init__(self):
        self.w = None
        self.r = {}


class PBuf(Buf):
    __slots__ = ()
    excl = True


class Prog:
    CE = ("pe", "act", "dve", "pool")
    SEM_ROT = 16000

    def __init__(self, n_dma_sems=12):
        self.nc = bass.Bass("TRN2", target_bir_lowering=False)
        self.es = ExitStack()
        self.pes = None
        self.ops = {e: [] for e in ("pe", "act", "dve", "pool", "sp")}
        self.cnt = {e: 0 for e in self.CE}
        self.sems = []
        self.cur_sem = {}
        self.seen = {e: {} for e in self.ops}
        for e in self.CE:
            self.cur_sem[e] = self._new_sem("c_" + e)
        self.dq = {}
        for q in ("sp", "pool", "act"):
            self.dq[q] = dict(sems=[self._new_sem("d_%s" % q) for i in range(n_dma_sems)], i=0)
        self.n_t = 0

    def _new_sem(self, name):
        h = self.es.enter_context(self.nc.semaphore(name + "_%d" % len(self.sems)))
        self.sems.append(h)
        return len(self.sems) - 1

    def dram(self, name, shape, dt, kind="Internal"):
        return self.nc.dram_tensor(name, list(shape), dt, kind=kind).ap()

    def begin(self):
        self.pes = ExitStack()

    def sb(self, shape, dt=F32):
        self.n_t += 1
        st = self.pes if self.pes is not None else self.es
        return st.enter_context(self.nc.sbuf_tensor("t%d" % self.n_t, list(shape), dt))

    def ps(self, shape, dt=F32):
        self.n_t += 1
        st = self.pes if self.pes is not None else self.es
        return st.enter_context(self.nc.psum_tensor("p%d" % self.n_t, list(shape), dt))

    def _waits(self, eng, reads, writes, extra=()):
        waits = {}

        def need(ev):
            if ev is None:
                return
            s, v, src = ev
            if src == "pe" and eng == "pe":
                return
            if self.seen[eng].get(s, 0) >= v:
                return
            if waits.get(s, 0) < v:
                waits[s] = v
        for b in reads:
            need(b.w)
            if getattr(b, "excl", False):
                for ev in b.r.values():
                    if ev[2] != eng:
                        need(ev)
        for b in writes:
            need(b.w)
            for ev in b.r.values():
                need(ev)
        for ev in extra:
            need(ev)
        for s, v in waits.items():
            self.seen[eng][s] = v
            self.ops[eng].append(lambda e, s=s, v=v: e.wait_ge(self.sems[s], v))

    def _post(self, ev, reads, writes):
        key = ev[2] if not ev[2].startswith("dma") else ("d", ev[0])
        for b in reads:
            b.r[key] = ev
        for b in writes:
            b.w = ev
            b.r = {}

    def op(self, eng, f, reads=(), writes=()):
        self._waits(eng, reads, writes)
        if self.cnt[eng] >= self.SEM_ROT:
            self.cur_sem[eng] = self._new_sem("c_" + eng)
            self.cnt[eng] = 0
        self.cnt[eng] += 1
        s = self.cur_sem[eng]
        ev = (s, self.cnt[eng], eng)
        self.ops[eng].append(lambda e, s=s: f(e).then_inc(self.sems[s], 1))
        self._post(ev, reads, writes)
        return ev

    def dma(self, q, out, in_, reads=(), writes=(), **kw):
        d = self.dq[q]
        K = len(d["sems"])
        k = d["i"] % K
        j = d["i"] // K
        d["i"] += 1
        s = d["sems"][k]
        extra = [(s, 16 * j, "dma" + q)] if j > 0 else []
        self._waits(q, reads, writes, extra)
        ev = (s, 16 * (j + 1), "dma" + q)
        self.ops[q].append(lambda e, s=s: e.dma_start(out=out, in_=in_, **kw).then_inc(self.sems[s], 16))
        self._post(ev, reads, writes)
        return ev

    def dma_f(self, q, f, reads=(), writes=()):
        d = self.dq[q]
        K = len(d["sems"])
        k = d["i"] % K
        j = d["i"] // K
        d["i"] += 1
        s = d["sems"][k]
        extra = [(s, 16 * j, "dma" + q)] if j > 0 else []
        self._waits(q, reads, writes, extra)
        ev = (s, 16 * (j + 1), "dma" + q)
        self.ops[q].append(lambda e, s=s: f(e).then_inc(self.sems[s], 16))
        self._post(ev, reads, writes)
        return ev

    def coll(self, src, dst, reads=(), writes=(), groups=((0, 1, 2, 3), (4, 5, 6, 7))):
        if not hasattr(self, "cc_sem"):
            self.cc_sem = self._new_sem("cc")
            self.cc_n = 0
        s = self.cc_sem
        self.cc_n += 1
        self._waits("pool", reads, writes)
        rg = [list(g) for g in groups]
        self.ops["pool"].append(lambda e: e.collective_compute("AllGather", ALU.bypass, replica_groups=rg,
                                                               ins=[src.opt()], outs=[dst.opt()]).then_inc(self.sems[s]))
        ev = (s, self.cc_n, "dmacc")
        self._post(ev, reads, writes)
        self.cc_events = getattr(self, "cc_events", []) + [ev]
        return ev

    def flush(self, final=False):
        if final:
            finals = {}
            for q, d in self.dq.items():
                K = len(d["sems"])
                for k, s in enumerate(d["sems"]):
                    n = (d["i"] - k + K - 1) // K if d["i"] > k else 0
                    if n > 0:
                        finals[s] = 16 * n
            for e in self.CE:
                if self.cnt[e] > 0:
                    finals[self.cur_sem[e]] = self.cnt[e]
            for ev in getattr(self, "cc_events", []):
                finals[ev[0]] = ev[1]
            for s, v in finals.items():
                if self.seen["sp"].get(s, 0) < v:
                    self.seen["sp"][s] = v
                    self.ops["sp"].append(lambda e, s=s, v=v: e.wait_ge(self.sems[s], v))
        ops = self.ops
        with self.nc.Block() as block:
            @block.tensor
            def _(e):
                for f in ops["pe"]:
                    f(e)

            @block.scalar
            def _(e):
                for f in ops["act"]:
                    f(e)

            @block.vector
            def _(e):
                for f in ops["dve"]:
                    f(e)

            @block.gpsimd
            def _(e):
                for f in ops["pool"]:
                    f(e)

            @block.sync
            def _(e):
                for f in ops["sp"]:
                    f(e)
        self.ops = {e: [] for e in ops}
        if self.pes is not None:
            self.pes.close()
            self.pes = None
        if final:
            self.es.close()

    def mm(self, out, lhsT, rhs, start, stop, reads, writes):
        return self.op("pe", lambda e: e.matmul(out, lhsT, rhs, start=start, stop=stop), reads, writes)

    def tr(self, out, in_, ident, reads, writes):
        return self.op("pe", lambda e: e.transpose(out, in_, ident), reads, writes)

    def act(self, out, in_, func, reads, writes, bias=None, scale=None):
        kw = {}
        if bias is not None:
            kw["bias"] = bias
        if scale is not None:
            kw["scale"] = scale
        return self.op("act", lambda e: e.activation(out, in_, func, **kw), reads, writes)

    def tt(self, out, a, b, op, reads, writes, eng="dve"):
        return self.op(eng, lambda e: e.tensor_tensor(out, a, b, op), reads, writes)

    def ts(self, out, a, s1, s2, op0, op1, reads, writes):
        if s2 is None:
            return self.op("dve", lambda e: e.tensor_scalar(out, a, s1, None, op0), reads, writes)
        return self.op("dve", lambda e: e.tensor_scalar(out, a, s1, s2, op0, op1), reads, writes)

    def stt(self, out, a, s, b, op0, op1, reads, writes):
        return self.op("dve", lambda e: e.scalar_tensor_tensor(out, a, s, b, op0, op1), reads, writes)

    def cp(self, eng, out, in_, reads, writes):
        if eng == "act":
            return self.op("act", lambda e: e.copy(out, in_), reads, writes)
        return self.op(eng, lambda e: e.tensor_copy(out, in_), reads, writes)


def emit_mod_rows(P, ccols, wmod, bmod, ncols, mbc, add_one_groups, ident=None, mcol=None, bmcol=None, col_groups=None):
    P.begin()
    nco = len(ccols)
    col_groups = col_groups or {}
    if col_groups:
        idt = P.sb([128, 128]); bid = Buf()
        P.dma("sp", idt[:], ident, [], [bid])
        ptc = P.ps([128, 512]); bptc = PBuf()
    ones = P.sb([128, 128]); b_ones = Buf()
    P.op("dve", lambda e: e.memset(ones[:], 1.0), [], [b_ones])
    cbl = []
    for ci, cc in enumerate(ccols):
        cs = P.sb([128, 16]); bcs = Buf()
        P.dma("sp", cs[:], cc, [], [bcs])
        sc = P.sb([128, 16]); bsc = Buf()
        P.act(sc[:], cs[:], AF.Silu, [bcs], [bsc])
        cb = P.sb([128, 16, 128]); bcb = Buf()
        for k in range(16):
            P.ts(cb[:, k, :], ones[:], sc[:, k:k + 1], None, ALU.mult, None, [b_ones, bsc], [bcb])
        cbl.append((cb, bcb))
    bm = P.sb([1, ncols]); bbm = Buf()
    P.dma("sp", bm[:], bmod, [], [bbm])
    wts = [(P.sb([128, 8, 512]), Buf()) for _ in range(2)]
    pss = [(P.ps([128, 512]), PBuf()) for _ in range(2)]
    outs = [(P.sb([128, 512]), Buf()) for _ in range(2)]
    wv = wmod.rearrange("(k p) c -> p k c", p=128)
    n = 0
    for g in range(ncols // 512):
        cs_ = slice(g * 512, (g + 1) * 512)
        for half in range(2):
            wt, bwt = wts[half]
            P.dma("sp", wt[:], wv[:, half * 8:(half + 1) * 8, cs_], [], [bwt])
        for ci in range(nco):
            cb, bcb = cbl[ci]
            pp, bpp = pss[n % 2]
            ot, bot = outs[n % 2]
            n += 1
            P.mm(pp[:], ones[0:1, :], bm[0:1, cs_], True, False, [b_ones, bbm], [bpp])
            for k in range(16):
                wt, bwt = wts[k // 8]
                P.mm(pp[:], cb[:, k, :], wt[:, k % 8, :], False, k == 15, [bcb, bwt], [bpp])
            if g in add_one_groups:
                P.ts(ot[:], pp[:], 1.0, None, ALU.add, None, [bpp], [bot])
            else:
                P.cp("act", ot[:], pp[:], [bpp], [bot])
            if mbc is not None:
                P.dma("pool", mbc[ci, :, cs_], ot[:], [bot], [])
            if g in col_groups:
                for j in range(4):
                    P.tr(ptc[:, j * 128:(j + 1) * 128], ot[:, j * 128:(j + 1) * 128], idt[:], [bot, bid], [bptc])
                sl0 = col_groups[g] * 4
                P.cp("dve", mcol[:, ci, sl0:sl0 + 4], ptc[:].rearrange("p (a b) -> p a b", a=4)[:, :, 0], [bptc], [bmcol])
    P.flush()


def emit_ln(P, z, bz, out, bout, g_bc, b_bc, bconst, tmp):
    st, bst, mv, bmv = tmp
    for c4 in range(4):
        P.op("dve", lambda e, c4=c4: e.bn_stats(st[:, c4, :], z[:, c4 * 512:(c4 + 1) * 512]), [bz], [bst])
    P.op("dve", lambda e: e.bn_aggr(mv[:, 0:2], st[:]), [bst], [bmv])
    P.ts(mv[:, 2:3], mv[:, 1:2], EPS, None, ALU.add, None, [bmv], [bmv])
    P.op("act", lambda e: e.sqrt(mv[:, 2:3], mv[:, 2:3]), [bmv], [bmv])
    P.op("dve", lambda e: e.reciprocal(mv[:, 2:3], mv[:, 2:3]), [bmv], [bmv])
    P.ts(out[:], z[:], mv[:, 0:1], mv[:, 2:3], ALU.subtract, ALU.mult, [bz, bmv], [bout])
    P.tt(out[:], out[:], g_bc[:], ALU.mult, [bout, bconst], [bout])
    P.tt(out[:], out[:], b_bc[:], ALU.add, [bout, bconst], [bout])


def build_tokloc(NT, ctx_tile, stop=None, sub=99):
    P = Prog()
    emit_tokloc(P, "", NT, ctx_tile)
    P.begin()
    P.flush(final=True)
    return P.nc


def emit_tokloc(P, pre, NT, ctx_tile, mix=None, xres=None, xout=None, stop=None, sub=99, NE=32, wsrc=None, out_cm=None, xres_cm=None):
    NP = NT * 128
    I = lambda n, s, d=F32: P.dram(pre + n, s, d, kind="ExternalInput")
    mixT = I("mixT", [D, NP], BF16) if mix is None else None
    if xres is None:
        xres = I("xres", [NP, D])
    ccol = I("ccol", [128, 16])
    cccol = I("cccol", [128, 16]) if ctx_tile else None
    wmod = I("wmod", [D, 8192]); bmod = I("bmod", [1, 8192])
    lnp = I("lnp", [4, 128, D])
    wout = I("wout", [D, D])
    wr = I("wr", [D, 36]); brb = I("brb", [128, 36])
    if wsrc is None:
        wg = I("wg", [NE, D, 512]); wu = I("wu", [NE, D, 512]); wd = I("wd", [NE, 512, D])
        b_w = Buf()
    else:
        wg, wu, wd, b_w = wsrc
    ident = I("ident", [128, 128])
    if xout is None:
        xout = P.dram(pre + "xout", [NP, D], F32, kind="ExternalOutput")
    mbc = P.dram(pre + "mbc", [2, 128, 8192], F32)
    x1s = P.dram(pre + "x1s", [NP, D], F32)
    u2Ts = P.dram(pre + "u2Ts", [D, NP], BF16)
    Gs = P.dram(pre + "Gs", [NP, 32], F32)
    ys = P.dram(pre + "ys", [NP, D], F32)
    b_mbc = Buf(); b_x1s = Buf(); b_u2Ts = Buf(); b_Gs = Buf(); b_ys = Buf(); b_wbf = Buf()

    mcol = P.sb([128, 2, 32]); bmcol = Buf()
    cg_ = {4: 0, 5: 1, 6: 2, 7: 3, 8: 4, 9: 5, 10: 6, 11: 7}
    emit_mod_rows(P, [ccol[:, :], cccol[:, :]] if ctx_tile else [ccol[:, :]], wmod, bmod[:, :], 8192, mbc, (8, 9, 10, 11),
                  ident=ident[:, :], mcol=mcol, bmcol=bmcol, col_groups=cg_)
    d = P.dq["pool"]
    K = len(d["sems"])

    def drain_queue_events(q):
        d = P.dq[q]
        K = len(d["sems"])
        evs = []
        for k, s in enumerate(d["sems"]):
            n = (d["i"] - k + K - 1) // K if d["i"] > k else 0
            if n > 0:
                evs.append((s, 16 * n, "dma" + q))
        return evs

    def barrier_dram(bufs):
        evs = drain_queue_events("pool") + drain_queue_events("sp") + drain_queue_events("act")
        for q in ("sp", "pool", "act", "dve", "pe"):
            P._waits(q, [], [], evs)

    barrier_dram([b_mbc])

    P.begin()
    idt = P.sb([128, 128]); bid = Buf()
    P.dma("sp", idt[:], ident[:, :], [], [bid])
    woutb = P.sb([128, 16, D], BF16); bwo = Buf()
    stg = [(P.sb([128, D]), Buf()) for _ in range(2)]
    wov = wout.rearrange("(k p) c -> p k c", p=128)
    for j in range(16):
        s_, bs_ = stg[j % 2]
        P.dma("sp", s_[:], wov[:, j, :], [], [bs_])
        P.cp("act" if j % 2 else "dve", woutb[:, j, :], s_[:], [bs_], [bwo])
    wrs = P.sb([128, 16, 36]); bwr = Buf()
    P.dma("sp", wrs[:], wr.rearrange("(k p) c -> p k c", p=128), [], [bwr])
    brs = P.sb([128, 36]); bbr = Buf()
    P.dma("sp", brs[:], brb[:, :], [], [bbr])
    cA = P.sb([128, 3, D]); bcA = Buf()

    def load_consts_A(ci):
        P.dma("sp", cA[:, 0, :], mbc[ci, :, 0:2048], [], [bcA])
    load_consts_A(0)
    P.dma("sp", cA[:, 1, :], lnp[0], [], [bcA])
    P.dma("sp", cA[:, 2, :], lnp[1], [], [bcA])
    mts = [(P.sb([128, 16, 128], BF16), Buf()) for _ in range(2)]
    xts = [(P.sb([128, D]), Buf()) for _ in range(2)]
    zs = [(P.sb([128, D]), Buf()) for _ in range(2)]
    u2f = [(P.sb([128, 16, 128]), Buf()) for _ in range(1)]
    u2b = [(P.sb([128, 16, 128], BF16), Buf()) for _ in range(2)]
    py = [(P.ps([128, 512]), PBuf()) for _ in range(4)]
    ptr = [(P.ps([128, 512]), PBuf()) for _ in range(2 if mix is not None else 3)]
    prr = (P.ps([128, 64]), PBuf())
    if mix is not None:
        idb = P.sb([128, 128], BF16)
        P.cp("dve", idb[:], idt[:], [bid], [bid])
        mtms = [(P.sb([128, 4, 512], BF16), Buf()) for _ in range(2)]
        for m_, bm_ in mtms:
            P.op("dve", lambda e, m_=m_: e.memset(m_[:], 0.0), [], [bm_])
        pmt = (P.ps([128, 1024], BF16), PBuf())
        CR_ = mix["CR"]; S_ = mix["S"]; SQ_ = mix["SQ"]
        qcache = {}
        nq_ = SQ_ // CR_
        mixmine = P.dram(pre + "mixmine", [NP, 4, 512], BF16)
        bmm = [Buf() for _ in range(nq_ + 1)]
        for cb_ in range(nq_):
            def dynb(e, cb_=cb_):
                if "qoff" not in qcache:
                    qcache["q"] = e.partition_id() % 4
                    qcache["qoff"] = qcache["q"] * nq_
                return e.dma_start(out=mixmine[cb_ * CR_:(cb_ + 1) * CR_, :, :],
                                   in_=Gfull[cb_:cb_ + 3 * nq_ + 1][bass.ds(qcache["qoff"], 1), :, :, :].rearrange("c w r f -> (c w) r f"))
            P.dma_f("sp", dynb, [mix["bG"]], [bmm[cb_]])
        if ctx_tile:
            def dync(e):
                if "qoff" not in qcache:
                    qcache["q"] = e.partition_id() % 4
                    qcache["qoff"] = qcache["q"] * nq_
                return e.dma_start(out=mixmine[SQ_:SQ_ + mix["CQ"], :, :], in_=Glast[bass.ds(qcache["q"] * mix["CQ"], mix["CQ"]), :, :])
            P.dma_f("sp", dync, [mix["bG"]], [bmm[-1]])
        Gfull = mix["G"][0:4 * S_, :].rearrange("(c r w) f -> c w r f", r=4, w=CR_)
        if ctx_tile:
            Glast = mix["G"][4 * S_:4 * S_ + 4 * mix["NCTX"], :].rearrange("(r w) f -> w r f", r=4)
    lnt = (P.sb([128, 4, 6]), Buf(), P.sb([128, 4]), Buf())
    rt = P.sb([128, 128]); brt = Buf()
    lg = P.sb([128, 36]); blg = Buf()
    Gt = [(P.sb([128, 32]), Buf()) for _ in range(2)]
    mixv = mixT.rearrange("(k p) t -> p k t", p=128) if mix is None else None
    u2Tv = u2Ts.rearrange("(k p) t -> p k t", p=128)
    for t in range(NT):
        if ctx_tile and t == NT - 1:
            load_consts_A(1)
        tsl = slice(t * 128, (t + 1) * 128)
        mt, bmt = mts[t % 2]; xt, bxt = xts[t % 2]; z, bz = zs[t % 2]
        uf, buf_ = u2f[0]; ub, bub = u2b[t % 2]; G, bG = Gt[t % 2]
        ci = 1 if (ctx_tile and t == NT - 1) else 0
        if mix is None:
            P.dma("sp", mt[:], mixv[:, :, tsl], [], [bmt])
        else:
            mtm, bmtm = mtms[t % 2]
            if ctx_tile and t == NT - 1:
                P.dma("sp", mtm[0:mix["CQ"]], mixmine[SQ_:SQ_ + mix["CQ"], :, :], [bmm[-1]], [bmtm])
            else:
                P.dma("sp", mtm[:], mixmine[t * 128:(t + 1) * 128, :, :], [bmm[(t * 128) // CR_]], [bmtm])
            for rd in range(2):
                for k8 in range(8):
                    k = rd * 8 + k8
                    r_, kk = divmod(k, 4)
                    P.tr(pmt[0][:, k8 * 128:(k8 + 1) * 128], mtm[:, r_, kk * 128:(kk + 1) * 128], idb[:], [bmtm, bid], [pmt[1]])
                P.cp("act" if rd else "dve", mt[:, rd * 8:(rd + 1) * 8, :], pmt[0][:].rearrange("p (a b) -> p a b", b=128), [pmt[1]], [bmt])
        if xres_cm is None:
            P.dma("sp", xt[:], xres[tsl, :], [], [bxt])
        else:
            xv_ = xres[0:xres_cm * 64, :].rearrange("(c kk) d -> kk c d", kk=xres_cm)
            for hf_ in range(2):
                P.dma("sp", xt[hf_ * 64:(hf_ + 1) * 64, :], xv_[2 * t + hf_], [], [bxt])
        for cg in range(4):
            pp, bpp = py[cg]
            for k in range(16):
                P.mm(pp[:], mt[:, k, :], woutb[:, k, cg * 512:(cg + 1) * 512], k == 0, k == 15, [bmt, bwo], [bpp])
            P.tt(z[:, cg * 512:(cg + 1) * 512], pp[:], cA[:, 0, cg * 512:(cg + 1) * 512], ALU.mult, [bpp, bcA], [bz])
        P.stt(xt[:], xt[:], ALPHA, z[:], ALU.mult, ALU.add, [bxt, bz], [bxt])
        if sub <= 1:
            continue
        x1, bx1 = z, bz
        emit_ln(P, xt, bxt, x1, bx1, cA[:, 1, :], cA[:, 2, :], bcA, lnt)
        P.dma("pool", x1s[tsl, :], x1[:], [bx1], [b_x1s])
        if sub <= 2:
            continue
        for k4 in range(4):
            pp, bpp = ptr[k4 % len(ptr)]
            for kk in range(4):
                k = k4 * 4 + kk
                P.tr(pp[:, kk * 128:(kk + 1) * 128], x1[:, k * 128:(k + 1) * 128], idt[:], [bx1, bid], [bpp])
            for kk in range(4):
                k = k4 * 4 + kk
                P.act(uf[:, k, :], pp[:, kk * 128:(kk + 1) * 128], AF.Identity, [bpp, bmcol], [buf_],
                      bias=mcol[:, ci, k:k + 1], scale=mcol[:, ci, 16 + k:17 + k])
            P.cp("dve", ub[:, k4 * 4:(k4 + 1) * 4, :], uf[:, k4 * 4:(k4 + 1) * 4, :], [buf_], [bub])
        if sub <= 3:
            continue
        P.dma("act", u2Tv[:, :, tsl], ub[:], [bub], [b_u2Ts])
        if sub <= 4:
            continue
        pr, bpr = prr
        for k in range(16):
            P.mm(pr[:, 0:36], uf[:, k, :], wrs[:, k, :], k == 0, k == 15, [buf_, bwr], [bpr])
        P.tt(lg[:], pr[:, 0:36], brs[:], ALU.add, [bpr, bbr], [blg])
        if sub <= 5:
            continue
        R = [brt, blg]
        c = lambda i: rt[:, i:i + 1]
        gl = lg[:, 0:4]
        P.op("dve", lambda e: e.reduce_max(c(0), gl, AX.X), [blg], [brt])
        P.ts(rt[:, 8:12], gl, c(0), None, ALU.is_ge, None, R, [brt])
        P.ts(c(1), c(0), -1.0, None, ALU.mult, None, R, [brt])
        P.act(rt[:, 12:16], gl, AF.Exp, R, [brt], bias=c(1))
        P.op("dve", lambda e: e.reduce_sum(c(2), rt[:, 12:16], AX.X), R, [brt])
        P.op("dve", lambda e: e.reciprocal(c(2), c(2)), R, [brt])
        els = rt[:, 16:24]
        P.ts(els, lg[:, 4:12], c(8), None, ALU.mult, None, R, [brt])
        for g in range(1, 4):
            P.stt(els, lg[:, 4 + 8 * g:12 + 8 * g], c(8 + g), els, ALU.mult, ALU.add, R, [brt])
        P.op("dve", lambda e: e.reduce_max(c(3), els, AX.X), R, [brt])
        mk1 = rt[:, 24:32]
        P.ts(mk1, els, c(3), None, ALU.is_ge, None, R, [brt])
        els2 = rt[:, 32:40]
        P.stt(els2, mk1, NEG, els, ALU.mult, ALU.add, R, [brt])
        P.op("dve", lambda e: e.reduce_max(c(4), els2, AX.X), R, [brt])
        mk2 = rt[:, 40:48]
        P.ts(mk2, els2, c(4), None, ALU.is_ge, None, R, [brt])
        P.tt(c(5), c(4), c(3), ALU.subtract, R, [brt])
        P.act(c(5), c(5), AF.Exp, R, [brt])
        P.ts(c(6), c(5), 1.0, None, ALU.add, None, R, [brt])
        P.op("dve", lambda e: e.reciprocal(c(6), c(6)), R, [brt])
        P.tt(c(7), c(5), c(6), ALU.mult, R, [brt])
        P.tt(c(6), c(6), c(2), ALU.mult, R, [brt])
        P.tt(c(7), c(7), c(2), ALU.mult, R, [brt])
        gsel = rt[:, 48:56]
        P.ts(gsel, mk1, c(6), None, ALU.mult, None, R, [brt])
        P.stt(gsel, mk2, c(7), gsel, ALU.mult, ALU.add, R, [brt])
        for g in range(4):
            P.ts(G[:, 8 * g:8 * g + 8], gsel, c(8 + g), None, ALU.mult, None, R, [bG])
        P.dma("pool", Gs[tsl, :], G[:], [bG], [b_Gs])
    P.flush()
    barrier_dram([])

    P.begin()
    STT = 6
    n_st = (NT + STT - 1) // STT
    u2T = P.sb([128, 16, STT * 128], BF16); bu2T = Buf()
    Gst = P.sb([128, STT, 32]); bGst = Buf()
    acc = [(P.sb([128, D]), Buf()) for _ in range(STT)]
    slots = [(P.sb([128, 8192], BF16), Buf()) for _ in range(4)]
    stg = [(P.sb([128, 2048]), Buf()) for _ in range(4)]
    hT = [(P.sb([128, 4, 512], BF16), Buf()) for _ in range(2)]
    sil = [(P.sb([128, 512]), Buf()) for _ in range(2)]
    pg = [(P.ps([128, 512]), PBuf()) for _ in range(2)]
    pu = [(P.ps([128, 512]), PBuf()) for _ in range(2)]
    pd = [(P.ps([128, 512]), PBuf()) for _ in range(4)]
    wsrc = [wg, wu, wd]
    nslot = 0
    nstg = 0
    nh = 0
    for st in range(n_st):
        t0 = st * STT
        nt = min(STT, NT - t0)
        ntok = nt * 128
        P.dma("sp", u2T[:, :, 0:ntok], u2Tv[:, :, t0 * 128:t0 * 128 + ntok], [b_u2Ts], [bu2T])
        P.dma("sp", Gst[:, 0:nt, :], Gs[t0 * 128:t0 * 128 + ntok, :].rearrange("(t p) e -> p t e", p=128), [b_Gs], [bGst])
        for e_ in range(NE):
            mats = []
            for mi in range(3):
                sl, bsl = slots[nslot % 4]
                nslot += 1
                if mi < 2:
                    sv = sl[:].rearrange("p (k f) -> p k f", k=16)
                    srcv = wsrc[mi][e_].rearrange("(k p) f -> p k f", p=128)
                    pieces = [(sv[:, 4 * j:4 * j + 4, :], srcv[:, 4 * j:4 * j + 4, :], 4) for j in range(4)]
                else:
                    sv = sl[:].rearrange("p (k c) -> p k c", k=4)
                    srcv = wsrc[mi][e_].rearrange("(k p) c -> p k c", p=128)
                    pieces = [(sv[:, j:j + 1, :], srcv[:, j:j + 1, :], 1) for j in range(4)]
                for j, (dv, sv_, a) in enumerate(pieces):
                    sg_, bsg_ = stg[nstg % 4]
                    nstg += 1
                    P.dma("sp", sg_[:].rearrange("p (a b) -> p a b", a=a), sv_, [b_w], [bsg_])
                    P.cp("act" if (nstg % 2) else "dve", dv, sg_[:].rearrange("p (a b) -> p a b", a=a), [bsg_], [bsl])
                mats.append((sv, bsl))
            (Wg, bWg), (Wu, bWu), (Wd, bWd) = mats
            for tg in range(0, ntok, 512):
                n = min(512, ntok - tg)
                h, bh = hT[nh % 2]
                nh += 1
                for f in range(4):
                    g_, bg_ = pg[f % 2]; u_, bu_ = pu[f % 2]; s_, bs_ = sil[f % 2]
                    for k in range(16):
                        P.mm(g_[:, 0:n], Wg[:, k, f * 128:(f + 1) * 128], u2T[:, k, tg:tg + n], k == 0, k == 15, [bWg, bu2T], [bg_])
                    for k in range(16):
                        P.mm(u_[:, 0:n], Wu[:, k, f * 128:(f + 1) * 128], u2T[:, k, tg:tg + n], k == 0, k == 15, [bWu, bu2T], [bu_])
                    P.act(s_[:, 0:n], g_[:, 0:n], AF.Silu, [bg_], [bs_])
                    P.tt(h[:, f, 0:n], s_[:, 0:n], u_[:, 0:n], ALU.mult, [bs_, bu_], [bh])
                for ti in range(n // 128):
                    tl = (tg // 128) + ti
                    a_, ba_ = acc[tl]
                    for cg in range(4):
                        pp, bpp = pd[cg]
                        for f in range(4):
                            P.mm(pp[:], h[:, f, ti * 128:(ti + 1) * 128], Wd[:, f, cg * 512:(cg + 1) * 512], f == 0, f == 3, [bh, bWd], [bpp])
                        cs_ = slice(cg * 512, (cg + 1) * 512)
                        if e_ == 0:
                            P.ts(a_[:, cs_], pp[:], Gst[:, tl, 0:1], None, ALU.mult, None, [bpp, bGst], [ba_])
                        else:
                            P.stt(a_[:, cs_], pp[:], Gst[:, tl, e_:e_ + 1], a_[:, cs_], ALU.mult, ALU.add, [bpp, bGst, ba_], [ba_])
        for tl in range(nt):
            a_, ba_ = acc[tl]
            P.dma("pool", ys[(t0 + tl) * 128:(t0 + tl + 1) * 128, :], a_[:], [ba_], [b_ys])
    P.flush()
    barrier_dram([])

    P.begin()
    cC = P.sb([128, 3, D]); bcC = Buf()

    def load_consts_C(ci):
        P.dma("sp", cC[:, 0, :], mbc[ci, :, 6144:8192], [], [bcC])
        P.dma("sp", cC[:, 1, :], lnp[2], [], [bcC])
        P.dma("sp", cC[:, 2, :], lnp[3], [], [bcC])
    load_consts_C(0)
    xa = [(P.sb([128, D]), Buf()) for _ in range(2)]
    ya = [(P.sb([128, D]), Buf()) for _ in range(2)]
    oa = [(P.sb([128, D]), Buf()) for _ in range(2)]
    lnt = (P.sb([128, 4, 6]), Buf(), P.sb([128, 4]), Buf())
    for t in range(NT):
        if ctx_tile and t == NT - 1:
            load_consts_C(1)
        tsl = slice(t * 128, (t + 1) * 128)
        x1, bx1 = xa[t % 2]; y, by = ya[t % 2]; o, bo = oa[t % 2]
        P.dma("sp", x1[:], x1s[tsl, :], [b_x1s], [bx1])
        P.dma("sp", y[:], ys[tsl, :], [b_ys], [by])
        P.tt(y[:], y[:], cC[:, 0, :], ALU.mult, [by, bcC], [by])
        P.stt(y[:], x1[:], ALPHA, y[:], ALU.mult, ALU.add, [bx1, by], [by])
        emit_ln(P, y, by, o, bo, cC[:, 1, :], cC[:, 2, :], bcC, lnt)
        if out_cm is None or (ctx_tile and t == NT - 1):
            P.dma("pool", xout[tsl, :], o[:], [bo], [])
        else:
            ov_ = xout[0:out_cm * 64, :].rearrange("(c kk) d -> kk c d", kk=out_cm)
            for hf_ in range(2):
                P.dma("pool", ov_[2 * t + hf_], o[hf_ * 64:(hf_ + 1) * 64, :], [bo], [])
    P.flush()


def emit_projection(P, xin, T, n_ctx_tiles, win, ncols, mcol, bmcol, ident, tm_cols, tokmaj, fm_specs, load_x=None):
    P.begin()
    idt = P.sb([128, 128]); bid = Buf()
    P.dma("sp", idt[:], ident, [], [bid])
    Wb = P.sb([128, 16, ncols], BF16); bW = Buf()
    stg = [(P.sb([128, ncols]), Buf()) for _ in range(2)]
    wv = win.rearrange("(k p) c -> p k c", p=128)
    for k in range(16):
        s_, bs_ = stg[k % 2]
        P.dma("sp", s_[:], wv[:, k, :], [], [bs_])
        P.cp("act" if k % 2 else "dve", Wb[:, k, :], s_[:], [bs_], [bW])
    xts = [(P.sb([128, D]), Buf()) for _ in range(2)]
    uTs = [(P.sb([128, 16, 128], BF16), Buf()) for _ in range(2)]
    ptr = [(P.ps([128, 512]), PBuf()) for _ in range(2)]
    ptm = [(P.ps([128, 512]), PBuf()) for _ in range(3)]
    pfm = [(P.ps([128, 512]), PBuf()) for _ in range(2)]
    tms = [(P.sb([128, tm_cols]), Buf()) for _ in range(2)]
    fms = [(P.sb([128, 4, 128]), Buf()) for _ in range(2)]
    nfm = 0
    for t in range(T // 128):
        ci = 1 if t < n_ctx_tiles else 0
        tsl = slice(t * 128, (t + 1) * 128)
        xt, bxt = xts[t % 2]; uT, buT = uTs[t % 2]; tm, btm = tms[t % 2]
        if load_x is None:
            P.dma("sp", xt[:], xin[tsl, :], [], [bxt])
        else:
            load_x(t, xt, bxt)
        for k4 in range(4):
            pp, bpp = ptr[k4 % 2]
            for kk in range(4):
                k = k4 * 4 + kk
                P.tr(pp[:, kk * 128:(kk + 1) * 128], xt[:, k * 128:(k + 1) * 128], idt[:], [bxt, bid], [bpp])
            for kk in range(4):
                k = k4 * 4 + kk
                if k4 % 2 == 0:
                    P.act(uT[:, k, :], pp[:, kk * 128:(kk + 1) * 128], AF.Identity, [bpp, bmcol], [buT],
                          bias=mcol[:, ci, k:k + 1], scale=mcol[:, ci, 16 + k:17 + k])
                else:
                    P.ts(uT[:, k, :], pp[:, kk * 128:(kk + 1) * 128], mcol[:, ci, 16 + k:17 + k], mcol[:, ci, k:k + 1],
                         ALU.mult, ALU.add, [bpp, bmcol], [buT])
        nb = (tm_cols + 511) // 512
        for b_ in range(nb):
            c0 = b_ * 512
            w = min(512, tm_cols - c0)
            pp, bpp = ptm[b_ % 3]
            for k in range(16):
                P.mm(pp[:, 0:w], uT[:, k, :], Wb[:, k, c0:c0 + w], k == 0, k == 15, [buT, bW], [bpp])
            P.cp("act" if b_ % 2 else "dve", tm[:, c0:c0 + w], pp[:, 0:w], [bpp], [btm])
        P.dma("pool", tokmaj[tsl, :], tm[:], [btm], [])
        for j0 in range(0, len(fm_specs), 4):
            grp = fm_specs[j0:j0 + 4]
            pp, bpp = pfm[nfm % 2]; fm, bfm = fms[nfm % 2]
            nfm += 1
            for j, (c0, w, dst) in enumerate(grp):
                for k in range(16):
                    P.mm(pp[0:w, j * 128:(j + 1) * 128], Wb[:, k, c0:c0 + w], uT[:, k, :], k == 0, k == 15, [bW, buT], [bpp])
            P.cp("act" if nfm % 2 else "dve", fm[:, 0:len(grp), :], pp[:, 0:len(grp) * 128].rearrange("p (a b) -> p a b", b=128), [bpp], [bfm])
            for j, (c0, w, dst) in enumerate(grp):
                P.dma("act", dst[:, tsl], fm[0:w, j, :], [bfm], [])
    P.flush()


def all_dma_barrier(P):
    evs = []
    for q in ("sp", "pool", "act"):
        d = P.dq[q]
        K = len(d["sems"])
        for k, s in enumerate(d["sems"]):
            n = (d["i"] - k + K - 1) // K if d["i"] > k else 0
            if n > 0:
                evs.append((s, 16 * n, "dma" + q))
    for q in ("sp", "pool", "act", "dve", "pe"):
        P._waits(q, [], [], evs)


def emit_finish(P, T, nh, tokmaj, gate_c0, oF, oB, gn_bc, gate_func, ident, mixT, out_tm=None, t_start=0):
    P.begin()
    idt = P.sb([128, 128]); bid = Buf()
    P.dma("sp", idt[:], ident, [], [bid])
    gn = P.sb([128, 256]); bgn = Buf()
    P.dma("sp", gn[:], gn_bc, [], [bgn])
    A = [(P.sb([128, 256]), Buf()) for _ in range(2)]
    Bt = [(P.sb([128, 256]), Buf()) for _ in range(2)]
    Gt = [(P.sb([128, 256]), Buf()) for _ in range(2)]
    sq = P.sb([128, 256]); bsq = Buf()
    sc = P.sb([128, 4]); bsc = Buf()
    pt = [(P.ps([128, 512]), PBuf()) for _ in range(2)]
    ot = [(P.sb([128, 2, 128], BF16), Buf()) for _ in range(2)]
    n = 0
    otm = [(P.sb([128, 256], BF16), Buf()) for _ in range(2)]
    for t in range(t_start, T // 128):
        tsl = slice(t * 128, (t + 1) * 128)
        for h in range(nh):
            a, ba = A[n % 2]; b, bb = Bt[n % 2]; g, bg = Gt[n % 2]; pp, bpp = pt[n % 2]; o, bo = ot[n % 2]
            o2, bo2 = otm[n % 2]
            n += 1
            P.dma("sp", a[:], oF[h][tsl, :], [], [ba])
            P.dma("sp", b[:], oB[h][tsl, :], [], [bb])
            P.dma("sp", g[:], tokmaj[tsl, gate_c0 + h * 256:gate_c0 + (h + 1) * 256], [], [bg])
            P.tt(a[:], a[:], b[:], ALU.add, [ba, bb], [ba])
            P.tt(sq[:], a[:], a[:], ALU.mult, [ba], [bsq])
            P.op("dve", lambda e: e.reduce_sum(sc[:, 0:1], sq[:], AX.X), [bsq], [bsc])
            P.ts(sc[:, 0:1], sc[:, 0:1], 1.0 / 256, EPS, ALU.mult, ALU.add, [bsc], [bsc])
            P.op("act", lambda e: e.sqrt(sc[:, 0:1], sc[:, 0:1]), [bsc], [bsc])
            P.op("dve", lambda e: e.reciprocal(sc[:, 0:1], sc[:, 0:1]), [bsc], [bsc])
            P.stt(a[:], a[:], sc[:, 0:1], gn[:], ALU.mult, ALU.mult, [ba, bsc, bgn], [ba])
            P.act(g[:], g[:], gate_func, [bg], [bg])
            if out_tm is not None:
                P.tt(o2[:], a[:], g[:], ALU.mult, [ba, bg], [bo2])
                out_tm(t, h, o2, bo2)
                continue
            P.tt(a[:], a[:], g[:], ALU.mult, [ba, bg], [ba])
            for j in range(2):
                P.tr(pp[:, j * 128:(j + 1) * 128], a[:, j * 128:(j + 1) * 128], idt[:], [ba, bid], [bpp])
            P.cp("act", o[:], pp[:, 0:256].rearrange("p (a b) -> p a b", b=128), [bpp], [bo])
            for j in range(2):
                P.dma("act", mixT[h * 256 + j * 128:h * 256 + (j + 1) * 128, tsl], o[:, j, :], [bo], [])
    P.flush()


def build_evenmix(TL, NCTX=256):
    P = Prog()
    emit_evenmix(P, "", TL, NCTX)
    P.begin()
    P.flush(final=True)
    return P.nc


def emit_evenmix(P, pre, TL, NCTX=256, mixloc=None):
    T = NCTX + TL
    I = lambda n, s, d=F32: P.dram(pre + n, s, d, kind="ExternalInput")
    xin = I("xin", [T, D]); ccol = I("ccol", [128, 16]); cccol = I("cccol", [128, 16])
    wmod = I("wmod", [D, 4096]); bmod = I("bmod", [1, 4096])
    win = I("win", [D, 1312])
    wa2 = I("wa2", [2, 16, 128]); barow = I("barow", [2, 1, 128])
    gn_bc = I("gn_bc", [128, 256])
    convw = I("convw", [128, 2, 4]); convb = I("convb", [128, 2])
    wr_ = I("wr_", [2, 2, 128, 128]); wi_ = I("wi_", [2, 2, 128, 128])
    brc = I("brc", [128, 2, 2]); bic = I("bic", [128, 2, 2]); lamc = I("lamc", [128, 2, 2])
    ident = I("ident", [128, 128])
    cm = I("cm", [6, 64, 64])
    mixT = P.dram(pre + "mixT", [512, T], BF16, kind="ExternalOutput") if mixloc is None else None
    tokmaj = P.dram(pre + "tokmaj", [T, 640], F32)
    qT = P.dram(pre + "qT", [128, T], F32); kT = P.dram(pre + "kT", [128, T], F32)
    xrT = [P.dram(pre + "xrT%d" % i, [128, T], F32) for i in range(2)]
    xgT = [P.dram(pre + "xgT%d" % i, [128, T], F32) for i in range(2)]
    afT = P.dram(pre + "afT", [16, T], F32); abT = P.dram(pre + "abT", [16, T], F32)
    hF = [P.dram(pre + "hF%d" % i, [128, T], F32) for i in range(2)]
    oF = P.dram(pre + "oF", [T, 256], F32); oB = P.dram(pre + "oB", [T, 256], F32)

    mcol = P.sb([128, 2, 32]); bmcol = Buf()
    emit_mod_rows(P, [ccol[:, :], cccol[:, :]], wmod, bmod[:, :], 4096, None, (4, 5, 6, 7), ident=ident[:, :], mcol=mcol, bmcol=bmcol,
                  col_groups={g: g for g in range(8)})
    fm = [(640, 128, qT), (0, 128, kT), (768, 128, xrT[0]), (896, 128, xrT[1]), (1024, 128, xgT[0]), (1152, 128, xgT[1]),
          (1280, 16, afT), (1296, 16, abT)]
    emit_projection(P, xin, T, NCTX // 128, win, 1312, mcol, bmcol, ident[:, :], 640, tokmaj, fm)
    all_dma_barrier(P)

    P.begin()
    cw = P.sb([128, 2, 4]); cb = P.sb([128, 2]); bcw = Buf()
    P.dma("sp", cw[:], convw[:, :, :], [], [bcw]); P.dma("sp", cb[:], convb[:, :], [], [bcw])
    Wr = P.sb([128, 4, 128]); Wi = P.sb([128, 4, 128]); bWg = Buf()
    for d_ in range(2):
        for bl in range(2):
            P.dma("sp", Wr[:, d_ * 2 + bl, :], wr_[d_, bl], [], [bWg])
            P.dma("sp", Wi[:, d_ * 2 + bl, :], wi_[d_, bl], [], [bWg])
    gb = P.sb([128, 3, 4]); bgb = Buf()
    P.dma("sp", gb[:, 0, :], brc.rearrange("p a b -> p (a b)"), [], [bgb])
    P.dma("sp", gb[:, 1, :], bic.rearrange("p a b -> p (a b)"), [], [bgb])
    P.dma("sp", gb[:, 2, :], lamc.rearrange("p a b -> p (a b)"), [], [bgb])
    P.act(gb[:, 2, :], gb[:, 2, :], AF.Sigmoid, [bgb], [bgb])
    P.act(gb[:, 2, :], gb[:, 2, :], AF.Ln, [bgb], [bgb])
    P.ts(gb[:, 2, :], gb[:, 2, :], 8.0, None, ALU.mult, None, [bgb], [bgb])
    xh = [(P.sb([128, 516]), Buf()) for _ in range(2)]
    xc = P.sb([128, 512]); bxc = Buf()
    pr = (P.ps([128, 512]), PBuf()); pi = (P.ps([128, 512]), PBuf())
    r = P.sb([128, 512]); br = Buf(); i_ = P.sb([128, 512]); bi = Buf()
    a = P.sb([128, 512]); ba = Buf(); s = P.sb([128, 512]); bs = Buf()
    hh = [(P.sb([128, 512]), Buf()) for _ in range(2)]
    hst = P.sb([128, 1]); bhst = Buf()
    hf = [(P.sb([128, 512]), Buf()) for _ in range(2)]
    xg = [(P.sb([128, 512]), Buf()) for _ in range(2)]
    yo = [(P.sb([128, 512], BF16), Buf()) for _ in range(2)]
    if mixloc is not None:
        idl = P.sb([128, 128]); bidl = Buf()
        P.dma("sp", idl[:], ident[:, :], [], [bidl])
        yf = P.sb([128, 512]); byf = Buf()
        pyt = (P.ps([128, 512]), PBuf())
        yT = [(P.sb([128, 4, 128], BF16), Buf()) for _ in range(2)]
    seqs = [(0, NCTX)] + [(NCTX + g * 512, min(512, TL - g * 512)) for g in range((TL + 511) // 512)]
    n = 0
    for bl in range(2):
        for d_ in range(2):
            gi = d_ * 2 + bl
            P.op("dve", lambda e: e.memset(hst[:], 0.0), [], [bhst])
            order = seqs if d_ == 0 else [seqs[0]] + seqs[:0:-1]
            for (g0, gn_) in order:
                s0, s1 = (0, NCTX) if g0 < NCTX else (NCTX, T)
                x_, bx_ = xh[n % 2]; h_, bh_ = hh[n % 2]; hf_, bhf_ = hf[n % 2]; xg_, bxg_ = xg[n % 2]; y_, by_ = yo[n % 2]
                n += 1
                lo = max(g0 - 2, s0); hi = min(g0 + gn_ + 1, s1)
                P.op("dve", lambda e, x_=x_: e.memset(x_[:], 0.0), [], [bx_])
                P.dma("sp", x_[:, lo - (g0 - 2):hi - (g0 - 2)], xrT[bl][:, lo:hi], [], [bx_])
                P.ts(xc[:, 0:gn_], x_[:, 0:gn_], cw[:, bl, 0:1], cb[:, bl:bl + 1], ALU.mult, ALU.add, [bx_, bcw], [bxc])
                for j in range(1, 4):
                    P.stt(xc[:, 0:gn_], x_[:, j:j + gn_], cw[:, bl, j:j + 1], xc[:, 0:gn_], ALU.mult, ALU.add, [bx_, bcw, bxc], [bxc])
                P.mm(pr[0][:, 0:gn_], Wr[:, gi, :], xc[:, 0:gn_], True, True, [bWg, bxc], [pr[1]])
                P.mm(pi[0][:, 0:gn_], Wi[:, gi, :], xc[:, 0:gn_], True, True, [bWg, bxc], [pi[1]])
                P.act(r[:, 0:gn_], pr[0][:, 0:gn_], AF.Sigmoid, [pr[1], bgb], [br], bias=gb[:, 0, gi:gi + 1])
                P.act(i_[:, 0:gn_], pi[0][:, 0:gn_], AF.Sigmoid, [pi[1], bgb], [bi], bias=gb[:, 1, gi:gi + 1])
                P.act(a[:, 0:gn_], r[:, 0:gn_], AF.Exp, [br, bgb], [ba], scale=gb[:, 2, gi:gi + 1])
                P.tt(s[:, 0:gn_], a[:, 0:gn_], a[:, 0:gn_], ALU.mult, [ba], [bs])
                P.act(s[:, 0:gn_], s[:, 0:gn_], AF.Sqrt, [bs], [bs], bias=1.0, scale=-1.0)
                P.tt(i_[:, 0:gn_], i_[:, 0:gn_], xc[:, 0:gn_], ALU.mult, [bi, bxc], [bi])
                P.tt(i_[:, 0:gn_], i_[:, 0:gn_], s[:, 0:gn_], ALU.mult, [bi, bs], [bi])
                if d_ == 0:
                    P.op("dve", lambda e, h_=h_, gn_=gn_: e.tensor_tensor_scan(h_[:, 0:gn_], a[:, 0:gn_], i_[:, 0:gn_], hst[:, 0:1], ALU.mult, ALU.add),
                         [ba, bi, bhst], [bh_])
                    P.cp("dve", hst[:], h_[:, gn_ - 1:gn_], [bh_], [bhst])
                    P.dma("pool", hF[bl][:, g0:g0 + gn_], h_[:, 0:gn_], [bh_], [])
                else:
                    P.op("dve", lambda e, h_=h_, gn_=gn_: e.tensor_tensor_scan(h_[:, 0:gn_][:, ::-1], a[:, 0:gn_][:, ::-1], i_[:, 0:gn_][:, ::-1], hst[:, 0:1], ALU.mult, ALU.add),
                         [ba, bi, bhst], [bh_])
                    P.cp("dve", hst[:], h_[:, 0:1], [bh_], [bhst])
                    P.dma("sp", hf_[:, 0:gn_], hF[bl][:, g0:g0 + gn_], [], [bhf_])
                    P.dma("sp", xg_[:, 0:gn_], xgT[bl][:, g0:g0 + gn_], [], [bxg_])
                    P.tt(h_[:, 0:gn_], h_[:, 0:gn_], hf_[:, 0:gn_], ALU.add, [bh_, bhf_], [bh_])
                    P.act(xg_[:, 0:gn_], xg_[:, 0:gn_], AF.Gelu_apprx_tanh, [bxg_], [bxg_])
                    if mixloc is None:
                        P.tt(y_[:, 0:gn_], h_[:, 0:gn_], xg_[:, 0:gn_], ALU.mult, [bh_, bxg_], [by_])
                        P.dma("pool", mixT[256 + bl * 128:256 + (bl + 1) * 128, g0:g0 + gn_], y_[:, 0:gn_], [by_], [])
                    else:
                        yT_, byT_ = yT[n % 2]
                        nb_ = gn_ // 128
                        P.tt(yf[:, 0:gn_], h_[:, 0:gn_], xg_[:, 0:gn_], ALU.mult, [bh_, bxg_], [byf])
                        for j in range(nb_):
                            P.tr(pyt[0][:, j * 128:(j + 1) * 128], yf[:, j * 128:(j + 1) * 128], idl[:], [byf, bidl], [pyt[1]])
                        P.cp("act", yT_[:, 0:nb_, :], pyt[0][:, 0:gn_].rearrange("p (a b) -> p a b", b=128), [pyt[1]], [byT_])
                        mr0 = (TL + g0) if g0 < NCTX else (g0 - NCTX)
                        P.dma("pool", mixloc[mr0:mr0 + gn_, 256 + bl * 128:256 + (bl + 1) * 128].rearrange("(j p) c -> p j c", p=128),
                              yT_[:, 0:nb_, :], [byT_], [])
            if d_ == 0:
                all_dma_barrier(P)
    P.flush()
    all_dma_barrier(P)

    P.begin()
    cms = P.sb([64, 6, 64]); bcm = Buf()
    P.dma("sp", cms[:], cm.rearrange("a s t -> s a t"), [], [bcm])
    ones = P.sb([1, 64]); P.op("dve", lambda e: e.memset(ones[:], 1.0), [], [bcm])
    wa = P.sb([16, 2, 128]); bwa = Buf()
    P.dma("sp", wa[:], wa2.rearrange("d r c -> r d c"), [], [bwa])
    bar = P.sb([1, 2, 128])
    P.dma("sp", bar[:], barow.rearrange("d o c -> o d c"), [], [bwa])
    qs = [(P.sb([128, 512]), Buf()) for _ in range(2)]
    ks = [(P.sb([128, 512]), Buf()) for _ in range(2)]
    as_ = [(P.sb([16, 512]), Buf()) for _ in range(2)]
    tk = [(P.sb([64, 8, 640]), Buf()) for _ in range(2)]
    pla = (P.ps([64, 128]), PBuf()); pb = (P.ps([128, 64]), PBuf()); pdd = (P.ps([64, 128]), PBuf())
    pat = (P.ps([64, 64]), PBuf()); po = (P.ps([64, 256]), PBuf()); pS = (P.ps([128, 256]), PBuf())
    la = P.sb([64, 128]); bla = Buf()
    e1 = P.sb([128, 64]); be1 = Buf(); e2 = P.sb([128, 64]); be2 = Buf(); e3 = P.sb([64, 128]); be3 = Buf()
    qd = P.sb([128, 64], BF16); bqd = Buf(); ki = P.sb([128, 64], BF16); bki = Buf(); ke = P.sb([64, 128], BF16); bke = Buf()
    vb = P.sb([64, 256], BF16); bvb = Buf(); am = P.sb([64, 64], BF16); bam = Buf()
    S = P.sb([128, 256]); bS = Buf(); Sb = P.sb([128, 256], BF16); bSb = Buf()
    ost = [(P.sb([64, 256]), Buf()) for _ in range(2)]
    sgroups = [(0, NCTX)] + [(NCTX + g * 512, min(512, TL - g * 512)) for g in range((TL + 511) // 512)]
    n = 0
    nc_ = 0
    for d_ in range(2):
        aT = afT if d_ == 0 else abT
        oD = oF if d_ == 0 else oB
        P.op("dve", lambda e: e.memset(S[:], 0.0), [], [bS])
        P.op("dve", lambda e: e.memset(Sb[:], 0.0), [], [bSb])
        order = sgroups if d_ == 0 else [sgroups[0]] + sgroups[:0:-1]
        for (g0, gn_) in order:
            q_, bq_ = qs[n % 2]; k_, bk_ = ks[n % 2]; a_, ba_ = as_[n % 2]; t_, bt_ = tk[n % 2]
            n += 1
            nch = gn_ // 64
            P.dma("sp", q_[:, 0:gn_], qT[:, g0:g0 + gn_], [], [bq_])
            P.dma("sp", k_[:, 0:gn_], kT[:, g0:g0 + gn_], [], [bk_])
            P.dma("sp", a_[:, 0:gn_], aT[:, g0:g0 + gn_], [], [ba_])
            P.dma("sp", t_[:, 0:nch, :], tokmaj[g0:g0 + gn_, :].rearrange("(c p) f -> p c f", p=64), [], [bt_])
            chunks = range(nch) if d_ == 0 else range(nch - 1, -1, -1)
            for c in chunks:
                cs = slice(c * 64, (c + 1) * 64)
                P.mm(pla[0][:], a_[:, cs], wa[:, d_, :], True, False, [ba_, bwa], [pla[1]])
                P.mm(pla[0][:], ones[0:1, :], bar[0:1, d_, :], False, True, [bcm, bwa], [pla[1]])
                P.act(la[:], pla[0][:], AF.Sigmoid, [pla[1]], [bla])
                P.act(la[:], la[:], AF.Ln, [bla], [bla])
                P.mm(pb[0][:], la[:], cms[:, 3 * d_ + 0, :], True, True, [bla, bcm], [pb[1]])
                P.mm(pdd[0][:], cms[:, 3 * d_ + 1, :], la[:], True, True, [bla, bcm], [pdd[1]])
                P.act(e1[:], pb[0][:], AF.Exp, [pb[1]], [be1])
                P.act(e2[:], pb[0][:], AF.Exp, [pb[1]], [be2], scale=-1.0)
                P.act(e3[:], pdd[0][:], AF.Exp, [pdd[1]], [be3])
                P.stt(qd[:], q_[:, cs], 128.0 ** -0.5, e1[:], ALU.mult, ALU.mult, [bq_, be1], [bqd])
                P.tt(ki[:], k_[:, cs], e2[:], ALU.mult, [bk_, be2], [bki])
                P.tt(ke[:], t_[:, c, 0:128], e3[:], ALU.mult, [bt_, be3], [bke])
                P.cp("act", vb[:], t_[:, c, 128:384], [bt_], [bvb])
                P.mm(pat[0][:], ki[:], qd[:], True, True, [bki, bqd], [pat[1]])
                P.tt(am[:], pat[0][:], cms[:, 3 * d_ + 2, :], ALU.mult, [pat[1], bcm], [bam])
                P.mm(po[0][:], am[:], vb[:], True, False, [bam, bvb], [po[1]])
                P.mm(po[0][:], qd[:], Sb[:], False, True, [bqd, bSb], [po[1]])
                P.mm(pS[0][:], ke[:], vb[:], True, True, [bke, bvb], [pS[1]])
                el = e1[:, 63:64] if d_ == 0 else e1[:, 0:1]
                P.stt(S[:], S[:], el, pS[0][:], ALU.mult, ALU.add, [bS, be1, pS[1]], [bS])
                P.cp("act", Sb[:], S[:], [bS], [bSb])
                o_, bo_ = ost[nc_ % 2]
                nc_ += 1
                P.cp("act", o_[:], po[0][:], [po[1]], [bo_])
                P.dma("pool", oD[g0 + c * 64:g0 + (c + 1) * 64, :], o_[:], [bo_], [])
    P.flush()
    all_dma_barrier(P)
    out_tm = None
    if mixloc is not None:
        def out_tm(t, h, o2, bo2):
            mr0 = (TL + t * 128) if t < NCTX // 128 else (t * 128 - NCTX)
            P.dma("pool", mixloc[mr0:mr0 + 128, 0:256], o2[:], [bo2], [])
    emit_finish(P, T, 1, tokmaj, 384, [oF], [oB], gn_bc[:, :], AF.Silu, ident[:, :], mixT, out_tm=out_tm)


def build_oddmix(TL, NCTX=256):
    P = Prog()
    emit_oddmix(P, "", TL, NCTX)
    P.begin()
    P.flush(final=True)
    return P.nc


def emit_oddmix(P, pre, TL, NCTX=256, xsrc=None, mixloc=None):
    T = NCTX + TL
    GW = 64
    rows = TL // GW
    I = lambda n, s, d=F32: P.dram(pre + n, s, d, kind="ExternalInput")
    xin = I("xin", [T, D]) if xsrc is None else None
    ccol = I("ccol", [128, 16]); cccol = I("cccol", [128, 16])
    wmod = I("wmod", [D, 4096]); bmod = I("bmod", [1, 4096])
    win = I("win", [D, 1544])
    bgb = I("bgb", [64, 2, 4]); gn_bc = I("gn_bc", [128, 256])
    ident = I("ident", [128, 128])
    cm = I("cm", [10, 64, 64])
    mixT = P.dram(pre + "mixT", [512, T], BF16, kind="ExternalOutput") if mixloc is None else None
    tokmaj = P.dram(pre + "tokmaj", [T, 1288], F32)
    qT = [P.dram(pre + "qT%d" % i, [128, T], F32) for i in range(2)]
    kT = [P.dram(pre + "kT%d" % i, [128, T], F32) for i in range(2)]
    hF = [P.dram(pre + "hF%d" % i, [T, 256], F32) for i in range(2)]
    hB = [P.dram(pre + "hB%d" % i, [T, 256], F32) for i in range(2)]
    load_x = None
    if xsrc is not None:
        G = xsrc["G"]; SQ = TL // 4; CQ = NCTX // 4; RQ = rows // 4
        G1v = G.rearrange("(tl r w) d -> tl r w d", r=4, w=128)

        def load_x(t, xt, bxt):
            if t < NCTX // 128:
                for hf_ in range(128 // CQ):
                    r = t * (128 // CQ) + hf_
                    P.dma("sp", xt[hf_ * CQ:(hf_ + 1) * CQ, :], G1v[SQ // 128, r, 0:CQ, :], [xsrc["bG"]], [bxt])
            else:
                j0 = (t - NCTX // 128) * 128
                pos = j0
                while pos < j0 + 128:
                    c_, rw = divmod(pos, rows)
                    r, lr = divmod(rw, RQ)
                    n_ = min(RQ - lr, j0 + 128 - pos)
                    ip = c_ * RQ + lr
                    P.dma("sp", xt[pos - j0:pos - j0 + n_, :], G1v[ip // 128, r, ip % 128:ip % 128 + n_, :], [xsrc["bG"]], [bxt])
                    pos += n_
    mcol = P.sb([128, 2, 32]); bmcol = Buf()
    emit_mod_rows(P, [ccol[:, :], cccol[:, :]], wmod, bmod[:, :], 4096, None, (4, 5, 6, 7), ident=ident[:, :], mcol=mcol, bmcol=bmcol,
                  col_groups={g: g for g in range(8)})
    fm = [(1288, 128, qT[0]), (1416, 128, qT[1]), (0, 128, kT[0]), (128, 128, kT[1])]
    emit_projection(P, xin, T, NCTX // 128, win, 1544, mcol, bmcol, ident[:, :], 1288, tokmaj, fm, load_x=load_x)
    all_dma_barrier(P)

    P.begin()
    SC = 128.0 ** -0.5
    cms = P.sb([64, 10, 64]); bcm = Buf()
    P.dma("sp", cms[:], cm.rearrange("a s t -> s a t"), [], [bcm])
    ones = P.sb([64, 128]); P.op("dve", lambda e: e.memset(ones[:], 1.0), [], [bcm])
    bg = P.sb([64, 2, 4]); P.dma("sp", bg[:], bgb[:, :, :], [], [bcm])
    qs = [(P.sb([128, 512]), Buf()) for _ in range(2)]
    ks = [(P.sb([128, 512]), Buf()) for _ in range(2)]
    qb = [(P.sb([128, 512], BF16), Buf()) for _ in range(2)]
    kb = [(P.sb([128, 512], BF16), Buf()) for _ in range(2)]
    tk = [(P.sb([64, 8, 1288]), Buf()) for _ in range(2)]
    pD = (P.ps([64, 64]), PBuf()); pG = (P.ps([128, 64]), PBuf()); pc = (P.ps([128, 8]), PBuf())
    pQK = (P.ps([64, 64]), PBuf()); pT = (P.ps([64, 64]), PBuf()); pN = (P.ps([64, 257]), PBuf())
    pQC = (P.ps([64, 257]), PBuf()); pC = (P.ps([128, 257]), PBuf())
    gc = P.sb([64, 8]); bgc = Buf()
    A1 = P.sb([64, 64]); bA1 = Buf(); Fb = P.sb([64, 128]); bFb = Buf(); Ib = P.sb([64, 128]); bIb = Buf()
    Dk = P.sb([64, 64]); bDk = Buf(); wi = P.sb([64, 64]); bwi = Buf(); sm = P.sb([64, 64]); bsm = Buf()
    smT = P.sb([64, 64], BF16); bsmT = Buf()
    v = P.sb([128, 16]); bv = Buf()
    va = [(P.sb([64, 257], BF16), Buf()) for _ in range(2)]
    for va_, bva_ in va:
        P.op("dve", lambda e, va_=va_: e.memset(va_[:], 1.0), [], [bva_])
    nA = P.sb([64, 257]); bnA = Buf(); tot = P.sb([64, 257]); btot = Buf()
    kw = P.sb([64, 128], BF16); bkw = Buf()
    C = P.sb([128, 257]); bC = Buf(); Cb = P.sb([128, 257], BF16); bCb = Buf()
    m = P.sb([128, 1]); bm = Buf()
    ho = [(P.sb([64, 256]), Buf()) for _ in range(2)]
    sgroups = [(0, NCTX)] + [(NCTX + g * 512, min(512, TL - g * 512)) for g in range((TL + 511) // 512)]
    n = 0; nc_ = 0
    col = lambda i, p=64: v[0:p, i:i + 1]
    V = [bv]
    for h in range(2):
        for d_ in range(2):
            hD = hF[h] if d_ == 0 else hB[h]
            cb_ = 5 * d_
            P.op("dve", lambda e: e.memset(C[:], 0.0), [], [bC])
            P.op("dve", lambda e: e.memset(Cb[:], 0.0), [], [bCb])
            P.op("dve", lambda e: e.memset(m[:], NEG), [], [bm])
            order = sgroups if d_ == 0 else [sgroups[0]] + sgroups[:0:-1]
            for (g0, gn_) in order:
                q_, bq_ = qs[n % 2]; k_, bk_ = ks[n % 2]; t_, bt_ = tk[n % 2]; qb_, bqb_ = qb[n % 2]; kb_, bkb_ = kb[n % 2]
                n += 1
                nch = gn_ // 64
                P.dma("sp", q_[:, 0:gn_], qT[h][:, g0:g0 + gn_], [], [bq_])
                P.dma("sp", k_[:, 0:gn_], kT[h][:, g0:g0 + gn_], [], [bk_])
                P.dma("sp", t_[:, 0:nch, :], tokmaj[g0:g0 + gn_, :].rearrange("(c p) f -> p c f", p=64), [], [bt_])
                P.cp("dve", qb_[:, 0:gn_], q_[:, 0:gn_], [bq_], [bqb_])
                P.cp("act", kb_[:, 0:gn_], k_[:, 0:gn_], [bk_], [bkb_])
                chunks = range(nch) if d_ == 0 else range(nch - 1, -1, -1)
                for c in chunks:
                    cs = slice(c * 64, (c + 1) * 64)
                    va_, bva_ = va[nc_ % 2]; ho_, bho_ = ho[nc_ % 2]
                    nc_ += 1
                    P.tt(gc[:, 0:4], t_[:, c, 1280 + 4 * h:1284 + 4 * h], bg[:, h, :], ALU.add, [bt_, bcm], [bgc])
                    ic = gc[:, 2 * d_:2 * d_ + 1]; fc = gc[:, 4:5]
                    P.act(fc, gc[:, 2 * d_ + 1:2 * d_ + 2], AF.Sigmoid, [bgc], [bgc])
                    P.act(fc, fc, AF.Ln, [bgc], [bgc])
                    P.ts(A1[:], cms[:, cb_ + 0, :], fc, None, ALU.mult, None, [bcm, bgc], [bA1])
                    P.ts(Fb[:], ones[:], fc, None, ALU.mult, None, [bcm, bgc], [bFb])
                    P.ts(Ib[:], ones[:], ic, None, ALU.mult, None, [bcm, bgc], [bIb])
                    P.mm(pD[0][:], A1[:], ones[:, 0:64], True, False, [bA1, bcm], [pD[1]])
                    P.mm(pD[0][:], Fb[:, 0:64], cms[:, cb_ + 1, :], False, False, [bFb, bcm], [pD[1]])
                    P.mm(pD[0][:], Ib[:, 0:64], cms[:, cb_ + 4, :], False, True, [bIb, bcm], [pD[1]])
                    P.mm(pG[0][:], Fb[:], cms[:, cb_ + 2, :], True, False, [bFb, bcm], [pG[1]])
                    P.mm(pG[0][:], Ib[:], cms[:, cb_ + 4, :], False, True, [bIb, bcm], [pG[1]])
                    P.mm(pc[0][0:64, 0:1], cms[:, cb_ + 0, :], fc, True, True, [bcm, bgc], [pc[1]])
                    P.mm(pc[0][:, 1:2], ones[:], fc, True, True, [bcm, bgc], [pc[1]])
                    P.mm(pc[0][0:64, 2:3], cms[:, cb_ + 2, :], fc, True, True, [bcm, bgc], [pc[1]])
                    P.mm(pQK[0][:], qb_[:, cs], kb_[:, cs], True, True, [bqb_, bkb_], [pQK[1]])
                    P.tt(Dk[:], pD[0][:], cms[:, cb_ + 3, :], ALU.add, [pD[1], bcm], [bDk])
                    P.op("dve", lambda e: e.reduce_max(col(0), Dk[:], AX.X), [bDk], V)
                    P.tt(col(1), pc[0][0:64, 0:1], m[0:64, :], ALU.add, [pc[1], bm], V)
                    P.tt(col(2), col(1), col(0), ALU.max, V, V)
                    P.ts(col(3), col(2), -1.0, None, ALU.mult, None, V, V)
                    P.act(wi[:], Dk[:], AF.Exp, [bDk, bv], [bwi], bias=col(3))
                    P.act(col(4), col(1), AF.Exp, V, V, bias=col(3))
                    P.act(col(5), col(3), AF.Exp, V, V)
                    P.stt(sm[:], pQK[0][:], SC, wi[:], ALU.mult, ALU.mult, [pQK[1], bwi], [bsm])
                    P.tr(pT[0][:], sm[:], cms[:, cb_ + 4, :], [bsm, bcm], [pT[1]])
                    P.cp("act", smT[:], pT[0][:], [pT[1]], [bsmT])
                    P.cp("dve", va_[:, 0:256], t_[:, c, 256 + h * 256:512 + h * 256], [bt_], [bva_])
                    P.mm(pN[0][:], smT[:], va_[:], True, True, [bsmT, bva_], [pN[1]])
                    P.mm(pQC[0][:], qb_[:, cs], Cb[:], True, True, [bqb_, bCb], [pQC[1]])
                    P.cp("act", nA[:], pN[0][:], [pN[1]], [bnA])
                    P.stt(tot[:], pQC[0][:], col(4), nA[:], ALU.mult, ALU.add, [pQC[1], bv, bnA], [btot])
                    P.ts(col(13), tot[:, 256:257], -1.0, None, ALU.mult, None, [btot], V)
                    P.tt(col(6), tot[:, 256:257], col(13), ALU.max, [btot, bv], V)
                    P.tt(col(6), col(6), col(5), ALU.max, V, V)
                    P.op("dve", lambda e: e.reciprocal(col(6), col(6)), V, V)
                    P.ts(ho_[:], tot[:, 0:256], col(6), None, ALU.mult, None, [btot, bv], [bho_])
                    P.dma("pool", hD[g0 + c * 64:g0 + (c + 1) * 64, :], ho_[:], [bho_], [])
                    P.op("dve", lambda e: e.reduce_max(col(7, 128), pG[0][:], AX.X), [pG[1]], V)
                    P.tt(col(8, 128), pc[0][:, 1:2], m[:], ALU.add, [pc[1], bm], V)
                    P.tt(col(9, 128), col(8, 128), col(7, 128), ALU.max, V, V)
                    P.ts(col(10, 128), col(9, 128), -1.0, None, ALU.mult, None, V, V)
                    P.tt(col(11), pc[0][0:64, 2:3], ic, ALU.add, [pc[1], bgc], V)
                    P.act(col(11), col(11), AF.Exp, V, V, bias=col(10))
                    P.act(col(12, 128), col(8, 128), AF.Exp, V, V, bias=col(10, 128))
                    P.ts(kw[:], t_[:, c, h * 128:(h + 1) * 128], col(11), SC, ALU.mult, ALU.mult, [bt_, bv], [bkw])
                    P.mm(pC[0][:], kw[:], va_[:], True, True, [bkw, bva_], [pC[1]])
                    P.stt(C[:], C[:], col(12, 128), pC[0][:], ALU.mult, ALU.add, [bC, bv, pC[1]], [bC])
                    P.cp("act", Cb[:], C[:], [bC], [bCb])
                    P.cp("dve", m[:], col(9, 128), V, [bm])
    P.flush()
    all_dma_barrier(P)
    out_tm = None
    if mixloc is not None:
        mv_ = mixloc.rearrange("(r c) f -> c r f", c=GW)

        def out_tm(t, h, o2, bo2):
            j0 = (t - NCTX // 128) * 128
            pos = j0
            while pos < j0 + 128:
                c_, rw = divmod(pos, rows)
                n_ = min(rows - rw, j0 + 128 - pos)
                P.dma("pool", mv_[c_, rw:rw + n_, h * 256:(h + 1) * 256], o2[pos - j0:pos - j0 + n_, :], [bo2], [])
                pos += n_
    emit_finish(P, T, 2, tokmaj, 768, hF, hB, gn_bc[:, :], AF.Sigmoid, ident[:, :], mixT, out_tm=out_tm,
                t_start=(NCTX // 128 if mixloc is not None else 0))


def col16(v):
    return np.ascontiguousarray(v.reshape(16, 128).T)


def bc(v, n=128):
    return np.ascontiguousarray(np.broadcast_to(v, (n, v.shape[-1])))


def tri_consts():
    s = np.arange(64)[:, None]; t = np.arange(64)[None, :]
    f = np.float32
    sixteenth = np.float32(0.0625)
    return np.stack([np.where(s <= t, sixteenth, 0).astype(f), np.where(s > t, sixteenth, 0).astype(f), (s <= t).astype(f),
                     np.where(s >= t, sixteenth, 0).astype(f), np.where(s < t, sixteenth, 0).astype(f), (s >= t).astype(f)])


def odd_consts():
    u = np.arange(64)[:, None]; t = np.arange(64)[None, :]
    f = np.float32
    out = []
    for d in range(2):
        tri = (u <= t) if d == 0 else (u >= t)
        st = (u > t) if d == 0 else (u < t)
        ok = (t <= u) if d == 0 else (t >= u)
        out += [tri.astype(f), np.where(tri, -1.0, 0.0).astype(f), st.astype(f), np.where(ok, 0, NEG).astype(f), np.eye(64, dtype=f)]
    return np.stack(out)


def even_inputs(inp, j, x_b, ctx_b, c_b, c_ctx, hg):
    l = 2 * j
    w = inp["ev_w_in"][j]
    q0, k0, v0, r0, af0, ab0, xr0, xg0 = 0, 512, 1024, 2048, 3072, 3088, 3104, 4128
    h = hg
    cols = np.concatenate([np.arange(k0 + h * 128, k0 + (h + 1) * 128), np.arange(v0 + h * 256, v0 + (h + 1) * 256),
                           np.arange(r0 + h * 256, r0 + (h + 1) * 256), np.arange(q0 + h * 128, q0 + (h + 1) * 128),
                           np.arange(xr0 + h * 256, xr0 + (h + 1) * 256), np.arange(xg0 + h * 256, xg0 + (h + 1) * 256),
                           np.arange(af0, af0 + 16), np.arange(ab0, ab0 + 16)])
    ch = slice(h * 256, (h + 1) * 256)

    def colblk(v):
        return v[..., ch].reshape(v.shape[:-1] + (2, 128))
    return dict(
        xin=np.ascontiguousarray(np.concatenate([ctx_b, x_b], 0)), ccol=col16(c_b), cccol=col16(c_ctx),
        wmod=np.ascontiguousarray(inp["w_mod"][l][:, 0:4096]), bmod=np.ascontiguousarray(inp["b_mod"][l][None, 0:4096]),
        win=np.ascontiguousarray(w[:, cols]),
        wa2=np.ascontiguousarray(inp["gla_w_a2"][j][:, :, h * 128:(h + 1) * 128]),
        barow=np.ascontiguousarray(inp["gla_b_a"][j][:, None, h * 128:(h + 1) * 128]),
        gn_bc=bc(inp["gla_norm"][j]),
        convw=np.ascontiguousarray(colblk(inp["lru_conv_w"][j]).transpose(2, 1, 0)),
        convb=np.ascontiguousarray(colblk(inp["lru_conv_b"][j]).T),
        wr_=np.ascontiguousarray(inp["lru_w_r"][j][:, 2 * h:2 * h + 2]), wi_=np.ascontiguousarray(inp["lru_w_i"][j][:, 2 * h:2 * h + 2]),
        brc=np.ascontiguousarray(colblk(inp["lru_b_r"][j]).transpose(2, 0, 1)), bic=np.ascontiguousarray(colblk(inp["lru_b_i"][j]).transpose(2, 0, 1)),
        lamc=np.ascontiguousarray(colblk(inp["lru_lam"][j]).transpose(2, 0, 1)),
        ident=np.eye(128, dtype=np.float32), cm=tri_consts())


def odd_inputs(inp, j, xscan_b, c_b, c_ctx, hg):
    l = 2 * j + 1
    w = inp["od_w_in"][j]
    hs = [2 * hg, 2 * hg + 1]
    q0, k0, v0, o0, g0 = 0, 1024, 2048, 4096, 6144
    cols = np.concatenate([np.arange(k0 + h * 128, k0 + (h + 1) * 128) for h in hs] + [np.arange(v0 + h * 256, v0 + (h + 1) * 256) for h in hs]
                          + [np.arange(o0 + h * 256, o0 + (h + 1) * 256) for h in hs] + [np.array([g0 + h, g0 + 8 + h, g0 + 16 + h, g0 + 24 + h]) for h in hs]
                          + [np.arange(q0 + h * 128, q0 + (h + 1) * 128) for h in hs])
    bgate = inp["mlstm_b_gate"][j]
    bgb = np.ascontiguousarray(np.broadcast_to(np.stack([bgate[:, h] for h in hs])[None], (64, 2, 4))).astype(np.float32)
    return dict(xin=(None if xscan_b is None else np.ascontiguousarray(xscan_b)), ccol=col16(c_b), cccol=col16(c_ctx),
                wmod=np.ascontiguousarray(inp["w_mod"][l][:, 0:4096]), bmod=np.ascontiguousarray(inp["b_mod"][l][None, 0:4096]),
                win=np.ascontiguousarray(w[:, cols]), bgb=bgb, gn_bc=bc(inp["mlstm_norm"][j]),
                ident=np.eye(128, dtype=np.float32), cm=odd_consts())


def tok_inputs(inp, l, mixT, xres, c_b, c_ctx, wout):
    return dict(mixT=mixT, xres=xres, ccol=col16(c_b), cccol=col16(c_ctx),
                wmod=np.ascontiguousarray(inp["w_mod"][l][:, 4096:12288]), bmod=np.ascontiguousarray(inp["b_mod"][l][None, 4096:12288]),
                lnp=np.stack([bc(inp["ln_g"][l, 0]), bc(inp["ln_b"][l, 0]), bc(inp["ln_g"][l, 1]), bc(inp["ln_b"][l, 1])]),
                wout=wout, wr=np.ascontiguousarray(np.concatenate([inp["moe_w_group"][l], inp["moe_w_expert"][l]], 1)),
                brb=bc(np.concatenate([inp["moe_b_group"][l], inp["moe_b_expert"][l]])),
                wg=inp["moe_w_gate"][l], wu=inp["moe_w_up"][l], wd=inp["moe_w_down"][l], ident=np.eye(128, dtype=np.float32))


def gather_chunks(P, src, nrows, CR, dst):
    all_dma_barrier(P)
    bG = Buf()
    evs = []
    r0 = 0
    off = 0
    while r0 < nrows:
        n = min(CR, nrows - r0)
        evs.append(P.coll(src[r0:r0 + n, :], dst[off:off + 4 * n, :], [], [bG]))
        r0 += n
        off += 4 * n
    for q in ("sp", "act", "pool"):
        P._waits(q, [], [], evs)
    return bG


def build_fused(S, NCTX, NE=32):
    P = Prog()
    T = NCTX + S
    SQ = S // 4
    CQ = NCTX // 4
    NT0 = SQ // 128 + 1
    NP0 = NT0 * 128
    NT1 = SQ // 128
    CR = min(1024, SQ)
    mix0 = P.dram("mix0", [T, 512], BF16); G0 = P.dram("G0", [4 * T, 512], BF16)
    emit_evenmix(P, "e0_", S, NCTX, mixloc=mix0)
    bG0 = gather_chunks(P, mix0, T, CR, G0)
    xout0 = P.dram("xout0", [NP0, D], F32); G1 = P.dram("G1", [4 * NP0, D], F32)
    md = dict(G=G0, CR=CR, S=S, SQ=SQ, NCTX=NCTX, CQ=CQ, bG=bG0)
    RQ = S // 64 // 4
    emit_tokloc(P, "t0_", NT0, True, mix=md, xout=xout0, NE=NE, out_cm=RQ)
    bG1 = gather_chunks(P, xout0, NP0, 128, G1)
    mix1 = P.dram("mix1", [S, 512], BF16); Gm1 = P.dram("Gm1", [4 * S, 512], BF16)
    emit_oddmix(P, "o1_", S, NCTX, xsrc=dict(G=G1, bG=bG1), mixloc=mix1)
    bGm1 = gather_chunks(P, mix1, S, CR, Gm1)
    md1 = dict(G=Gm1, CR=CR, S=S, SQ=SQ, NCTX=NCTX, CQ=CQ, bG=bGm1)
    emit_tokloc(P, "t1_", NT1, False, mix=md1, xres=xout0, NE=NE, xres_cm=RQ)
    P.begin()
    P.flush(final=True)
    return P.nc


def fused_inputs(inp, b, q, S, NCTX, NE=32):
    x = inp["x"]; ctx = inp["ctx"]; c = inp["c"]; c_ctx = inp["c_ctx"]
    SQ = S // 4; CQ = NCTX // 4
    NP0 = (SQ // 128 + 1) * 128
    d = {}
    for k, v in even_inputs(inp, 0, x[b], ctx[b], c[b], c_ctx, q).items():
        d["e0_" + k] = v
    xr = np.zeros((NP0, D), np.float32)
    xr[0:SQ] = x[b, q * SQ:(q + 1) * SQ]
    xr[NP0 - 128:NP0 - 128 + CQ] = ctx[b, q * CQ:(q + 1) * CQ]
    perm0 = np.concatenate([np.concatenate([np.arange(r * 256, (r + 1) * 256), np.arange(1024 + r * 256, 1024 + (r + 1) * 256)]) for r in range(4)])
    for k, v in tok_inputs(inp, 0, None, xr, c[b], c_ctx, np.ascontiguousarray(inp["ev_w_out"][0][perm0])).items():
        if k != "mixT":
            d["t0_" + k] = v[:NE] if k in ("wg", "wu", "wd") else v
    for k, v in odd_inputs(inp, 0, None, c[b], c_ctx, q).items():
        if k != "xin":
            d["o1_" + k] = v
    for k, v in tok_inputs(inp, 1, None, None, c[b], c_ctx, inp["od_w_out"][0]).items():
        if k not in ("mixT", "xres", "cccol"):
            d["t1_" + k] = v[:NE] if k in ("wg", "wu", "wd") else v
    return d


def kernel(_NE=32, **inp):
    inp = {k: np.asarray(v) for k, v in inp.items()}
    x = inp["x"]
    B, S, _ = x.shape
    NCTX = inp["ctx"].shape[1]
    SQ = S // 4
    cores = [(b, q) for b in range(B) for q in range(4)]
    nc = build_fused(S, NCTX, _NE)
    res = run_bass_kernel_spmd(nc, [fused_inputs(inp, b, q, S, NCTX, _NE) for (b, q) in cores], core_ids=list(range(len(cores))))
    out = np.zeros_like(x)
    for i, (b, q) in enumerate(cores):
        out[b, q * SQ:(q + 1) * SQ] = res.results[i]["t1_xout"][0:SQ]
    return out
```

```python
import numpy as np
from contextlib import ExitStack
import ml_dtypes
import concourse.bass as bass
import concourse.mybir as mybir
from concourse.bass_utils import run_bass_kernel_spmd

F32 = mybir.dt.float32
BF16 = mybir.dt.bfloat16
AF = mybir.ActivationFunctionType
ALU = mybir.AluOpType
AX = mybir.AxisListType

D = 2048
ALPHA = 4.0 ** 0.25
EPS = 1e-5
NEG = -1e30
N_STG = 3
STT_TILES = 7


class Buf:
    __slots__ = ("w", "r")

    def __init__(self):
        self.w = None
        self.r = {}


class PBuf(Buf):
    __slots__ = ()
    excl = True


class Prog:
    CE = ("pe", "act", "dve", "pool")
    SEM_ROT = 16000

    def __init__(self, n_dma_sems=12):
        self.nc = bass.Bass("TRN2", target_bir_lowering=False)
        self.es = ExitStack()
        self.pes = None
        self.ops = {e: [] for e in ("pe", "act", "dve", "pool", "sp")}
        self.cnt = {e: 0 for e in self.CE}
        self.sems = []
        self.cur_sem = {}
        self.seen = {e: {} for e in self.ops}
        for e in self.CE:
            self.cur_sem[e] = self._new_sem("c_" + e)
        self.dq = {}
        for q in ("sp", "pool", "act"):
            self.dq[q] = dict(sems=[self._new_sem("d_%s" % q) for i in range(n_dma_sems)], i=0)
        self.n_t = 0

    def _new_sem(self, name):
        h = self.es.enter_context(self.nc.semaphore(name + "_%d" % len(self.sems)))
        self.sems.append(h)
        return len(self.sems) - 1

    def dram(self, name, shape, dt, kind="Internal"):
        return self.nc.dram_tensor(name, list(shape), dt, kind=kind).ap()

    def begin(self):
        self.pes = ExitStack()

    def sb(self, shape, dt=F32):
        self.n_t += 1
        st = self.pes if self.pes is not None else self.es
        return st.enter_context(self.nc.sbuf_tensor("t%d" % self.n_t, list(shape), dt))

    def ps(self, shape, dt=F32):
        self.n_t += 1
        st = self.pes if self.pes is not None else self.es
        return st.enter_context(self.nc.psum_tensor("p%d" % self.n_t, list(shape), dt))

    def _waits(self, eng, reads, writes, extra=()):
        waits = {}

        def need(ev):
            if ev is None:
                return
            s, v, src = ev
            if src == "pe" and eng == "pe":
                return
            if self.seen[eng].get(s, 0) >= v:
                return
            if waits.get(s, 0) < v:
                waits[s] = v
        for b in reads:
            need(b.w)
            if getattr(b, "excl", False):
                for ev in b.r.values():
                    if ev[2] != eng:
                        need(ev)
        for b in writes:
            need(b.w)
            for ev in b.r.values():
                need(ev)
        for ev in extra:
            need(ev)
        for s, v in waits.items():
            self.seen[eng][s] = v
            self.ops[eng].append(lambda e, s=s, v=v: e.wait_ge(self.sems[s], v))

    def _post(self, ev, reads, writes):
        key = ev[2] if not ev[2].startswith("dma") else ("d", ev[0])
        for b in reads:
            b.r[key] = ev
        for b in writes:
            b.w = ev
            b.r = {}

    def op(self, eng, f, reads=(), writes=()):
        self._waits(eng, reads, writes)
        if self.cnt[eng] >= self.SEM_ROT:
            self.cur_sem[eng] = self._new_sem("c_" + eng)
            self.cnt[eng] = 0
        self.cnt[eng] += 1
        s = self.cur_sem[eng]
        ev = (s, self.cnt[eng], eng)
        self.ops[eng].append(lambda e, s=s: f(e).then_inc(self.sems[s], 1))
        self._post(ev, reads, writes)
        return ev

    def dma(self, q, out, in_, reads=(), writes=(), **kw):
        d = self.dq[q]
        K = len(d["sems"])
        k = d["i"] % K
        j = d["i"] // K
        d["i"] += 1
        s = d["sems"][k]
        extra = [(s, 16 * j, "dma" + q)] if j > 0 else []
        self._waits(q, reads, writes, extra)
        ev = (s, 16 * (j + 1), "dma" + q)
        self.ops[q].append(lambda e, s=s: e.dma_start(out=out, in_=in_, **kw).then_inc(self.sems[s], 16))
        self._post(ev, reads, writes)
        return ev

    def dma_f(self, q, f, reads=(), writes=()):
        d = self.dq[q]
        K = len(d["sems"])
        k = d["i"] % K
        j = d["i"] // K
        d["i"] += 1
        s = d["sems"][k]
        extra = [(s, 16 * j, "dma" + q)] if j > 0 else []
        self._waits(q, reads, writes, extra)
        ev = (s, 16 * (j + 1), "dma" + q)
        self.ops[q].append(lambda e, s=s: f(e).then_inc(self.sems[s], 16))
        self._post(ev, reads, writes)
        return ev

    def coll(self, src, dst, reads=(), writes=(), groups=((0, 1, 2, 3), (4, 5, 6, 7))):
        if not hasattr(self, "cc_sem"):
            self.cc_sem = self._new_sem("cc")
            self.cc_n = 0
        s = self.cc_sem
        self.cc_n += 1
        self._waits("pool", reads, writes)
        rg = [list(g) for g in groups]
        self.ops["pool"].append(lambda e: e.collective_compute("AllGather", ALU.bypass, replica_groups=rg,
                                                               ins=[src.opt()], outs=[dst.opt()]).then_inc(self.sems[s]))
        ev = (s, self.cc_n, "dmacc")
        self._post(ev, reads, writes)
        self.cc_events = getattr(self, "cc_events", []) + [ev]
        return ev

    def flush(self, final=False):
        if final:
            finals = {}
            for q, d in self.dq.items():
                K = len(d["sems"])
                for k, s in enumerate(d["sems"]):
                    n = (d["i"] - k + K - 1) // K if d["i"] > k else 0
                    if n > 0:
                        finals[s] = 16 * n
            for e in self.CE:
                if self.cnt[e] > 0:
                    finals[self.cur_sem[e]] = self.cnt[e]
            for ev in getattr(self, "cc_events", []):
                finals[ev[0]] = ev[1]
            for s, v in finals.items():
                if self.seen["sp"].get(s, 0) < v:
                    self.seen["sp"][s] = v
                    self.ops["sp"].append(lambda e, s=s, v=v: e.wait_ge(self.sems[s], v))
        ops = self.ops
        with self.nc.Block() as block:
            @block.tensor
            def _(e):
                for f in ops["pe"]:
                    f(e)

            @block.scalar
            def _(e):
                for f in ops["act"]:
                    f(e)

            @block.vector
            def _(e):
                for f in ops["dve"]:
                    f(e)

            @block.gpsimd
            def _(e):
                for f in ops["pool"]:
                    f(e)

            @block.sync
            def _(e):
                for f in ops["sp"]:
                    f(e)
        self.ops = {e: [] for e in ops}
        if self.pes is not None:
            self.pes.close()
            self.pes = None
        if final:
            self.es.close()

    def mm(self, out, lhsT, rhs, start, stop, reads, writes):
        return self.op("pe", lambda e: e.matmul(out, lhsT, rhs, start=start, stop=stop), reads, writes)

    def tr(self, out, in_, ident, reads, writes):
        return self.op("pe", lambda e: e.transpose(out, in_, ident), reads, writes)

    def act(self, out, in_, func, reads, writes, bias=None, scale=None):
        kw = {}
        if bias is not None:
            kw["bias"] = bias
        if scale is not None:
            kw["scale"] = scale
        return self.op("act", lambda e: e.activation(out, in_, func, **kw), reads, writes)

    def tt(self, out, a, b, op, reads, writes, eng="dve"):
        return self.op(eng, lambda e: e.tensor_tensor(out, a, b, op), reads, writes)

    def ts(self, out, a, s1, s2, op0, op1, reads, writes):
        if s2 is None:
            return self.op("dve", lambda e: e.tensor_scalar(out, a, s1, None, op0), reads, writes)
        return self.op("dve", lambda e: e.tensor_scalar(out, a, s1, s2, op0, op1), reads, writes)

    def stt(self, out, a, s, b, op0, op1, reads, writes):
        return self.op("dve", lambda e: e.scalar_tensor_tensor(out, a, s, b, op0, op1), reads, writes)

    def cp(self, eng, out, in_, reads, writes):
        if eng == "act":
            return self.op("act", lambda e: e.copy(out, in_), reads, writes)
        return self.op(eng, lambda e: e.tensor_copy(out, in_), reads, writes)


def emit_mod_rows(P, ccols, wmod, bmod, ncols, mbc, add_one_groups, ident=None, mcol=None, bmcol=None, col_groups=None):
    P.begin()
    nco = len(ccols)
    col_groups = col_groups or {}
    if col_groups:
        idt = P.sb([128, 128]); bid = Buf()
        P.dma("sp", idt[:], ident, [], [bid])
        ptc = P.ps([128, 512]); bptc = PBuf()
    ones = P.sb([128, 128]); b_ones = Buf()
    P.op("dve", lambda e: e.memset(ones[:], 1.0), [], [b_ones])
    cbl = []
    for ci, cc in enumerate(ccols):
        cs = P.sb([128, 16]); bcs = Buf()
        P.dma("sp", cs[:], cc, [], [bcs])
        sc = P.sb([128, 16]); bsc = Buf()
        P.act(sc[:], cs[:], AF.Silu, [bcs], [bsc])
        cb = P.sb([128, 16, 128]); bcb = Buf()
        for k in range(16):
            P.ts(cb[:, k, :], ones[:], sc[:, k:k + 1], None, ALU.mult, None, [b_ones, bsc], [bcb])
        cbl.append((cb, bcb))
    bm = P.sb([1, ncols]); bbm = Buf()
    P.dma("sp", bm[:], bmod, [], [bbm])
    wts = [(P.sb([128, 8, 512]), Buf()) for _ in range(2)]
    pss = [(P.ps([128, 512]), PBuf()) for _ in range(2)]
    outs = [(P.sb([128, 512]), Buf()) for _ in range(2)]
    wv = wmod.rearrange("(k p) c -> p k c", p=128)
    n = 0
    for g in range(ncols // 512):
        cs_ = slice(g * 512, (g + 1) * 512)
        for half in range(2):
            wt, bwt = wts[half]
            P.dma("sp", wt[:], wv[:, half * 8:(half + 1) * 8, cs_], [], [bwt])
        for ci in range(nco):
            cb, bcb = cbl[ci]
            pp, bpp = pss[n % 2]
            ot, bot = outs[n % 2]
            n += 1
            P.mm(pp[:], ones[0:1, :], bm[0:1, cs_], True, False, [b_ones, bbm], [bpp])
            for k in range(16):
                wt, bwt = wts[k // 8]
                P.mm(pp[:], cb[:, k, :], wt[:, k % 8, :], False, k == 15, [bcb, bwt], [bpp])
            if g in add_one_groups:
                P.ts(ot[:], pp[:], 1.0, None, ALU.add, None, [bpp], [bot])
            else:
                P.cp("act", ot[:], pp[:], [bpp], [bot])
            if mbc is not None:
                P.dma("pool", mbc[ci, :, cs_], ot[:], [bot], [])
            if g in col_groups:
                for j in range(4):
                    P.tr(ptc[:, j * 128:(j + 1) * 128], ot[:, j * 128:(j + 1) * 128], idt[:], [bot, bid], [bptc])
                sl0 = col_groups[g] * 4
                P.cp("dve", mcol[:, ci, sl0:sl0 + 4], ptc[:].rearrange("p (a b) -> p a b", a=4)[:, :, 0], [bptc], [bmcol])
    P.flush()


def emit_ln(P, z, bz, out, bout, g_bc, b_bc, bconst, tmp):
    st, bst, mv, bmv = tmp
    for c4 in range(4):
        P.op("dve", lambda e, c4=c4: e.bn_stats(st[:, c4, :], z[:, c4 * 512:(c4 + 1) * 512]), [bz], [bst])
    P.op("dve", lambda e: e.bn_aggr(mv[:, 0:2], st[:]), [bst], [bmv])
    P.ts(mv[:, 2:3], mv[:, 1:2], EPS, None, ALU.add, None, [bmv], [bmv])
    P.op("act", lambda e: e.sqrt(mv[:, 2:3], mv[:, 2:3]), [bmv], [bmv])
    P.op("dve", lambda e: e.reciprocal(mv[:, 2:3], mv[:, 2:3]), [bmv], [bmv])
    P.ts(out[:], z[:], mv[:, 0:1], mv[:, 2:3], ALU.subtract, ALU.mult, [bz, bmv], [bout])
    P.tt(out[:], out[:], g_bc[:], ALU.mult, [bout, bconst], [bout])
    P.tt(out[:], out[:], b_bc[:], ALU.add, [bout, bconst], [bout])


def build_tokloc(NT, ctx_tile, stop=None, sub=99):
    P = Prog()
    emit_tokloc(P, "", NT, ctx_tile)
    P.begin()
    P.flush(final=True)
    return P.nc


def emit_tokloc(P, pre, NT, ctx_tile, mix=None, xres=None, xout=None, stop=None, sub=99, NE=32, wsrc=None, out_cm=None, xres_cm=None):
    NP = NT * 128
    I = lambda n, s, d=F32: P.dram(pre + n, s, d, kind="ExternalInput")
    mixT = I("mixT", [D, NP], BF16) if mix is None else None
    if xres is None:
        xres = I("xres", [NP, D])
    ccol = I("ccol", [128, 16])
    cccol = I("cccol", [128, 16]) if ctx_tile else None
    wmod = I("wmod", [D, 8192]); bmod = I("bmod", [1, 8192])
    lnp = I("lnp", [4, 128, D])
    wout = I("wout", [D, D])
    wr = I("wr", [D, 36]); brb = I("brb", [128, 36])
    if wsrc is None:
        wg = I("wg", [NE, D, 512]); wu = I("wu", [NE, D, 512]); wd = I("wd", [NE, 512, D])
        b_w = Buf()
    else:
        wg, wu, wd, b_w = wsrc
    ident = I("ident", [128, 128])
    if xout is None:
        xout = P.dram(pre + "xout", [NP, D], F32, kind="ExternalOutput")
    mbc = P.dram(pre + "mbc", [2, 128, 8192], F32)
    x1s = P.dram(pre + "x1s", [NP, D], F32)
    u2Ts = P.dram(pre + "u2Ts", [D, NP], BF16)
    Gs = P.dram(pre + "Gs", [NP, 32], F32)
    ys = P.dram(pre + "ys", [NP, D], F32)
    b_mbc = Buf(); b_x1s = Buf(); b_u2Ts = Buf(); b_Gs = Buf(); b_ys = Buf(); b_wbf = Buf()

    mcol = P.sb([128, 2, 32]); bmcol = Buf()
    cg_ = {4: 0, 5: 1, 6: 2, 7: 3, 8: 4, 9: 5, 10: 6, 11: 7}
    emit_mod_rows(P, [ccol[:, :], cccol[:, :]] if ctx_tile else [ccol[:, :]], wmod, bmod[:, :], 8192, mbc, (8, 9, 10, 11),
                  ident=ident[:, :], mcol=mcol, bmcol=bmcol, col_groups=cg_)
    d = P.dq["pool"]
    K = len(d["sems"])

    def drain_queue_events(q):
        d = P.dq[q]
        K = len(d["sems"])
        evs = []
        for k, s in enumerate(d["sems"]):
            n = (d["i"] - k + K - 1) // K if d["i"] > k else 0
            if n > 0:
                evs.append((s, 16 * n, "dma" + q))
        return evs

    def barrier_dram(bufs):
        evs = drain_queue_events("pool") + drain_queue_events("sp") + drain_queue_events("act")
        for q in ("sp", "pool", "act", "dve", "pe"):
            P._waits(q, [], [], evs)

    barrier_dram([b_mbc])

    P.begin()
    idt = P.sb([128, 128]); bid = Buf()
    P.dma("sp", idt[:], ident[:, :], [], [bid])
    woutb = P.sb([128, 16, D], BF16); bwo = Buf()
    stg = [(P.sb([128, D]), Buf()) for _ in range(2)]
    wov = wout.rearrange("(k p) c -> p k c", p=128)
    for j in range(16):
        s_, bs_ = stg[j % 2]
        P.dma("sp", s_[:], wov[:, j, :], [], [bs_])
        P.cp("act" if j % 2 else "dve", woutb[:, j, :], s_[:], [bs_], [bwo])
    wrs = P.sb([128, 16, 36]); bwr = Buf()
    P.dma("sp", wrs[:], wr.rearrange("(k p) c -> p k c", p=128), [], [bwr])
    brs = P.sb([128, 36]); bbr = Buf()
    P.dma("sp", brs[:], brb[:, :], [], [bbr])
    cA = P.sb([128, 3, D]); bcA = Buf()

    def load_consts_A(ci):
        P.dma("sp", cA[:, 0, :], mbc[ci, :, 0:2048], [], [bcA])
    load_consts_A(0)
    P.dma("sp", cA[:, 1, :], lnp[0], [], [bcA])
    P.dma("sp", cA[:, 2, :], lnp[1], [], [bcA])
    mts = [(P.sb([128, 16, 128], BF16), Buf()) for _ in range(2)]
    xts = [(P.sb([128, D]), Buf()) for _ in range(2)]
    zs = [(P.sb([128, D]), Buf()) for _ in range(2)]
    u2f = [(P.sb([128, 16, 128]), Buf()) for _ in range(1)]
    u2b = [(P.sb([128, 16, 128], BF16), Buf()) for _ in range(2)]
    py = [(P.ps([128, 512]), PBuf()) for _ in range(4)]
    ptr = [(P.ps([128, 512]), PBuf()) for _ in range(2 if mix is not None else 3)]
    prr = (P.ps([128, 64]), PBuf())
    if mix is not None:
        idb = P.sb([128, 128], BF16)
        P.cp("dve", idb[:], idt[:], [bid], [bid])
        mtms = [(P.sb([128, 4, 512], BF16), Buf()) for _ in range(2)]
        for m_, bm_ in mtms:
            P.op("dve", lambda e, m_=m_: e.memset(m_[:], 0.0), [], [bm_])
        pmt = (P.ps([128, 1024], BF16), PBuf())
        CR_ = mix["CR"]; S_ = mix["S"]; SQ_ = mix["SQ"]
        qcache = {}
        nq_ = SQ_ // CR_
        mixmine = P.dram(pre + "mixmine", [NP, 4, 512], BF16)
        bmm = [Buf() for _ in range(nq_ + 1)]
        for cb_ in range(nq_):
            def dynb(e, cb_=cb_):
                if "qoff" not in qcache:
                    qcache["q"] = e.partition_id() % 4
                    qcache["qoff"] = qcache["q"] * nq_
                return e.dma_start(out=mixmine[cb_ * CR_:(cb_ + 1) * CR_, :, :],
                                   in_=Gfull[cb_:cb_ + 3 * nq_ + 1][bass.ds(qcache["qoff"], 1), :, :, :].rearrange("c w r f -> (c w) r f"))
            P.dma_f("sp", dynb, [mix["bG"]], [bmm[cb_]])
        if ctx_tile:
            def dync(e):
                if "qoff" not in qcache:
                    qcache["q"] = e.partition_id() % 4
                    qcache["qoff"] = qcache["q"] * nq_
                return e.dma_start(out=mixmine[SQ_:SQ_ + mix["CQ"], :, :], in_=Glast[bass.ds(qcache["q"] * mix["CQ"], mix["CQ"]), :, :])
            P.dma_f("sp", dync, [mix["bG"]], [bmm[-1]])
        Gfull = mix["G"][0:4 * S_, :].rearrange("(c r w) f -> c w r f", r=4, w=CR_)
        if ctx_tile:
            Glast = mix["G"][4 * S_:4 * S_ + 4 * mix["NCTX"], :].rearrange("(r w) f -> w r f", r=4)
    lnt = (P.sb([128, 4, 6]), Buf(), P.sb([128, 4]), Buf())
    rt = P.sb([128, 128]); brt = Buf()
    lg = P.sb([128, 36]); blg = Buf()
    Gt = [(P.sb([128, 32]), Buf()) for _ in range(2)]
    mixv = mixT.rearrange("(k p) t -> p k t", p=128) if mix is None else None
    u2Tv = u2Ts.rearrange("(k p) t -> p k t", p=128)
    for t in range(NT):
        if ctx_tile and t == NT - 1:
            load_consts_A(1)
        tsl = slice(t * 128, (t + 1) * 128)
        mt, bmt = mts[t % 2]; xt, bxt = xts[t % 2]; z, bz = zs[t % 2]
        uf, buf_ = u2f[0]; ub, bub = u2b[t % 2]; G, bG = Gt[t % 2]
        ci = 1 if (ctx_tile and t == NT - 1) else 0
        if mix is None:
            P.dma("sp", mt[:], mixv[:, :, tsl], [], [bmt])
        else:
            mtm, bmtm = mtms[t % 2]
            if ctx_tile and t == NT - 1:
                P.dma("sp", mtm[0:mix["CQ"]], mixmine[SQ_:SQ_ + mix["CQ"], :, :], [bmm[-1]], [bmtm])
            else:
                P.dma("sp", mtm[:], mixmine[t * 128:(t + 1) * 128, :, :], [bmm[(t * 128) // CR_]], [bmtm])
            for rd in range(2):
                for k8 in range(8):
                    k = rd * 8 + k8
                    r_, kk = divmod(k, 4)
                    P.tr(pmt[0][:, k8 * 128:(k8 + 1) * 128], mtm[:, r_, kk * 128:(kk + 1) * 128], idb[:], [bmtm, bid], [pmt[1]])
                P.cp("act" if rd else "dve", mt[:, rd * 8:(rd + 1) * 8, :], pmt[0][:].rearrange("p (a b) -> p a b", b=128), [pmt[1]], [bmt])
        if xres_cm is None:
            P.dma("sp", xt[:], xres[tsl, :], [], [bxt])
        else:
            xv_ = xres[0:xres_cm * 64, :].rearrange("(c kk) d -> kk c d", kk=xres_cm)
            for hf_ in range(2):
                P.dma("sp", xt[hf_ * 64:(hf_ + 1) * 64, :], xv_[2 * t + hf_], [], [bxt])
        for cg in range(4):
            pp, bpp = py[cg]
            for k in range(16):
                P.mm(pp[:], mt[:, k, :], woutb[:, k, cg * 512:(cg + 1) * 512], k == 0, k == 15, [bmt, bwo], [bpp])
            P.tt(z[:, cg * 512:(cg + 1) * 512], pp[:], cA[:, 0, cg * 512:(cg + 1) * 512], ALU.mult, [bpp, bcA], [bz])
        P.stt(xt[:], xt[:], ALPHA, z[:], ALU.mult, ALU.add, [bxt, bz], [bxt])
        if sub <= 1:
            continue
        x1, bx1 = z, bz
        emit_ln(P, xt, bxt, x1, bx1, cA[:, 1, :], cA[:, 2, :], bcA, lnt)
        P.dma("pool", x1s[tsl, :], x1[:], [bx1], [b_x1s])
        if sub <= 2:
            continue
        for k4 in range(4):
            pp, bpp = ptr[k4 % len(ptr)]
            for kk in range(4):
                k = k4 * 4 + kk
                P.tr(pp[:, kk * 128:(kk + 1) * 128], x1[:, k * 128:(k + 1) * 128], idt[:], [bx1, bid], [bpp])
            for kk in range(4):
                k = k4 * 4 + kk
                P.act(uf[:, k, :], pp[:, kk * 128:(kk + 1) * 128], AF.Identity, [bpp, bmcol], [buf_],
                      bias=mcol[:, ci, k:k + 1], scale=mcol[:, ci, 16 + k:17 + k])
            P.cp("dve", ub[:, k4 * 4:(k4 + 1) * 4, :], uf[:, k4 * 4:(k4 + 1) * 4, :], [buf_], [bub])
        if sub <= 3:
            continue
        P.dma("act", u2Tv[:, :, tsl], ub[:], [bub], [b_u2Ts])
        if sub <= 4:
            continue
        pr, bpr = prr
        for k in range(16):
            P.mm(pr[:, 0:36], uf[:, k, :], wrs[:, k, :], k == 0, k == 15, [buf_, bwr], [bpr])
        P.tt(lg[:], pr[:, 0:36], brs[:], ALU.add, [bpr, bbr], [blg])
        if sub <= 5:
            continue
        R = [brt, blg]
        c = lambda i: rt[:, i:i + 1]
        gl = lg[:, 0:4]
        P.op("dve", lambda e: e.reduce_max(c(0), gl, AX.X), [blg], [brt])
        P.ts(rt[:, 8:12], gl, c(0), None, ALU.is_ge, None, R, [brt])
        P.ts(c(1), c(0), -1.0, None, ALU.mult, None, R, [brt])
        P.act(rt[:, 12:16], gl, AF.Exp, R, [brt], bias=c(1))
        P.op("dve", lambda e: e.reduce_sum(c(2), rt[:, 12:16], AX.X), R, [brt])
        P.op("dve", lambda e: e.reciprocal(c(2), c(2)), R, [brt])
        els = rt[:, 16:24]
        P.ts(els, lg[:, 4:12], c(8), None, ALU.mult, None, R, [brt])
        for g in range(1, 4):
            P.stt(els, lg[:, 4 + 8 * g:12 + 8 * g], c(8 + g), els, ALU.mult, ALU.add, R, [brt])
        P.op("dve", lambda e: e.reduce_max(c(3), els, AX.X), R, [brt])
        mk1 = rt[:, 24:32]
        P.ts(mk1, els, c(3), None, ALU.is_ge, None, R, [brt])
        els2 = rt[:, 32:40]
        P.stt(els2, mk1, NEG, els, ALU.mult, ALU.add, R, [brt])
        P.op("dve", lambda e: e.reduce_max(c(4), els2, AX.X), R, [brt])
        mk2 = rt[:, 40:48]
        P.ts(mk2, els2, c(4), None, ALU.is_ge, None, R, [brt])
        P.tt(c(5), c(4), c(3), ALU.subtract, R, [brt])
        P.act(c(5), c(5), AF.Exp, R, [brt])
        P.ts(c(6), c(5), 1.0, None, ALU.add, None, R, [brt])
        P.op("dve", lambda e: e.reciprocal(c(6), c(6)), R, [brt])
        P.tt(c(7), c(5), c(6), ALU.mult, R, [brt])
        P.tt(c(6), c(6), c(2), ALU.mult, R, [brt])
        P.tt(c(7), c(7), c(2), ALU.mult, R, [brt])
        gsel = rt[:, 48:56]
        P.ts(gsel, mk1, c(6), None, ALU.mult, None, R, [brt])
        P.stt(gsel, mk2, c(7), gsel, ALU.mult, ALU.add, R, [brt])
        for g in range(4):
            P.ts(G[:, 8 * g:8 * g + 8], gsel, c(8 + g), None, ALU.mult, None, R, [bG])
        P.dma("pool", Gs[tsl, :], G[:], [bG], [b_Gs])
    P.flush()
    barrier_dram([])

    P.begin()
    STT = STT_TILES
    n_st = (NT + STT - 1) // STT
    u2T = P.sb([128, 16, STT * 128], BF16); bu2T = Buf()
    Gst = P.sb([128, STT, 32]); bGst = Buf()
    acc = [(P.sb([128, D]), Buf()) for _ in range(STT)]
    slots = [(P.sb([128, 8192], BF16), Buf()) for _ in range(4)]
    stg = [(P.sb([128, 2048]), Buf()) for _ in range(N_STG)]
    hT = [(P.sb([128, 4, 512], BF16), Buf()) for _ in range(2)]
    sil = [(P.sb([128, 512]), Buf()) for _ in range(2)]
    pg = [(P.ps([128, 512]), PBuf()) for _ in range(2)]
    pu = [(P.ps([128, 512]), PBuf()) for _ in range(2)]
    pd = [(P.ps([128, 512]), PBuf()) for _ in range(4)]
    wsrc = [wg, wu, wd]
    nslot = 0
    nstg = 0
    nh = 0
    for st in range(n_st):
        t0 = st * STT
        nt = min(STT, NT - t0)
        ntok = nt * 128
        P.dma("sp", u2T[:, :, 0:ntok], u2Tv[:, :, t0 * 128:t0 * 128 + ntok], [b_u2Ts], [bu2T])
        P.dma("sp", Gst[:, 0:nt, :], Gs[t0 * 128:t0 * 128 + ntok, :].rearrange("(t p) e -> p t e", p=128), [b_Gs], [bGst])
        for e_ in range(NE):
            mats = []
            for mi in range(3):
                sl, bsl = slots[nslot % 4]
                nslot += 1
                if mi < 2:
                    sv = sl[:].rearrange("p (k f) -> p k f", k=16)
                    srcv = wsrc[mi][e_].rearrange("(k p) f -> p k f", p=128)
                    pieces = [(sv[:, 4 * j:4 * j + 4, :], srcv[:, 4 * j:4 * j + 4, :], 4) for j in range(4)]
                else:
                    sv = sl[:].rearrange("p (k c) -> p k c", k=4)
                    srcv = wsrc[mi][e_].rearrange("(k p) c -> p k c", p=128)
                    pieces = [(sv[:, j:j + 1, :], srcv[:, j:j + 1, :], 1) for j in range(4)]
                for j, (dv, sv_, a) in enumerate(pieces):
                    sg_, bsg_ = stg[nstg % N_STG]
                    nstg += 1
                    P.dma("sp", sg_[:].rearrange("p (a b) -> p a b", a=a), sv_, [b_w], [bsg_])
                    P.cp("act" if (nstg % 2) else "dve", dv, sg_[:].rearrange("p (a b) -> p a b", a=a), [bsg_], [bsl])
                mats.append((sv, bsl))
            (Wg, bWg), (Wu, bWu), (Wd, bWd) = mats
            for tg in range(0, ntok, 512):
                n = min(512, ntok - tg)
                h, bh = hT[nh % 2]
                nh += 1
                for f in range(4):
                    g_, bg_ = pg[f % 2]; u_, bu_ = pu[f % 2]; s_, bs_ = sil[f % 2]
                    for k in range(16):
                        P.mm(g_[:, 0:n], Wg[:, k, f * 128:(f + 1) * 128], u2T[:, k, tg:tg + n], k == 0, k == 15, [bWg, bu2T], [bg_])
                    for k in range(16):
                        P.mm(u_[:, 0:n], Wu[:, k, f * 128:(f + 1) * 128], u2T[:, k, tg:tg + n], k == 0, k == 15, [bWu, bu2T], [bu_])
                    P.act(s_[:, 0:n], g_[:, 0:n], AF.Silu, [bg_], [bs_])
                    P.tt(h[:, f, 0:n], s_[:, 0:n], u_[:, 0:n], ALU.mult, [bs_, bu_], [bh])
                for ti in range(n // 128):
                    tl = (tg // 128) + ti
                    a_, ba_ = acc[tl]
                    for cg in range(4):
                        pp, bpp = pd[cg]
                        for f in range(4):
                            P.mm(pp[:], h[:, f, ti * 128:(ti + 1) * 128], Wd[:, f, cg * 512:(cg + 1) * 512], f == 0, f == 3, [bh, bWd], [bpp])
                        cs_ = slice(cg * 512, (cg + 1) * 512)
                        if e_ == 0:
                            P.ts(a_[:, cs_], pp[:], Gst[:, tl, 0:1], None, ALU.mult, None, [bpp, bGst], [ba_])
                        else:
                            P.stt(a_[:, cs_], pp[:], Gst[:, tl, e_:e_ + 1], a_[:, cs_], ALU.mult, ALU.add, [bpp, bGst, ba_], [ba_])
        for tl in range(nt):
            a_, ba_ = acc[tl]
            P.dma("pool", ys[(t0 + tl) * 128:(t0 + tl + 1) * 128, :], a_[:], [ba_], [b_ys])
    P.flush()
    barrier_dram([])

    P.begin()
    cC = P.sb([128, 3, D]); bcC = Buf()

    def load_consts_C(ci):
        P.dma("sp", cC[:, 0, :], mbc[ci, :, 6144:8192], [], [bcC])
        P.dma("sp", cC[:, 1, :], lnp[2], [], [bcC])
        P.dma("sp", cC[:, 2, :], lnp[3], [], [bcC])
    load_consts_C(0)
    xa = [(P.sb([128, D]), Buf()) for _ in range(2)]
    ya = [(P.sb([128, D]), Buf()) for _ in range(2)]
    oa = [(P.sb([128, D]), Buf()) for _ in range(2)]
    lnt = (P.sb([128, 4, 6]), Buf(), P.sb([128, 4]), Buf())
    for t in range(NT):
        if ctx_tile and t == NT - 1:
            load_consts_C(1)
        tsl = slice(t * 128, (t + 1) * 128)
        x1, bx1 = xa[t % 2]; y, by = ya[t % 2]; o, bo = oa[t % 2]
        P.dma("sp", x1[:], x1s[tsl, :], [b_x1s], [bx1])
        P.dma("sp", y[:], ys[tsl, :], [b_ys], [by])
        P.tt(y[:], y[:], cC[:, 0, :], ALU.mult, [by, bcC], [by])
        P.stt(y[:], x1[:], ALPHA, y[:], ALU.mult, ALU.add, [bx1, by], [by])
        emit_ln(P, y, by, o, bo, cC[:, 1, :], cC[:, 2, :], bcC, lnt)
        if out_cm is None or (ctx_tile and t == NT - 1):
            P.dma("pool", xout[tsl, :], o[:], [bo], [])
        else:
            ov_ = xout[0:out_cm * 64, :].rearrange("(c kk) d -> kk c d", kk=out_cm)
            for hf_ in range(2):
                P.dma("pool", ov_[2 * t + hf_], o[hf_ * 64:(hf_ + 1) * 64, :], [bo], [])
    P.flush()


def emit_projection(P, xin, T, n_ctx_tiles, win, ncols, mcol, bmcol, ident, tm_cols, tokmaj, fm_specs, load_x=None):
    P.begin()
    idt = P.sb([128, 128]); bid = Buf()
    P.dma("sp", idt[:], ident, [], [bid])
    Wb = P.sb([128, 16, ncols], BF16); bW = Buf()
    stg = [(P.sb([128, ncols]), Buf()) for _ in range(2)]
    wv = win.rearrange("(k p) c -> p k c", p=128)
    for k in range(16):
        s_, bs_ = stg[k % 2]
        P.dma("sp", s_[:], wv[:, k, :], [], [bs_])
        P.cp("act" if k % 2 else "dve", Wb[:, k, :], s_[:], [bs_], [bW])
    xts = [(P.sb([128, D]), Buf()) for _ in range(2)]
    uTs = [(P.sb([128, 16, 128], BF16), Buf()) for _ in range(2)]
    ptr = [(P.ps([128, 512]), PBuf()) for _ in range(2)]
    ptm = [(P.ps([128, 512]), PBuf()) for _ in range(3)]
    pfm = [(P.ps([128, 512]), PBuf()) for _ in range(2)]
    tms = [(P.sb([128, tm_cols]), Buf()) for _ in range(2)]
    fms = [(P.sb([128, 4, 128]), Buf()) for _ in range(2)]
    nfm = 0
    for t in range(T // 128):
        ci = 1 if t < n_ctx_tiles else 0
        tsl = slice(t * 128, (t + 1) * 128)
        xt, bxt = xts[t % 2]; uT, buT = uTs[t % 2]; tm, btm = tms[t % 2]
        if load_x is None:
            P.dma("sp", xt[:], xin[tsl, :], [], [bxt])
        else:
            load_x(t, xt, bxt)
        for k4 in range(4):
            pp, bpp = ptr[k4 % 2]
            for kk in range(4):
                k = k4 * 4 + kk
                P.tr(pp[:, kk * 128:(kk + 1) * 128], xt[:, k * 128:(k + 1) * 128], idt[:], [bxt, bid], [bpp])
            for kk in range(4):
                k = k4 * 4 + kk
                if k4 % 2 == 0:
                    P.act(uT[:, k, :], pp[:, kk * 128:(kk + 1) * 128], AF.Identity, [bpp, bmcol], [buT],
                          bias=mcol[:, ci, k:k + 1], scale=mcol[:, ci, 16 + k:17 + k])
                else:
                    P.ts(uT[:, k, :], pp[:, kk * 128:(kk + 1) * 128], mcol[:, ci, 16 + k:17 + k], mcol[:, ci, k:k + 1],
                         ALU.mult, ALU.add, [bpp, bmcol], [buT])
        nb = (tm_cols + 511) // 512
        for b_ in range(nb):
            c0 = b_ * 512
            w = min(512, tm_cols - c0)
            pp, bpp = ptm[b_ % 3]
            for k in range(16):
                P.mm(pp[:, 0:w], uT[:, k, :], Wb[:, k, c0:c0 + w], k == 0, k == 15, [buT, bW], [bpp])
            P.cp("act" if b_ % 2 else "dve", tm[:, c0:c0 + w], pp[:, 0:w], [bpp], [btm])
        P.dma("pool", tokmaj[tsl, :], tm[:], [btm], [])
        for j0 in range(0, len(fm_specs), 4):
            grp = fm_specs[j0:j0 + 4]
            pp, bpp = pfm[nfm % 2]; fm, bfm = fms[nfm % 2]
            nfm += 1
            for j, (c0, w, dst) in enumerate(grp):
                for k in range(16):
                    P.mm(pp[0:w, j * 128:(j + 1) * 128], Wb[:, k, c0:c0 + w], uT[:, k, :], k == 0, k == 15, [bW, buT], [bpp])
            P.cp("act" if nfm % 2 else "dve", fm[:, 0:len(grp), :], pp[:, 0:len(grp) * 128].rearrange("p (a b) -> p a b", b=128), [bpp], [bfm])
            for j, (c0, w, dst) in enumerate(grp):
                P.dma("act", dst[:, tsl], fm[0:w, j, :], [bfm], [])
    P.flush()


def all_dma_barrier(P):
    evs = []
    for q in ("sp", "pool", "act"):
        d = P.dq[q]
        K = len(d["sems"])
        for k, s in enumerate(d["sems"]):
            n = (d["i"] - k + K - 1) // K if d["i"] > k else 0
            if n > 0:
                evs.append((s, 16 * n, "dma" + q))
    for q in ("sp", "pool", "act", "dve", "pe"):
        P._waits(q, [], [], evs)


def emit_finish(P, T, nh, tokmaj, gate_c0, oF, oB, gn_bc, gate_func, ident, mixT, out_tm=None, t_start=0):
    P.begin()
    idt = P.sb([128, 128]); bid = Buf()
    P.dma("sp", idt[:], ident, [], [bid])
    gn = P.sb([128, 256]); bgn = Buf()
    P.dma("sp", gn[:], gn_bc, [], [bgn])
    A = [(P.sb([128, 256]), Buf()) for _ in range(2)]
    Bt = [(P.sb([128, 256]), Buf()) for _ in range(2)]
    Gt = [(P.sb([128, 256]), Buf()) for _ in range(2)]
    sq = P.sb([128, 256]); bsq = Buf()
    sc = P.sb([128, 4]); bsc = Buf()
    pt = [(P.ps([128, 512]), PBuf()) for _ in range(2)]
    ot = [(P.sb([128, 2, 128], BF16), Buf()) for _ in range(2)]
    n = 0
    otm = [(P.sb([128, 256], BF16), Buf()) for _ in range(2)]
    for t in range(t_start, T // 128):
        tsl = slice(t * 128, (t + 1) * 128)
        for h in range(nh):
            a, ba = A[n % 2]; b, bb = Bt[n % 2]; g, bg = Gt[n % 2]; pp, bpp = pt[n % 2]; o, bo = ot[n % 2]
            o2, bo2 = otm[n % 2]
            n += 1
            P.dma("sp", a[:], oF[h][tsl, :], [], [ba])
            P.dma("sp", b[:], oB[h][tsl, :], [], [bb])
            P.dma("sp", g[:], tokmaj[tsl, gate_c0 + h * 256:gate_c0 + (h + 1) * 256], [], [bg])
            P.tt(a[:], a[:], b[:], ALU.add, [ba, bb], [ba])
            P.tt(sq[:], a[:], a[:], ALU.mult, [ba], [bsq])
            P.op("dve", lambda e: e.reduce_sum(sc[:, 0:1], sq[:], AX.X), [bsq], [bsc])
            P.ts(sc[:, 0:1], sc[:, 0:1], 1.0 / 256, EPS, ALU.mult, ALU.add, [bsc], [bsc])
            P.op("act", lambda e: e.sqrt(sc[:, 0:1], sc[:, 0:1]), [bsc], [bsc])
            P.op("dve", lambda e: e.reciprocal(sc[:, 0:1], sc[:, 0:1]), [bsc], [bsc])
            P.stt(a[:], a[:], sc[:, 0:1], gn[:], ALU.mult, ALU.mult, [ba, bsc, bgn], [ba])
            P.act(g[:], g[:], gate_func, [bg], [bg])
            if out_tm is not None:
                P.tt(o2[:], a[:], g[:], ALU.mult, [ba, bg], [bo2])
                out_tm(t, h, o2, bo2)
                continue
            P.tt(a[:], a[:], g[:], ALU.mult, [ba, bg], [ba])
            for j in range(2):
                P.tr(pp[:, j * 128:(j + 1) * 128], a[:, j * 128:(j + 1) * 128], idt[:], [ba, bid], [bpp])
            P.cp("act", o[:], pp[:, 0:256].rearrange("p (a b) -> p a b", b=128), [bpp], [bo])
            for j in range(2):
                P.dma("act", mixT[h * 256 + j * 128:h * 256 + (j + 1) * 128, tsl], o[:, j, :], [bo], [])
    P.flush()


def build_evenmix(TL, NCTX=256):
    P = Prog()
    emit_evenmix(P, "", TL, NCTX)
    P.begin()
    P.flush(final=True)
    return P.nc


def emit_evenmix(P, pre, TL, NCTX=256, mixloc=None):
    T = NCTX + TL
    I = lambda n, s, d=F32: P.dram(pre + n, s, d, kind="ExternalInput")
    xin = I("xin", [T, D]); ccol = I("ccol", [128, 16]); cccol = I("cccol", [128, 16])
    wmod = I("wmod", [D, 4096]); bmod = I("bmod", [1, 4096])
    win = I("win", [D, 1312])
    wa2 = I("wa2", [2, 16, 128]); barow = I("barow", [2, 1, 128])
    gn_bc = I("gn_bc", [128, 256])
    convw = I("convw", [128, 2, 4]); convb = I("convb", [128, 2])
    wr_ = I("wr_", [2, 2, 128, 128]); wi_ = I("wi_", [2, 2, 128, 128])
    brc = I("brc", [128, 2, 2]); bic = I("bic", [128, 2, 2]); lamc = I("lamc", [128, 2, 2])
    ident = I("ident", [128, 128])
    cm = I("cm", [6, 64, 64])
    mixT = P.dram(pre + "mixT", [512, T], BF16, kind="ExternalOutput") if mixloc is None else None
    tokmaj = P.dram(pre + "tokmaj", [T, 640], F32)
    qT = P.dram(pre + "qT", [128, T], F32); kT = P.dram(pre + "kT", [128, T], F32)
    xrT = [P.dram(pre + "xrT%d" % i, [128, T], F32) for i in range(2)]
    xgT = [P.dram(pre + "xgT%d" % i, [128, T], F32) for i in range(2)]
    afT = P.dram(pre + "afT", [16, T], F32); abT = P.dram(pre + "abT", [16, T], F32)
    hF = [P.dram(pre + "hF%d" % i, [128, T], F32) for i in range(2)]
    oF = P.dram(pre + "oF", [T, 256], F32); oB = P.dram(pre + "oB", [T, 256], F32)

    mcol = P.sb([128, 2, 32]); bmcol = Buf()
    emit_mod_rows(P, [ccol[:, :], cccol[:, :]], wmod, bmod[:, :], 4096, None, (4, 5, 6, 7), ident=ident[:, :], mcol=mcol, bmcol=bmcol,
                  col_groups={g: g for g in range(8)})
    fm = [(640, 128, qT), (0, 128, kT), (768, 128, xrT[0]), (896, 128, xrT[1]), (1024, 128, xgT[0]), (1152, 128, xgT[1]),
          (1280, 16, afT), (1296, 16, abT)]
    emit_projection(P, xin, T, NCTX // 128, win, 1312, mcol, bmcol, ident[:, :], 640, tokmaj, fm)
    all_dma_barrier(P)

    P.begin()
    cw = P.sb([128, 2, 4]); cb = P.sb([128, 2]); bcw = Buf()
    P.dma("sp", cw[:], convw[:, :, :], [], [bcw]); P.dma("sp", cb[:], convb[:, :], [], [bcw])
    Wr = P.sb([128, 4, 128]); Wi = P.sb([128, 4, 128]); bWg = Buf()
    for d_ in range(2):
        for bl in range(2):
            P.dma("sp", Wr[:, d_ * 2 + bl, :], wr_[d_, bl], [], [bWg])
            P.dma("sp", Wi[:, d_ * 2 + bl, :], wi_[d_, bl], [], [bWg])
    gb = P.sb([128, 3, 4]); bgb = Buf()
    P.dma("sp", gb[:, 0, :], brc.rearrange("p a b -> p (a b)"), [], [bgb])
    P.dma("sp", gb[:, 1, :], bic.rearrange("p a b -> p (a b)"), [], [bgb])
    P.dma("sp", gb[:, 2, :], lamc.rearrange("p a b -> p (a b)"), [], [bgb])
    P.act(gb[:, 2, :], gb[:, 2, :], AF.Sigmoid, [bgb], [bgb])
    P.act(gb[:, 2, :], gb[:, 2, :], AF.Ln, [bgb], [bgb])
    P.ts(gb[:, 2, :], gb[:, 2, :], 8.0, None, ALU.mult, None, [bgb], [bgb])
    xh = [(P.sb([128, 516]), Buf()) for _ in range(2)]
    xc = P.sb([128, 512]); bxc = Buf()
    pr = (P.ps([128, 512]), PBuf()); pi = (P.ps([128, 512]), PBuf())
    r = P.sb([128, 512]); br = Buf(); i_ = P.sb([128, 512]); bi = Buf()
    a = P.sb([128, 512]); ba = Buf(); s = P.sb([128, 512]); bs = Buf()
    hh = [(P.sb([128, 512]), Buf()) for _ in range(2)]
    hst = P.sb([128, 1]); bhst = Buf()
    hf = [(P.sb([128, 512]), Buf()) for _ in range(2)]
    xg = [(P.sb([128, 512]), Buf()) for _ in range(2)]
    yo = [(P.sb([128, 512], BF16), Buf()) for _ in range(2)]
    if mixloc is not None:
        idl = P.sb([128, 128]); bidl = Buf()
        P.dma("sp", idl[:], ident[:, :], [], [bidl])
        yf = P.sb([128, 512]); byf = Buf()
        pyt = (P.ps([128, 512]), PBuf())
        yT = [(P.sb([128, 4, 128], BF16), Buf()) for _ in range(2)]
    seqs = [(0, NCTX)] + [(NCTX + g * 512, min(512, TL - g * 512)) for g in range((TL + 511) // 512)]
    n = 0
    for bl in range(2):
        for d_ in range(2):
            gi = d_ * 2 + bl
            P.op("dve", lambda e: e.memset(hst[:], 0.0), [], [bhst])
            order = seqs if d_ == 0 else [seqs[0]] + seqs[:0:-1]
            for (g0, gn_) in order:
                s0, s1 = (0, NCTX) if g0 < NCTX else (NCTX, T)
                x_, bx_ = xh[n % 2]; h_, bh_ = hh[n % 2]; hf_, bhf_ = hf[n % 2]; xg_, bxg_ = xg[n % 2]; y_, by_ = yo[n % 2]
                n += 1
                lo = max(g0 - 2, s0); hi = min(g0 + gn_ + 1, s1)
                P.op("dve", lambda e, x_=x_: e.memset(x_[:], 0.0), [], [bx_])
                P.dma("sp", x_[:, lo - (g0 - 2):hi - (g0 - 2)], xrT[bl][:, lo:hi], [], [bx_])
                P.ts(xc[:, 0:gn_], x_[:, 0:gn_], cw[:, bl, 0:1], cb[:, bl:bl + 1], ALU.mult, ALU.add, [bx_, bcw], [bxc])
                for j in range(1, 4):
                    P.stt(xc[:, 0:gn_], x_[:, j:j + gn_], cw[:, bl, j:j + 1], xc[:, 0:gn_], ALU.mult, ALU.add, [bx_, bcw, bxc], [bxc])
                P.mm(pr[0][:, 0:gn_], Wr[:, gi, :], xc[:, 0:gn_], True, True, [bWg, bxc], [pr[1]])
                P.mm(pi[0][:, 0:gn_], Wi[:, gi, :], xc[:, 0:gn_], True, True, [bWg, bxc], [pi[1]])
                P.act(r[:, 0:gn_], pr[0][:, 0:gn_], AF.Sigmoid, [pr[1], bgb], [br], bias=gb[:, 0, gi:gi + 1])
                P.act(i_[:, 0:gn_], pi[0][:, 0:gn_], AF.Sigmoid, [pi[1], bgb], [bi], bias=gb[:, 1, gi:gi + 1])
                P.act(a[:, 0:gn_], r[:, 0:gn_], AF.Exp, [br, bgb], [ba], scale=gb[:, 2, gi:gi + 1])
                P.tt(s[:, 0:gn_], a[:, 0:gn_], a[:, 0:gn_], ALU.mult, [ba], [bs])
                P.act(s[:, 0:gn_], s[:, 0:gn_], AF.Sqrt, [bs], [bs], bias=1.0, scale=-1.0)
                P.tt(i_[:, 0:gn_], i_[:, 0:gn_], xc[:, 0:gn_], ALU.mult, [bi, bxc], [bi])
                P.tt(i_[:, 0:gn_], i_[:, 0:gn_], s[:, 0:gn_], ALU.mult, [bi, bs], [bi])
                if d_ == 0:
                    P.op("dve", lambda e, h_=h_, gn_=gn_: e.tensor_tensor_scan(h_[:, 0:gn_], a[:, 0:gn_], i_[:, 0:gn_], hst[:, 0:1], ALU.mult, ALU.add),
                         [ba, bi, bhst], [bh_])
                    P.cp("dve", hst[:], h_[:, gn_ - 1:gn_], [bh_], [bhst])
                    P.dma("pool", hF[bl][:, g0:g0 + gn_], h_[:, 0:gn_], [bh_], [])
                else:
                    P.op("dve", lambda e, h_=h_, gn_=gn_: e.tensor_tensor_scan(h_[:, 0:gn_][:, ::-1], a[:, 0:gn_][:, ::-1], i_[:, 0:gn_][:, ::-1], hst[:, 0:1], ALU.mult, ALU.add),
                         [ba, bi, bhst], [bh_])
                    P.cp("dve", hst[:], h_[:, 0:1], [bh_], [bhst])
                    P.dma("sp", hf_[:, 0:gn_], hF[bl][:, g0:g0 + gn_], [], [bhf_])
                    P.dma("sp", xg_[:, 0:gn_], xgT[bl][:, g0:g0 + gn_], [], [bxg_])
                    P.tt(h_[:, 0:gn_], h_[:, 0:gn_], hf_[:, 0:gn_], ALU.add, [bh_, bhf_], [bh_])
                    P.act(xg_[:, 0:gn_], xg_[:, 0:gn_], AF.Gelu_apprx_tanh, [bxg_], [bxg_])
                    if mixloc is None:
                        P.tt(y_[:, 0:gn_], h_[:, 0:gn_], xg_[:, 0:gn_], ALU.mult, [bh_, bxg_], [by_])
                        P.dma("pool", mixT[256 + bl * 128:256 + (bl + 1) * 128, g0:g0 + gn_], y_[:, 0:gn_], [by_], [])
                    else:
                        yT_, byT_ = yT[n % 2]
                        nb_ = gn_ // 128
                        P.tt(yf[:, 0:gn_], h_[:, 0:gn_], xg_[:, 0:gn_], ALU.mult, [bh_, bxg_], [byf])
                        for j in range(nb_):
                            P.tr(pyt[0][:, j * 128:(j + 1) * 128], yf[:, j * 128:(j + 1) * 128], idl[:], [byf, bidl], [pyt[1]])
                        P.cp("act", yT_[:, 0:nb_, :], pyt[0][:, 0:gn_].rearrange("p (a b) -> p a b", b=128), [pyt[1]], [byT_])
                        mr0 = (TL + g0) if g0 < NCTX else (g0 - NCTX)
                        P.dma("pool", mixloc[mr0:mr0 + gn_, 256 + bl * 128:256 + (bl + 1) * 128].rearrange("(j p) c -> p j c", p=128),
                              yT_[:, 0:nb_, :], [byT_], [])
            if d_ == 0:
                all_dma_barrier(P)
    P.flush()
    all_dma_barrier(P)

    P.begin()
    cms = P.sb([64, 6, 64]); bcm = Buf()
    P.dma("sp", cms[:], cm.rearrange("a s t -> s a t"), [], [bcm])
    ones = P.sb([1, 64]); P.op("dve", lambda e: e.memset(ones[:], 1.0), [], [bcm])
    wa = P.sb([16, 2, 128]); bwa = Buf()
    P.dma("sp", wa[:], wa2.rearrange("d r c -> r d c"), [], [bwa])
    bar = P.sb([1, 2, 128])
    P.dma("sp", bar[:], barow.rearrange("d o c -> o d c"), [], [bwa])
    qs = [(P.sb([128, 512]), Buf()) for _ in range(2)]
    ks = [(P.sb([128, 512]), Buf()) for _ in range(2)]
    as_ = [(P.sb([16, 512]), Buf()) for _ in range(2)]
    tk = [(P.sb([64, 8, 640]), Buf()) for _ in range(2)]
    pla = (P.ps([64, 128]), PBuf()); pb = (P.ps([128, 64]), PBuf()); pdd = (P.ps([64, 128]), PBuf())
    pat = (P.ps([64, 64]), PBuf()); po = (P.ps([64, 256]), PBuf()); pS = (P.ps([128, 256]), PBuf())
    la = P.sb([64, 128]); bla = Buf()
    e1 = P.sb([128, 64]); be1 = Buf(); e2 = P.sb([128, 64]); be2 = Buf(); e3 = P.sb([64, 128]); be3 = Buf()
    qd = P.sb([128, 64], BF16); bqd = Buf(); ki = P.sb([128, 64], BF16); bki = Buf(); ke = P.sb([64, 128], BF16); bke = Buf()
    vb = P.sb([64, 256], BF16); bvb = Buf(); am = P.sb([64, 64], BF16); bam = Buf()
    S = P.sb([128, 256]); bS = Buf(); Sb = P.sb([128, 256], BF16); bSb = Buf()
    ost = [(P.sb([64, 256]), Buf()) for _ in range(2)]
    sgroups = [(0, NCTX)] + [(NCTX + g * 512, min(512, TL - g * 512)) for g in range((TL + 511) // 512)]
    n = 0
    nc_ = 0
    for d_ in range(2):
        aT = afT if d_ == 0 else abT
        oD = oF if d_ == 0 else oB
        P.op("dve", lambda e: e.memset(S[:], 0.0), [], [bS])
        P.op("dve", lambda e: e.memset(Sb[:], 0.0), [], [bSb])
        order = sgroups if d_ == 0 else [sgroups[0]] + sgroups[:0:-1]
        for (g0, gn_) in order:
            q_, bq_ = qs[n % 2]; k_, bk_ = ks[n % 2]; a_, ba_ = as_[n % 2]; t_, bt_ = tk[n % 2]
            n += 1
            nch = gn_ // 64
            P.dma("sp", q_[:, 0:gn_], qT[:, g0:g0 + gn_], [], [bq_])
            P.dma("sp", k_[:, 0:gn_], kT[:, g0:g0 + gn_], [], [bk_])
            P.dma("sp", a_[:, 0:gn_], aT[:, g0:g0 + gn_], [], [ba_])
            P.dma("sp", t_[:, 0:nch, :], tokmaj[g0:g0 + gn_, :].rearrange("(c p) f -> p c f", p=64), [], [bt_])
            chunks = range(nch) if d_ == 0 else range(nch - 1, -1, -1)
            for c in chunks:
                cs = slice(c * 64, (c + 1) * 64)
                P.mm(pla[0][:], a_[:, cs], wa[:, d_, :], True, False, [ba_, bwa], [pla[1]])
                P.mm(pla[0][:], ones[0:1, :], bar[0:1, d_, :], False, True, [bcm, bwa], [pla[1]])
                P.act(la[:], pla[0][:], AF.Exp, [pla[1]], [bla], scale=-1.0)
                P.act(la[:], la[:], AF.Ln, [bla], [bla], bias=1.0)
                P.mm(pb[0][:], la[:], cms[:, 3 * d_ + 0, :], True, True, [bla, bcm], [pb[1]])
                P.mm(pdd[0][:], cms[:, 3 * d_ + 1, :], la[:], True, True, [bla, bcm], [pdd[1]])
                P.act(e1[:], pb[0][:], AF.Exp, [pb[1]], [be1])
                P.act(e2[:], pb[0][:], AF.Exp, [pb[1]], [be2], scale=-1.0)
                P.act(e3[:], pdd[0][:], AF.Exp, [pdd[1]], [be3])
                P.stt(qd[:], q_[:, cs], 128.0 ** -0.5, e1[:], ALU.mult, ALU.mult, [bq_, be1], [bqd])
                P.tt(ki[:], k_[:, cs], e2[:], ALU.mult, [bk_, be2], [bki])
                P.tt(ke[:], t_[:, c, 0:128], e3[:], ALU.mult, [bt_, be3], [bke])
                P.cp("act", vb[:], t_[:, c, 128:384], [bt_], [bvb])
                P.mm(pat[0][:], ki[:], qd[:], True, True, [bki, bqd], [pat[1]])
                P.tt(am[:], pat[0][:], cms[:, 3 * d_ + 2, :], ALU.mult, [pat[1], bcm], [bam])
                P.mm(po[0][:], am[:], vb[:], True, False, [bam, bvb], [po[1]])
                P.mm(po[0][:], qd[:], Sb[:], False, True, [bqd, bSb], [po[1]])
                P.mm(pS[0][:], ke[:], vb[:], True, True, [bke, bvb], [pS[1]])
                el = e1[:, 63:64] if d_ == 0 else e1[:, 0:1]
                P.stt(S[:], S[:], el, pS[0][:], ALU.mult, ALU.add, [bS, be1, pS[1]], [bS])
                P.cp("act", Sb[:], S[:], [bS], [bSb])
                o_, bo_ = ost[nc_ % 2]
                nc_ += 1
                P.cp("act", o_[:], po[0][:], [po[1]], [bo_])
                P.dma("pool", oD[g0 + c * 64:g0 + (c + 1) * 64, :], o_[:], [bo_], [])
    P.flush()
    all_dma_barrier(P)
    out_tm = None
    if mixloc is not None:
        def out_tm(t, h, o2, bo2):
            mr0 = (TL + t * 128) if t < NCTX // 128 else (t * 128 - NCTX)
            P.dma("pool", mixloc[mr0:mr0 + 128, 0:256], o2[:], [bo2], [])
    emit_finish(P, T, 1, tokmaj, 384, [oF], [oB], gn_bc[:, :], AF.Silu, ident[:, :], mixT, out_tm=out_tm)


def build_oddmix(TL, NCTX=256):
    P = Prog()
    emit_oddmix(P, "", TL, NCTX)
    P.begin()
    P.flush(final=True)
    return P.nc


def emit_oddmix(P, pre, TL, NCTX=256, xsrc=None, mixloc=None):
    T = NCTX + TL
    GW = 64
    rows = TL // GW
    I = lambda n, s, d=F32: P.dram(pre + n, s, d, kind="ExternalInput")
    xin = I("xin", [T, D]) if xsrc is None else None
    ccol = I("ccol", [128, 16]); cccol = I("cccol", [128, 16])
    wmod = I("wmod", [D, 4096]); bmod = I("bmod", [1, 4096])
    win = I("win", [D, 1544])
    bgb = I("bgb", [64, 2, 4]); gn_bc = I("gn_bc", [128, 256])
    ident = I("ident", [128, 128])
    cm = I("cm", [10, 64, 64])
    mixT = P.dram(pre + "mixT", [512, T], BF16, kind="ExternalOutput") if mixloc is None else None
    tokmaj = P.dram(pre + "tokmaj", [T, 1288], F32)
    qT = [P.dram(pre + "qT%d" % i, [128, T], F32) for i in range(2)]
    kT = [P.dram(pre + "kT%d" % i, [128, T], F32) for i in range(2)]
    hF = [P.dram(pre + "hF%d" % i, [T, 256], F32) for i in range(2)]
    hB = [P.dram(pre + "hB%d" % i, [T, 256], F32) for i in range(2)]
    load_x = None
    if xsrc is not None:
        G = xsrc["G"]; SQ = TL // 4; CQ = NCTX // 4; RQ = rows // 4
        G1v = G.rearrange("(tl r w) d -> tl r w d", r=4, w=128)

        def load_x(t, xt, bxt):
            if t < NCTX // 128:
                for hf_ in range(128 // CQ):
                    r = t * (128 // CQ) + hf_
                    P.dma("sp", xt[hf_ * CQ:(hf_ + 1) * CQ, :], G1v[SQ // 128, r, 0:CQ, :], [xsrc["bG"]], [bxt])
            else:
                j0 = (t - NCTX // 128) * 128
                pos = j0
                while pos < j0 + 128:
                    c_, rw = divmod(pos, rows)
                    r, lr = divmod(rw, RQ)
                    n_ = min(RQ - lr, j0 + 128 - pos)
                    ip = c_ * RQ + lr
                    P.dma("sp", xt[pos - j0:pos - j0 + n_, :], G1v[ip // 128, r, ip % 128:ip % 128 + n_, :], [xsrc["bG"]], [bxt])
                    pos += n_
    mcol = P.sb([128, 2, 32]); bmcol = Buf()
    emit_mod_rows(P, [ccol[:, :], cccol[:, :]], wmod, bmod[:, :], 4096, None, (4, 5, 6, 7), ident=ident[:, :], mcol=mcol, bmcol=bmcol,
                  col_groups={g: g for g in range(8)})
    fm = [(1288, 128, qT[0]), (1416, 128, qT[1]), (0, 128, kT[0]), (128, 128, kT[1])]
    emit_projection(P, xin, T, NCTX // 128, win, 1544, mcol, bmcol, ident[:, :], 1288, tokmaj, fm, load_x=load_x)
    all_dma_barrier(P)

    P.begin()
    SC = 128.0 ** -0.5
    cms = P.sb([64, 10, 64]); bcm = Buf()
    P.dma("sp", cms[:], cm.rearrange("a s t -> s a t"), [], [bcm])
    ones = P.sb([64, 128]); P.op("dve", lambda e: e.memset(ones[:], 1.0), [], [bcm])
    bg = P.sb([64, 2, 4]); P.dma("sp", bg[:], bgb[:, :, :], [], [bcm])
    qs = [(P.sb([128, 512]), Buf()) for _ in range(2)]
    ks = [(P.sb([128, 512]), Buf()) for _ in range(2)]
    qb = [(P.sb([128, 512], BF16), Buf()) for _ in range(2)]
    kb = [(P.sb([128, 512], BF16), Buf()) for _ in range(2)]
    tk = [(P.sb([64, 8, 1288]), Buf()) for _ in range(2)]
    pD = (P.ps([64, 64]), PBuf()); pG = (P.ps([128, 64]), PBuf()); pc = (P.ps([128, 8]), PBuf())
    pQK = (P.ps([64, 64]), PBuf()); pT = (P.ps([64, 64]), PBuf()); pN = (P.ps([64, 257]), PBuf())
    pQC = (P.ps([64, 257]), PBuf()); pC = (P.ps([128, 257]), PBuf())
    gc = P.sb([64, 8]); bgc = Buf()
    A1 = P.sb([64, 64]); bA1 = Buf(); Fb = P.sb([64, 128]); bFb = Buf(); Ib = P.sb([64, 128]); bIb = Buf()
    Dk = P.sb([64, 64]); bDk = Buf(); wi = P.sb([64, 64]); bwi = Buf(); sm = P.sb([64, 64]); bsm = Buf()
    smT = P.sb([64, 64], BF16); bsmT = Buf()
    v = P.sb([128, 16]); bv = Buf()
    va = [(P.sb([64, 257], BF16), Buf()) for _ in range(2)]
    for va_, bva_ in va:
        P.op("dve", lambda e, va_=va_: e.memset(va_[:], 1.0), [], [bva_])
    nA = P.sb([64, 257]); bnA = Buf(); tot = P.sb([64, 257]); btot = Buf()
    kw = P.sb([64, 128], BF16); bkw = Buf()
    C = P.sb([128, 257]); bC = Buf(); Cb = P.sb([128, 257], BF16); bCb = Buf()
    m = P.sb([128, 1]); bm = Buf()
    ho = [(P.sb([64, 256]), Buf()) for _ in range(2)]
    sgroups = [(0, NCTX)] + [(NCTX + g * 512, min(512, TL - g * 512)) for g in range((TL + 511) // 512)]
    n = 0; nc_ = 0
    col = lambda i, p=64: v[0:p, i:i + 1]
    V = [bv]
    for h in range(2):
        for d_ in range(2):
            hD = hF[h] if d_ == 0 else hB[h]
            cb_ = 5 * d_
            P.op("dve", lambda e: e.memset(C[:], 0.0), [], [bC])
            P.op("dve", lambda e: e.memset(Cb[:], 0.0), [], [bCb])
            P.op("dve", lambda e: e.memset(m[:], NEG), [], [bm])
            order = sgroups if d_ == 0 else [sgroups[0]] + sgroups[:0:-1]
            for (g0, gn_) in order:
                q_, bq_ = qs[n % 2]; k_, bk_ = ks[n % 2]; t_, bt_ = tk[n % 2]; qb_, bqb_ = qb[n % 2]; kb_, bkb_ = kb[n % 2]
                n += 1
                nch = gn_ // 64
                P.dma("sp", q_[:, 0:gn_], qT[h][:, g0:g0 + gn_], [], [bq_])
                P.dma("sp", k_[:, 0:gn_], kT[h][:, g0:g0 + gn_], [], [bk_])
                P.dma("sp", t_[:, 0:nch, :], tokmaj[g0:g0 + gn_, :].rearrange("(c p) f -> p c f", p=64), [], [bt_])
                P.cp("dve", qb_[:, 0:gn_], q_[:, 0:gn_], [bq_], [bqb_])
                P.cp("act", kb_[:, 0:gn_], k_[:, 0:gn_], [bk_], [bkb_])
                chunks = range(nch) if d_ == 0 else range(nch - 1, -1, -1)
                for c in chunks:
                    cs = slice(c * 64, (c + 1) * 64)
                    va_, bva_ = va[nc_ % 2]; ho_, bho_ = ho[nc_ % 2]
                    nc_ += 1
                    P.tt(gc[:, 0:4], t_[:, c, 1280 + 4 * h:1284 + 4 * h], bg[:, h, :], ALU.add, [bt_, bcm], [bgc])
                    ic = gc[:, 2 * d_:2 * d_ + 1]; fc = gc[:, 4:5]
                    P.act(fc, gc[:, 2 * d_ + 1:2 * d_ + 2], AF.Exp, [bgc], [bgc], scale=-1.0)
                    P.act(fc, fc, AF.Ln, [bgc], [bgc], bias=1.0)
                    P.ts(fc, fc, -1.0, None, ALU.mult, None, [bgc], [bgc])
                    P.ts(A1[:], cms[:, cb_ + 0, :], fc, None, ALU.mult, None, [bcm, bgc], [bA1])
                    P.ts(Fb[:], ones[:], fc, None, ALU.mult, None, [bcm, bgc], [bFb])
                    P.ts(Ib[:], ones[:], ic, None, ALU.mult, None, [bcm, bgc], [bIb])
                    P.mm(pD[0][:], A1[:], ones[:, 0:64], True, False, [bA1, bcm], [pD[1]])
                    P.mm(pD[0][:], Fb[:, 0:64], cms[:, cb_ + 1, :], False, False, [bFb, bcm], [pD[1]])
                    P.mm(pD[0][:], Ib[:, 0:64], cms[:, cb_ + 4, :], False, True, [bIb, bcm], [pD[1]])
                    P.mm(pG[0][:], Fb[:], cms[:, cb_ + 2, :], True, False, [bFb, bcm], [pG[1]])
                    P.mm(pG[0][:], Ib[:], cms[:, cb_ + 4, :], False, True, [bIb, bcm], [pG[1]])
                    P.mm(pc[0][0:64, 0:1], cms[:, cb_ + 0, :], fc, True, True, [bcm, bgc], [pc[1]])
                    P.mm(pc[0][:, 1:2], ones[:], fc, True, True, [bcm, bgc], [pc[1]])
                    P.mm(pc[0][0:64, 2:3], cms[:, cb_ + 2, :], fc, True, True, [bcm, bgc], [pc[1]])
                    P.mm(pQK[0][:], qb_[:, cs], kb_[:, cs], True, True, [bqb_, bkb_], [pQK[1]])
                    P.tt(Dk[:], pD[0][:], cms[:, cb_ + 3, :], ALU.add, [pD[1], bcm], [bDk])
                    P.op("dve", lambda e: e.reduce_max(col(0), Dk[:], AX.X), [bDk], V)
                    P.tt(col(1), pc[0][0:64, 0:1], m[0:64, :], ALU.add, [pc[1], bm], V)
                    P.tt(col(2), col(1), col(0), ALU.max, V, V)
                    P.ts(col(3), col(2), -1.0, None, ALU.mult, None, V, V)
                    P.act(wi[:], Dk[:], AF.Exp, [bDk, bv], [bwi], bias=col(3))
                    P.act(col(4), col(1), AF.Exp, V, V, bias=col(3))
                    P.act(col(5), col(3), AF.Exp, V, V)
                    P.stt(sm[:], pQK[0][:], SC, wi[:], ALU.mult, ALU.mult, [pQK[1], bwi], [bsm])
                    P.tr(pT[0][:], sm[:], cms[:, cb_ + 4, :], [bsm, bcm], [pT[1]])
                    P.cp("act", smT[:], pT[0][:], [pT[1]], [bsmT])
                    P.cp("dve", va_[:, 0:256], t_[:, c, 256 + h * 256:512 + h * 256], [bt_], [bva_])
                    P.mm(pN[0][:], smT[:], va_[:], True, True, [bsmT, bva_], [pN[1]])
                    P.mm(pQC[0][:], qb_[:, cs], Cb[:], True, True, [bqb_, bCb], [pQC[1]])
                    P.cp("act", nA[:], pN[0][:], [pN[1]], [bnA])
                    P.stt(tot[:], pQC[0][:], col(4), nA[:], ALU.mult, ALU.add, [pQC[1], bv, bnA], [btot])
                    P.ts(col(13), tot[:, 256:257], -1.0, None, ALU.mult, None, [btot], V)
                    P.tt(col(6), tot[:, 256:257], col(13), ALU.max, [btot, bv], V)
                    P.tt(col(6), col(6), col(5), ALU.max, V, V)
                    P.op("dve", lambda e: e.reciprocal(col(6), col(6)), V, V)
                    P.ts(ho_[:], tot[:, 0:256], col(6), None, ALU.mult, None, [btot, bv], [bho_])
                    P.dma("pool", hD[g0 + c * 64:g0 + (c + 1) * 64, :], ho_[:], [bho_], [])
                    P.op("dve", lambda e: e.reduce_max(col(7, 128), pG[0][:], AX.X), [pG[1]], V)
                    P.tt(col(8, 128), pc[0][:, 1:2], m[:], ALU.add, [pc[1], bm], V)
                    P.tt(col(9, 128), col(8, 128), col(7, 128), ALU.max, V, V)
                    P.ts(col(10, 128), col(9, 128), -1.0, None, ALU.mult, None, V, V)
                    P.tt(col(11), pc[0][0:64, 2:3], ic, ALU.add, [pc[1], bgc], V)
                    P.act(col(11), col(11), AF.Exp, V, V, bias=col(10))
                    P.act(col(12, 128), col(8, 128), AF.Exp, V, V, bias=col(10, 128))
                    P.ts(kw[:], t_[:, c, h * 128:(h + 1) * 128], col(11), SC, ALU.mult, ALU.mult, [bt_, bv], [bkw])
                    P.mm(pC[0][:], kw[:], va_[:], True, True, [bkw, bva_], [pC[1]])
                    P.stt(C[:], C[:], col(12, 128), pC[0][:], ALU.mult, ALU.add, [bC, bv, pC[1]], [bC])
                    P.cp("act", Cb[:], C[:], [bC], [bCb])
                    P.cp("dve", m[:], col(9, 128), V, [bm])
    P.flush()
    all_dma_barrier(P)
    out_tm = None
    if mixloc is not None:
        mv_ = mixloc.rearrange("(r c) f -> c r f", c=GW)

        def out_tm(t, h, o2, bo2):
            j0 = (t - NCTX // 128) * 128
            pos = j0
            while pos < j0 + 128:
                c_, rw = divmod(pos, rows)
                n_ = min(rows - rw, j0 + 128 - pos)
                P.dma("pool", mv_[c_, rw:rw + n_, h * 256:(h + 1) * 256], o2[pos - j0:pos - j0 + n_, :], [bo2], [])
                pos += n_
    emit_finish(P, T, 2, tokmaj, 768, hF, hB, gn_bc[:, :], AF.Sigmoid, ident[:, :], mixT, out_tm=out_tm,
                t_start=(NCTX // 128 if mixloc is not None else 0))


def col16(v):
    return np.ascontiguousarray(v.reshape(16, 128).T)


def bc(v, n=128):
    return np.ascontiguousarray(np.broadcast_to(v, (n, v.shape[-1])))


def tri_consts():
    s = np.arange(64)[:, None]; t = np.arange(64)[None, :]
    f = np.float32
    sixteenth = np.float32(-0.0625)
    return np.stack([np.where(s <= t, sixteenth, 0).astype(f), np.where(s > t, sixteenth, 0).astype(f), (s <= t).astype(f),
                     np.where(s >= t, sixteenth, 0).astype(f), np.where(s < t, sixteenth, 0).astype(f), (s >= t).astype(f)])


def odd_consts():
    u = np.arange(64)[:, None]; t = np.arange(64)[None, :]
    f = np.float32
    out = []
    for d in range(2):
        tri = (u <= t) if d == 0 else (u >= t)
        st = (u > t) if d == 0 else (u < t)
        ok = (t <= u) if d == 0 else (t >= u)
        out += [tri.astype(f), np.where(tri, -1.0, 0.0).astype(f), st.astype(f), np.where(ok, 0, NEG).astype(f), np.eye(64, dtype=f)]
    return np.stack(out)


def even_inputs(inp, j, x_b, ctx_b, c_b, c_ctx, hg):
    l = 2 * j
    w = inp["ev_w_in"][j]
    q0, k0, v0, r0, af0, ab0, xr0, xg0 = 0, 512, 1024, 2048, 3072, 3088, 3104, 4128
    h = hg
    cols = np.concatenate([np.arange(k0 + h * 128, k0 + (h + 1) * 128), np.arange(v0 + h * 256, v0 + (h + 1) * 256),
                           np.arange(r0 + h * 256, r0 + (h + 1) * 256), np.arange(q0 + h * 128, q0 + (h + 1) * 128),
                           np.arange(xr0 + h * 256, xr0 + (h + 1) * 256), np.arange(xg0 + h * 256, xg0 + (h + 1) * 256),
                           np.arange(af0, af0 + 16), np.arange(ab0, ab0 + 16)])
    ch = slice(h * 256, (h + 1) * 256)

    def colblk(v):
        return v[..., ch].reshape(v.shape[:-1] + (2, 128))
    return dict(
        xin=np.ascontiguousarray(np.concatenate([ctx_b, x_b], 0)), ccol=col16(c_b), cccol=col16(c_ctx),
        wmod=np.ascontiguousarray(inp["w_mod"][l][:, 0:4096]), bmod=np.ascontiguousarray(inp["b_mod"][l][None, 0:4096]),
        win=np.ascontiguousarray(w[:, cols]),
        wa2=np.ascontiguousarray(inp["gla_w_a2"][j][:, :, h * 128:(h + 1) * 128]),
        barow=np.ascontiguousarray(inp["gla_b_a"][j][:, None, h * 128:(h + 1) * 128]),
        gn_bc=bc(inp["gla_norm"][j]),
        convw=np.ascontiguousarray(colblk(inp["lru_conv_w"][j]).transpose(2, 1, 0)),
        convb=np.ascontiguousarray(colblk(inp["lru_conv_b"][j]).T),
        wr_=np.ascontiguousarray(inp["lru_w_r"][j][:, 2 * h:2 * h + 2]), wi_=np.ascontiguousarray(inp["lru_w_i"][j][:, 2 * h:2 * h + 2]),
        brc=np.ascontiguousarray(colblk(inp["lru_b_r"][j]).transpose(2, 0, 1)), bic=np.ascontiguousarray(colblk(inp["lru_b_i"][j]).transpose(2, 0, 1)),
        lamc=np.ascontiguousarray(colblk(inp["lru_lam"][j]).transpose(2, 0, 1)),
        ident=np.eye(128, dtype=np.float32), cm=tri_consts())


def odd_inputs(inp, j, xscan_b, c_b, c_ctx, hg):
    l = 2 * j + 1
    w = inp["od_w_in"][j]
    hs = [2 * hg, 2 * hg + 1]
    q0, k0, v0, o0, g0 = 0, 1024, 2048, 4096, 6144
    cols = np.concatenate([np.arange(k0 + h * 128, k0 + (h + 1) * 128) for h in hs] + [np.arange(v0 + h * 256, v0 + (h + 1) * 256) for h in hs]
                          + [np.arange(o0 + h * 256, o0 + (h + 1) * 256) for h in hs] + [np.array([g0 + h, g0 + 8 + h, g0 + 16 + h, g0 + 24 + h]) for h in hs]
                          + [np.arange(q0 + h * 128, q0 + (h + 1) * 128) for h in hs])
    bgate = inp["mlstm_b_gate"][j]
    bgb = np.ascontiguousarray(np.broadcast_to(np.stack([bgate[:, h] for h in hs])[None], (64, 2, 4))).astype(np.float32)
    return dict(xin=(None if xscan_b is None else np.ascontiguousarray(xscan_b)), ccol=col16(c_b), cccol=col16(c_ctx),
                wmod=np.ascontiguousarray(inp["w_mod"][l][:, 0:4096]), bmod=np.ascontiguousarray(inp["b_mod"][l][None, 0:4096]),
                win=np.ascontiguousarray(w[:, cols]), bgb=bgb, gn_bc=bc(inp["mlstm_norm"][j]),
                ident=np.eye(128, dtype=np.float32), cm=odd_consts())


def tok_inputs(inp, l, mixT, xres, c_b, c_ctx, wout):
    return dict(mixT=mixT, xres=xres, ccol=col16(c_b), cccol=col16(c_ctx),
                wmod=np.ascontiguousarray(inp["w_mod"][l][:, 4096:12288]), bmod=np.ascontiguousarray(inp["b_mod"][l][None, 4096:12288]),
                lnp=np.stack([bc(inp["ln_g"][l, 0]), bc(inp["ln_b"][l, 0]), bc(inp["ln_g"][l, 1]), bc(inp["ln_b"][l, 1])]),
                wout=wout, wr=np.ascontiguousarray(np.concatenate([inp["moe_w_group"][l], inp["moe_w_expert"][l]], 1)),
                brb=bc(np.concatenate([inp["moe_b_group"][l], inp["moe_b_expert"][l]])),
                wg=inp["moe_w_gate"][l], wu=inp["moe_w_up"][l], wd=inp["moe_w_down"][l], ident=np.eye(128, dtype=np.float32))


def gather_chunks(P, src, nrows, CR, dst):
    all_dma_barrier(P)
    bG = Buf()
    evs = []
    r0 = 0
    off = 0
    while r0 < nrows:
        n = min(CR, nrows - r0)
        evs.append(P.coll(src[r0:r0 + n, :], dst[off:off + 4 * n, :], [], [bG]))
        r0 += n
        off += 4 * n
    for q in ("sp", "act", "pool"):
        P._waits(q, [], [], evs)
    return bG


def build_fused(S, NCTX, NE=32):
    P = Prog()
    T = NCTX + S
    SQ = S // 4
    CQ = NCTX // 4
    NT0 = SQ // 128 + 1
    NP0 = NT0 * 128
    NT1 = SQ // 128
    CR = min(1024, SQ)
    mix0 = P.dram("mix0", [T, 512], BF16); G0 = P.dram("G0", [4 * T, 512], BF16)
    emit_evenmix(P, "e0_", S, NCTX, mixloc=mix0)
    bG0 = gather_chunks(P, mix0, T, CR, G0)
    xout0 = P.dram("xout0", [NP0, D], F32); G1 = P.dram("G1", [4 * NP0, D], F32)
    md = dict(G=G0, CR=CR, S=S, SQ=SQ, NCTX=NCTX, CQ=CQ, bG=bG0)
    RQ = S // 64 // 4
    emit_tokloc(P, "t0_", NT0, True, mix=md, xout=xout0, NE=NE, out_cm=RQ)
    bG1 = gather_chunks(P, xout0, NP0, 128, G1)
    mix1 = P.dram("mix1", [S, 512], BF16); Gm1 = P.dram("Gm1", [4 * S, 512], BF16)
    emit_oddmix(P, "o1_", S, NCTX, xsrc=dict(G=G1, bG=bG1), mixloc=mix1)
    bGm1 = gather_chunks(P, mix1, S, CR, Gm1)
    md1 = dict(G=Gm1, CR=CR, S=S, SQ=SQ, NCTX=NCTX, CQ=CQ, bG=bGm1)
    emit_tokloc(P, "t1_", NT1, False, mix=md1, xres=xout0, NE=NE, xres_cm=RQ)
    P.begin()
    P.flush(final=True)
    return P.nc


def fused_inputs(inp, b, q, S, NCTX, NE=32):
    x = inp["x"]; ctx = inp["ctx"]; c = inp["c"]; c_ctx = inp["c_ctx"]
    SQ = S // 4; CQ = NCTX // 4
    NP0 = (SQ // 128 + 1) * 128
    d = {}
    for k, v in even_inputs(inp, 0, x[b], ctx[b], c[b], c_ctx, q).items():
        d["e0_" + k] = v
    xr = np.zeros((NP0, D), np.float32)
    xr[0:SQ] = x[b, q * SQ:(q + 1) * SQ]
    xr[NP0 - 128:NP0 - 128 + CQ] = ctx[b, q * CQ:(q + 1) * CQ]
    perm0 = np.concatenate([np.concatenate([np.arange(r * 256, (r + 1) * 256), np.arange(1024 + r * 256, 1024 + (r + 1) * 256)]) for r in range(4)])
    for k, v in tok_inputs(inp, 0, None, xr, c[b], c_ctx, np.ascontiguousarray(inp["ev_w_out"][0][perm0])).items():
        if k != "mixT":
            d["t0_" + k] = v[:NE] if k in ("wg", "wu", "wd") else v
    for k, v in odd_inputs(inp, 0, None, c[b], c_ctx, q).items():
        if k != "xin":
            d["o1_" + k] = v
    for k, v in tok_inputs(inp, 1, None, None, c[b], c_ctx, inp["od_w_out"][0]).items():
        if k not in ("mixT", "xres", "cccol"):
            d["t1_" + k] = v[:NE] if k in ("wg", "wu", "wd") else v
    return d


def kernel(_NE=32, **inp):
    inp = {k: np.asarray(v) for k, v in inp.items()}
    x = inp["x"]
    B, S, _ = x.shape
    NCTX = inp["ctx"].shape[1]
    SQ = S // 4
    cores = [(b, q) for b in range(B) for q in range(4)]
    nc = build_fused(S, NCTX, _NE)
    res = run_bass_kernel_spmd(nc, [fused_inputs(inp, b, q, S, NCTX, _NE) for (b, q) in cores], core_ids=list(range(len(cores))))
    out = np.zeros_like(x)
    for i, (b, q) in enumerate(cores):
        out[b, q * SQ:(q + 1) * SQ] = res.results[i]["t1_xout"][0:SQ]
    return out
```
